# Optimizing a Trainium2 kernel written in Bass

```python
import math
import jax
import jax.numpy as jnp
from jax import lax
import numpy as np

D_MODEL = 1024
BATCH = 2
SEQ = 16384
DEPTH = 2

GRID_W = 64
CTX_LEN = 256
N_EVEN = (DEPTH + 1) // 2
N_ODD = DEPTH // 2

CONV_WIDTH = D_MODEL // 2
CONV_WIDTH_TAPS = 31
DIFF_HEAD_DIM = 64
DIFF_V_DIM = 2 * DIFF_HEAD_DIM
DIFF_HEADS = (D_MODEL // 2) // DIFF_V_DIM
QK_W = DIFF_HEADS * 2 * DIFF_HEAD_DIM
V_W = DIFF_HEADS * DIFF_V_DIM
EV_A0 = 2 * QK_W + V_W
EVEN_IN = EV_A0 + 2 * CONV_WIDTH
EVEN_OUT = CONV_WIDTH + V_W
ROPE_BASE = 10000.0
Q_BLOCK = 128

CHUNK = 128
GMLP_GROUPS = 4
GMLP_W = D_MODEL // 2
GMLP_GROUP_DIM = GMLP_W // GMLP_GROUPS
FOURIER_GROUPS = 4
FOURIER_W = D_MODEL // 2
FOURIER_GROUP_DIM = FOURIER_W // FOURIER_GROUPS
ODD_IN = 2 * GMLP_W + FOURIER_W
ODD_OUT = GMLP_W + FOURIER_W

N_EXPERTS = 32
TOP_K = 4
D_EXPERT = D_MODEL
SWIGLU_LIMIT = 7.0
SWIGLU_ALPHA = 1.702
EXPERT_BLOCK = 128

LN_EPS = 1e-5
DEEPNORM_ALPHA = (2 * DEPTH) ** 0.25
DEEPNORM_BETA = (8 * DEPTH) ** -0.25

kernel_name = 'hybrid_conformer_diffattn_gmlp_fnet_moe_dit'


def layer_norm(x, g, b):
    xf = x.astype(jnp.float32)
    mu = jnp.mean(xf, -1, keepdims=True)
    var = jnp.mean(jnp.square(xf - mu), -1, keepdims=True)
    return ((xf - mu) * lax.rsqrt(var + LN_EPS) * g.astype(jnp.float32) + b.astype(jnp.float32)).astype(x.dtype)


def rms_norm(x, g):
    xf = x.astype(jnp.float32)
    return (xf * lax.rsqrt(jnp.mean(xf * xf, -1, keepdims=True) + LN_EPS) * g.astype(jnp.float32)).astype(x.dtype)


def modulate(x, shift, scale):
    return x * (1.0 + scale) + shift


def axial_rope_tables(length):
    rows = length // GRID_W
    row = jnp.repeat(jnp.arange(rows, dtype=jnp.float32), GRID_W)
    col = jnp.tile(jnp.arange(GRID_W, dtype=jnp.float32), rows)
    n_freq = DIFF_HEAD_DIM // 4
    inv_freq = ROPE_BASE ** (-jnp.arange(n_freq, dtype=jnp.float32) / n_freq)
    ang = jnp.stack([row[:, None] * inv_freq, col[:, None] * inv_freq], axis=1)
    return jnp.cos(ang), jnp.sin(ang)


def apply_axial_rope(x, cos, sin):
    xs = x.astype(jnp.float32).reshape(x.shape[:-1] + (2, 2, DIFF_HEAD_DIM // 4))
    x1, x2 = xs[..., 0, :], xs[..., 1, :]
    c, s = cos[:, None, None], sin[:, None, None]
    out = jnp.stack([x1 * c - x2 * s, x1 * s + x2 * c], axis=-2)
    return out.reshape(x.shape).astype(x.dtype)


def heads_qk(z):
    return z.reshape(z.shape[:2] + (DIFF_HEADS, 2, DIFF_HEAD_DIM))


def heads_v(z):
    return z.reshape(z.shape[:2] + (DIFF_HEADS, DIFF_V_DIM))


def diff_lambda(lq1, lk1, lq2, lk2, lam_init):
    f32 = jnp.float32
    return (jnp.exp(jnp.sum(lq1.astype(f32) * lk1.astype(f32)))
            - jnp.exp(jnp.sum(lq2.astype(f32) * lk2.astype(f32))) + lam_init)


def diff_attend(q, k, v, lam):
    s = jnp.einsum('bqhcd,bkhcd->bhcqk', q, k, preferred_element_type=jnp.float32) * (DIFF_HEAD_DIM ** -0.5)
    p = jax.nn.softmax(s, axis=-1)
    w = p[:, :, 0] - lam * p[:, :, 1]
    return jnp.einsum('bhqk,bkhv->bqhv', w.astype(v.dtype), v)


def diff_attend_blocks(q, k, v, lam):
    b, length = q.shape[:2]
    n_blk = length // Q_BLOCK
    qb = jnp.moveaxis(q.reshape((b, n_blk, Q_BLOCK) + q.shape[2:]), 1, 0)
    out = lax.map(lambda qi: diff_attend(qi, k, v, lam), qb)
    return jnp.moveaxis(out, 0, 1).reshape((b, length) + out.shape[3:])


def diff_heads_out(o, g, lam_init):
    b, length = o.shape[:2]
    return (rms_norm(o, g) * (1.0 - lam_init)).reshape(b, length, V_W)


def conformer_conv(z, conv_w, conv_b, ln_g, ln_b):
    a = z[..., EV_A0:EV_A0 + CONV_WIDTH] * jax.nn.sigmoid(z[..., EV_A0 + CONV_WIDTH:])
    y = lax.conv_general_dilated(a, conv_w[:, None, :], window_strides=(1,),
                                 padding=[(CONV_WIDTH_TAPS // 2, CONV_WIDTH_TAPS // 2)],
                                 dimension_numbers=('NWC', 'WIO', 'NWC'),
                                 feature_group_count=CONV_WIDTH)
    return jax.nn.silu(layer_norm(y + conv_b, ln_g, ln_b))


def chunk_gmlp(u, v, ln_g, ln_b, ws, bs):
    b, length, _ = u.shape
    u = jax.nn.gelu(u)
    v = layer_norm(jax.nn.gelu(v), ln_g, ln_b)
    vc = v.reshape(b, length // CHUNK, CHUNK, GMLP_GROUPS, GMLP_GROUP_DIM)
    sv = jnp.einsum('gpq,bnqgc->bnpgc', ws, vc) + bs.T[:, :, None]
    return u * sv.reshape(b, length, GMLP_W)


def fourier_mix(f, ln_g, ln_b):
    b, length, _ = f.shape
    fg = f.reshape(b, length, FOURIER_GROUPS, FOURIER_GROUP_DIM)
    fg = layer_norm(fg, ln_g.reshape(FOURIER_GROUPS, FOURIER_GROUP_DIM), ln_b.reshape(FOURIER_GROUPS, FOURIER_GROUP_DIM))
    spec = jnp.fft.fft2(fg.astype(jnp.float32), axes=(1, 3), norm='ortho')
    return jnp.real(spec).astype(f.dtype).reshape(b, length, FOURIER_W)


def odd_mixer(z, gln_g, gln_b, ws, bs, fln_g, fln_b):
    spatial = chunk_gmlp(z[..., :GMLP_W], z[..., GMLP_W:2 * GMLP_W], gln_g, gln_b, ws, bs)
    spectral = fourier_mix(z[..., 2 * GMLP_W:], fln_g, fln_b)
    return jnp.concatenate([spatial, spectral], -1)


def expert_ffn(xb, wgu, bgu, wd, bd):
    gu = xb @ wgu + bgu
    x_glu, x_lin = jnp.split(gu, 2, axis=-1)
    x_glu = jnp.minimum(x_glu, SWIGLU_LIMIT)
    x_lin = jnp.clip(x_lin, -SWIGLU_LIMIT, SWIGLU_LIMIT)
    act = x_glu * jax.nn.sigmoid(SWIGLU_ALPHA * x_glu) * (x_lin + 1.0)
    return act @ wd + bd


def moe_channel_mixer(h, router_w, router_b, wgu, bgu, wd, bd):
    n_tok, d = h.shape
    logits = (h @ router_w + router_b).astype(jnp.float32)
    top_logit, top_e = lax.top_k(logits, TOP_K)
    gate = jax.nn.softmax(top_logit, axis=-1)
    n_asg = n_tok * TOP_K
    flat_e = top_e.reshape(-1)
    order = jnp.argsort(flat_e)
    sorted_e = flat_e[order]
    counts = jnp.bincount(flat_e, length=N_EXPERTS)
    padded = (counts + EXPERT_BLOCK - 1) // EXPERT_BLOCK * EXPERT_BLOCK
    pad_end = jnp.cumsum(padded)
    start = jnp.cumsum(counts) - counts
    dest = (pad_end - padded)[sorted_e] + jnp.arange(n_asg) - start[sorted_e]
    n_blk = -(-n_asg // EXPERT_BLOCK) + N_EXPERTS
    n_rows = n_blk * EXPERT_BLOCK
    row_tok = jnp.full((n_rows,), n_tok, jnp.int32).at[dest].set((order // TOP_K).astype(jnp.int32))
    row_gate = jnp.zeros((n_rows,), h.dtype).at[dest].set(gate.reshape(-1)[order].astype(h.dtype))
    blk_e = jnp.minimum(jnp.searchsorted(pad_end, jnp.arange(n_blk) * EXPERT_BLOCK, side='right'), N_EXPERTS - 1)
    h_pad = jnp.concatenate([h, jnp.zeros((1, d), h.dtype)], 0)
    xb = h_pad[row_tok].reshape(n_blk, EXPERT_BLOCK, d)
    yb = lax.map(lambda a: expert_ffn(a[0], wgu[a[1]], bgu[a[1]], wd[a[1]], bd[a[1]]), (xb, blk_e))
    y = yb.reshape(n_rows, d) * row_gate[:, None]
    return jnp.zeros((n_tok + 1, d), h.dtype).at[row_tok].add(y)[:n_tok]


def setup_inputs(seed: int = 0) -> dict:
    key = jax.random.key(seed)
    ks = iter(jax.random.split(key, 64))

    def nrm(shape, scale):
        return jax.random.normal(next(ks), shape, jnp.float32) * scale

    def gain(shape):
        return 1.0 + nrm(shape, 0.02)

    d, ne, no = D_MODEL, N_EVEN, N_ODD
    return {
        'x': nrm((BATCH, SEQ, d), 1.0),
        'c': nrm((BATCH, d), 1.0),
        'ctx': nrm((BATCH, CTX_LEN, d), 1.0),
        'c_ctx': nrm((d,), 1.0),
        'w_mod': nrm((DEPTH, d, 6 * d), 0.5 * d ** -0.5),
        'b_mod': nrm((DEPTH, 6 * d), 0.02),
        'ln1_g': gain((DEPTH, d)),
        'ln1_b': nrm((DEPTH, d), 0.02),
        'ln2_g': gain((DEPTH, d)),
        'ln2_b': nrm((DEPTH, d), 0.02),
        'ev_w_in': nrm((ne, d, EVEN_IN), d ** -0.5),
        'ev_w_out': nrm((ne, EVEN_OUT, d), DEEPNORM_BETA * EVEN_OUT ** -0.5),
        'conv_w': nrm((ne, CONV_WIDTH_TAPS, CONV_WIDTH), CONV_WIDTH_TAPS ** -0.5),
        'conv_b': nrm((ne, CONV_WIDTH), 0.02),
        'conv_ln_g': gain((ne, CONV_WIDTH)),
        'conv_ln_b': nrm((ne, CONV_WIDTH), 0.02),
        'lam_q1': nrm((ne, DIFF_HEAD_DIM), 0.1),
        'lam_k1': nrm((ne, DIFF_HEAD_DIM), 0.1),
        'lam_q2': nrm((ne, DIFF_HEAD_DIM), 0.1),
        'lam_k2': nrm((ne, DIFF_HEAD_DIM), 0.1),
        'diff_norm_g': gain((ne, DIFF_V_DIM)),
        'od_w_in': nrm((no, d, ODD_IN), d ** -0.5),
        'od_w_out': nrm((no, ODD_OUT, d), DEEPNORM_BETA * ODD_OUT ** -0.5),
        'gmlp_ln_g': gain((no, GMLP_W)),
        'gmlp_ln_b': nrm((no, GMLP_W), 0.02),
        'gmlp_ws': nrm((no, GMLP_GROUPS, CHUNK, CHUNK), CHUNK ** -0.5),
        'gmlp_bs': 1.0 + nrm((no, GMLP_GROUPS, CHUNK), 0.1),
        'four_ln_g': gain((no, FOURIER_W)),
        'four_ln_b': nrm((no, FOURIER_W), 0.02),
        'router_w': nrm((DEPTH, d, N_EXPERTS), d ** -0.5),
        'router_b': nrm((DEPTH, N_EXPERTS), 0.01),
        'w_gate_up': nrm((DEPTH, N_EXPERTS, d, 2 * D_EXPERT), d ** -0.5),
        'b_gate_up': nrm((DEPTH, N_EXPERTS, 2 * D_EXPERT), 0.02),
        'w_down': nrm((DEPTH, N_EXPERTS, D_EXPERT, d), DEEPNORM_BETA * D_EXPERT ** -0.5),
        'b_down': nrm((DEPTH, N_EXPERTS, d), 0.02),
    }


def reference(x, c, ctx, c_ctx, w_mod, b_mod, ln1_g, ln1_b, ln2_g, ln2_b,
              ev_w_in, ev_w_out, conv_w, conv_b, conv_ln_g, conv_ln_b,
              lam_q1, lam_k1, lam_q2, lam_k2, diff_norm_g,
              od_w_in, od_w_out, gmlp_ln_g, gmlp_ln_b, gmlp_ws, gmlp_bs,
              four_ln_g, four_ln_b,
              router_w, router_b, w_gate_up, b_gate_up, w_down, b_down):
    b, length, d = x.shape
    n_lat = b * length
    cos, sin = axial_rope_tables(length)
    h, hc = x, ctx
    for layer in range(DEPTH):
        even = layer % 2 == 0
        j = layer // 2
        ctx_out = any(l2 % 2 == 0 for l2 in range(layer + 1, DEPTH))
        mod = jax.nn.silu(c) @ w_mod[layer] + b_mod[layer]
        sh1, sc1, g1, sh2, sc2, g2 = jnp.split(mod[:, None, :], 6, axis=-1)
        if even or ctx_out:
            csh1, csc1, cg1, csh2, csc2, cg2 = jnp.split(jax.nn.silu(c_ctx) @ w_mod[layer] + b_mod[layer], 6)
            uc = modulate(hc, csh1, csc1)
        u = modulate(h, sh1, sc1)
        if even:
            w_in = ev_w_in[j]
            lam_init = 0.8 - 0.6 * math.exp(-0.3 * layer)
            lam = diff_lambda(lam_q1[j], lam_k1[j], lam_q2[j], lam_k2[j], lam_init)
            z = u @ w_in
            zc = uc @ (w_in if ctx_out else w_in[:, QK_W:EV_A0])
            zc_kv = zc[..., QK_W:EV_A0] if ctx_out else zc
            k_c = heads_qk(zc_kv[..., :QK_W])
            v_c = heads_v(zc_kv[..., QK_W:])
            q = apply_axial_rope(heads_qk(z[..., :QK_W]), cos, sin)
            k = apply_axial_rope(heads_qk(z[..., QK_W:2 * QK_W]), cos, sin)
            v = heads_v(z[..., 2 * QK_W:EV_A0])
            att = diff_attend_blocks(q, jnp.concatenate([k, k_c], 1), jnp.concatenate([v, v_c], 1), lam)
            conv = conformer_conv(z, conv_w[j], conv_b[j], conv_ln_g[j], conv_ln_b[j])
            y = jnp.concatenate([conv, diff_heads_out(att, diff_norm_g[j], lam_init)], -1) @ ev_w_out[j]
            if ctx_out:
                att_c = diff_attend(heads_qk(zc[..., :QK_W]), k_c, v_c, lam)
                conv_c = conformer_conv(zc, conv_w[j], conv_b[j], conv_ln_g[j], conv_ln_b[j])
                yc = jnp.concatenate([conv_c, diff_heads_out(att_c, diff_norm_g[j], lam_init)], -1) @ ev_w_out[j]
        else:
            y = odd_mixer(u @ od_w_in[j], gmlp_ln_g[j], gmlp_ln_b[j], gmlp_ws[j], gmlp_bs[j],
                          four_ln_g[j], four_ln_b[j]) @ od_w_out[j]
            if ctx_out:
                yc = odd_mixer(uc @ od_w_in[j], gmlp_ln_g[j], gmlp_ln_b[j], gmlp_ws[j], gmlp_bs[j],
                               four_ln_g[j], four_ln_b[j]) @ od_w_out[j]
        h = layer_norm(DEEPNORM_ALPHA * h + g1 * y, ln1_g[layer], ln1_b[layer])
        u2 = modulate(h, sh2, sc2).reshape(n_lat, d)
        if ctx_out:
            hc = layer_norm(DEEPNORM_ALPHA * hc + cg1 * yc, ln1_g[layer], ln1_b[layer])
            u2 = jnp.concatenate([u2, modulate(hc, csh2, csc2).reshape(-1, d)], 0)
        y2 = moe_channel_mixer(u2, router_w[layer], router_b[layer], w_gate_up[layer], b_gate_up[layer],
                               w_down[layer], b_down[layer])
        h = layer_norm(DEEPNORM_ALPHA * h + g2 * y2[:n_lat].reshape(b, length, d), ln2_g[layer], ln2_b[layer])
        if ctx_out:
            hc = layer_norm(DEEPNORM_ALPHA * hc + cg2 * y2[n_lat:].reshape(hc.shape), ln2_g[layer], ln2_b[layer])
    return h
```

```python
import math
import numpy as np
import ml_dtypes
import concourse.bass as bass
import concourse.mybir as mybir
from concourse.bass_utils import run_bass_kernel_spmd

F32 = mybir.dt.float32
BF16 = mybir.dt.bfloat16
AF = mybir.ActivationFunctionType
ALU = mybir.AluOpType
AX = mybir.AxisListType

D = 1024
SEQ = 16384
NB = 2
TOK = 4096
NT = TOK // 512
CTX = 256
NKEY = SEQ + CTX
NKT = NKEY // 128
NE = 32
ALPHA = 4.0 ** 0.25
EPS = 1e-5
LAM_INIT0 = 0.8 - 0.6 * math.exp(-0.3 * 0)


class Buf:
    __slots__ = ("name", "w", "r")

    def __init__(self, name=""):
        self.name = name
        self.w = None
        self.r = []


class T:
    def __init__(self, t, name):
        self.t = t
        self.b = Buf(name)

    def __getitem__(self, idx):
        return self.t[idx]


class KB:
    ENG = ("pe", "act", "dve", "pool", "sp")

    def __init__(self, n_dma_sems=32):
        self.nc = nc = bass.Bass("TRN2", target_bir_lowering=False)
        self.e = dict(pe=nc.tensor, act=nc.scalar, dve=nc.vector, pool=nc.gpsimd, sp=nc.sync)
        self._ctx = []
        self._semctx = []
        self.gen = {}
        self.tot = {}
        self.sem = {}
        self.cnt = {}
        for n in self.ENG:
            self.sem[n] = self._enter(nc.semaphore("s_" + n))
            self.cnt[n] = 0
        self.dsem = [self._enter(nc.semaphore("d%d" % i)) for i in range(n_dma_sems)]
        self.dcnt = [0] * n_dma_sems
        self.dnext = 0
        self.semobj = {}
        for i, s in enumerate(self.dsem):
            self.semobj[("d", i)] = s
        self.waited = {n: {} for n in self.ENG}
        self.n_inst = 0

    SEM_ROT = 4000

    def _enter(self, cm):
        v = cm.__enter__()
        self._ctx.append(cm)
        return v

    def _sem_new(self, name):
        cm = self.nc.semaphore(name)
        v = cm.__enter__()
        self._semctx.append(cm)
        return v

    def mark(self):
        return len(self._ctx)

    def release(self, mark):
        self.barrier()
        while len(self._ctx) > mark:
            self._ctx.pop().__exit__(None, None, None)

    def close(self):
        while self._ctx:
            self._ctx.pop().__exit__(None, None, None)
        while self._semctx:
            self._semctx.pop().__exit__(None, None, None)

    def sb(self, name, shape, dtype):
        self._uid = getattr(self, "_uid", 0) + 1
        name = "%s_%d" % (name, self._uid)
        return T(self._enter(self.nc.sbuf_tensor(name, list(shape), dtype)), name)

    def ps(self, name, shape, dtype=F32):
        return T(self._enter(self.nc.psum_tensor(name, list(shape), dtype)), name)

    def _wait(self, eng, ev):
        if ev is None:
            return
        key, val = ev
        if key[0] == "e" and key[1] == "pe" and eng == "pe":
            return
        w = self.waited[eng]
        if w.get(key, 0) >= val:
            return
        w[key] = val
        self.e[eng].wait_ge(self.semobj[key], val)

    @staticmethod
    def _b(x):
        return x.b if isinstance(x, T) else x

    def _deps(self, eng, reads, writes):
        for b in reads:
            self._wait(eng, self._b(b).w)
        for b in writes:
            b = self._b(b)
            self._wait(eng, b.w)
            for ev in b.r:
                self._wait(eng, ev)

    def _commit(self, ev, reads, writes):
        for b in reads:
            b = self._b(b)
            b.r.append(ev)
            if len(b.r) > 16:
                d = {}
                for k, v in b.r:
                    if d.get(k, 0) < v:
                        d[k] = v
                b.r = list(d.items())
        for b in writes:
            b = self._b(b)
            b.w = ev
            b.r = []

    def op(self, eng, fn, reads=(), writes=(), inc=True):
        self._deps(eng, reads, writes)
        ins = fn(self.e[eng])
        self.n_inst += 1
        key = ("e", eng, self.gen.get(eng, 0))
        if key not in self.semobj:
            self.semobj[key] = self.sem[eng]
        ev = (key, self.cnt[eng] + 1)
        if inc:
            ins.then_inc(self.sem[eng], 1)
            self.cnt[eng] += 1
        self._commit(ev, reads, writes)
        if inc and self.cnt[eng] >= self.SEM_ROT:
            self.gen[eng] = self.gen.get(eng, 0) + 1
            self.sem[eng] = self._sem_new("s_%s_%d" % (eng, self.gen[eng]))
            self.cnt[eng] = 0
            self.tot[eng] = self.tot.get(eng, 0) + self.SEM_ROT
        return ins

    def dma(self, q, out, in_, reads=(), writes=(), **kw):
        j = self.dnext
        self.dnext = (self.dnext + 1) % len(self.dsem)
        self._deps(q, reads, writes)
        if self.dcnt[j]:
            self._wait(q, (("d", j), self.dcnt[j]))
        ins = self.e[q].dma_start(out=out, in_=in_, **kw)
        self.n_inst += 1
        self.dcnt[j] += 16
        ins.then_inc(self.dsem[j], 16)
        ev = (("d", j), self.dcnt[j])
        self._commit(ev, reads, writes)
        return ev

    def barrier(self, engines=None):
        for E in (engines or self.ENG):
            for F in self.ENG:
                if F != E and self.cnt[F] > 0:
                    self._wait(E, (("e", F, self.gen.get(F, 0)), self.cnt[F]))
            for j in range(len(self.dsem)):
                if self.dcnt[j]:
                    self._wait(E, (("d", j), self.dcnt[j]))


class Prog(KB):
    def __init__(self, mode, dbg=None):
        super().__init__()
        self.dbg = dbg
        self.mode = mode
        self.ins = {}
        self.psb = [self.ps("psb%d" % i, [128, 512], F32) for i in range(8)]
        self.rr = {}

    def inp(self, name, shape, dtype=F32):
        if False and name in ("w_gate_up", "w_down", "b_guT", "b_down", "od_w_in",
                                                            "gmlp_ln", "gmlp_wsT", "gmlp_bs", "four_ln", "dft_cs"):
            return None
        t = self.nc.dram_tensor(name, list(shape), dtype, kind="ExternalInput")
        self.ins[name] = (tuple(shape), dtype)
        return t.ap()

    def scratch(self, name, shape, dtype, boundary=None):
        kind = "Internal"
        if boundary is not None and self.mode != "ALL":
            kind = "ExternalOutput" if self.mode == "L1" else "ExternalInput"
            if kind == "ExternalInput":
                self.ins[name] = (tuple(shape), dtype)
        t = self.nc.dram_tensor(name, list(shape), dtype, kind=kind)
        return t.ap(), Buf(name)

    def alt(self, key, n):
        v = self.rr.get(key, 0)
        self.rr[key] = v + 1
        return v % n

    def mm(self, ps, lhsT, rhs, start, stop, reads, last=None, out=None):
        if last is None:
            last = stop
        o = ps[:] if out is None else out
        return self.op("pe", lambda e: e.matmul(o, lhsT, rhs, start=start, stop=stop),
                       reads=reads, writes=[ps], inc=last)

    def proj_fm(self, ps, w, col0, ub, ubuf_reads, ncols=512, nk=8):
        for k in range(nk):
            self.mm(ps, w[:, k, col0:col0 + 128], ub[:, k, 0:ncols], k == 0, k == nk - 1,
                    reads=ubuf_reads, out=ps[:, 0:ncols])

    def load_w_bf16(self, name, dram_ap, ncols, nk=8):
        t = self.sb(name, [128, nk, ncols], BF16)
        src = dram_ap.rearrange("(k p) n -> p k n", p=128)
        for k in range(nk):
            self.dma("pool", t[:, k, :], src[:, k, :], writes=[t])
        return t


def _f(v):
    return float(v)


def build(mode, dbg=None):
    P = Prog(mode, dbg)
    nc = P.nc
    L1 = mode == "L1"
    L3 = mode == "L3"
    L4 = mode == "L4"
    L6 = mode == "L6"

    def ext_out(name, shape, dtype):
        return nc.dram_tensor(name, list(shape), dtype, kind="ExternalOutput").ap(), Buf(name)

    def ext_in(name, shape, dtype=F32):
        return P.inp(name, shape, dtype), Buf(name)

    def internal(name, shape, dtype):
        return nc.dram_tensor(name, list(shape), dtype, kind="Internal").ap(), Buf(name)

    if L1:
        xT_full = P.inp("xT_full", [D, SEQ])
        xT_own = P.inp("xT_own", [D, TOK + 30])
        halo_mask = P.inp("halo_mask", [128, 30])
        ctxT = P.inp("ctxT", [D, CTX])
        cT = P.inp("cT", [128, 16])
        w_mod = P.inp("w_mod", [2, D, 6 * D])
        b_modT = P.inp("b_modT", [128, 2, 48])
        ev_w_in = P.inp("ev_w_in", [D, 2560])
        w_qk_perm = P.inp("w_qk_perm", [D, 1024])
        cs_full = P.inp("cs_full", [128, 2, SEQ])
        cs_own = P.inp("cs_own", [128, 2, TOK])
        conv_wT = P.inp("conv_wT", [128, 4, 31])
        conv_vecs = P.inp("conv_vecs", [128, 3, 4])
        lam_vecs = P.inp("lam_vecs", [1, 4, 64])
        diff_g = P.inp("diff_g", [128, 1])
        ev_w_out = P.inp("ev_w_out", [D, D])
    if L3:
        od_w_in = P.inp("od_w_in", [D, 1536])
        gmlp_ln = P.inp("gmlp_ln", [1, 2, 512])
        gmlp_wsT = P.inp("gmlp_wsT", [128, 4, 128])
        gmlp_bs = P.inp("gmlp_bs", [1, 4, 128])
        four_ln = P.inp("four_ln", [128, 2, 4])
        dft_cs = P.inp("dft_cs", [128, 256], BF16)
    if L4:
        od_w_out = P.inp("od_w_out", [D, D])
        c128 = P.inp("c128", [128, 256], BF16)
        tw_c = P.inp("tw_c", [128, 3, 128])
        cs32 = P.inp("cs32", [128, 64], BF16)
    ln_vecs = P.inp("ln_vecs", [128, 2, 4, 8])
    if L1 or L4:
        router_w = P.inp("router_w", [2, D, NE])
        router_b = P.inp("router_b", [2, 1, NE])
    ident = P.inp("ident", [128, 128])

    if L1:
        modT_d, modT_db = ext_out("modT_d", [128, 2, 48, 2], F32)
        kT_d, kT_db = internal("kT_d", [4, 128, NKEY], BF16)
        v_d, v_db = internal("v_d", [NKEY, 512], BF16)
        qT_d, qT_db = internal("qT_d", [4, 128, TOK], BF16)
        catT_d, catT_db = internal("catT_d", [D, TOK], BF16)
    else:
        modT_d, modT_db = ext_in("modT_d", [128, 2, 48, 2], F32)
    if L1 or L4:
        h1T_d, h1T_db = ext_out("h1T_d", [D, TOK], F32)
        u2T_d, u2T_db = ext_out("u2T_d", [D, TOK], BF16)
        gT_d, gT_db = ext_out("gT_d", [NE, TOK], F32)
    if L3 or L6:
        h1T_d, h1T_db = ext_in("h1T_d", [D, TOK], F32)
        y2p4, y2p4b = ext_in("y2p4", [4, D, TOK], F32)
    if L3:
        h2T_d, h2T_db = ext_out("h2T_d", [D, TOK], F32)
        spT_d, spT_db = ext_out("spT_d", [512, TOK], BF16)
        ab_own, ab_ownb = ext_out("ab_own", [TOK, 1024], BF16)
    if L4:
        h2T_d, h2T_db = ext_in("h2T_d", [D, TOK], F32)
        spT_d, spT_db = ext_in("spT_d", [512, TOK], BF16)
        ab_full, ab_fullb = ext_in("ab_full", [SEQ, 1024], BF16)
        Zd, Zdb = internal("Zd", [128, 128, 1024], BF16)
        fouT_d, fouT_db = internal("fouT_d", [512, TOK], BF16)
    if L6:
        outT, outT_b = ext_out("outT", [D, TOK], F32)

    psb = P.psb

    psb = P.psb

    ones_bf = P.sb("ones_bf", [128, 128], BF16)
    P.op("dve", lambda e: e.memset(ones_bf[:], 1.0), writes=[ones_bf])
    modT = P.sb("modT", [128, 2, 48, 2], F32)
    mod1p = P.sb("mod1p", [128, 2, 48, 2], F32)
    lnv = P.sb("lnv", [128, 2, 4, 8], F32)
    P.dma("sp", lnv[:], ln_vecs, writes=[lnv])
    ident_sb = P.sb("ident_sb", [128, 128], F32)
    P.dma("sp", ident_sb[:], ident, writes=[ident_sb])

    def MOD(l, idx, ch, m=0, plus1=False):
        t = mod1p if plus1 else modT
        return t[:, l, idx * 8 + ch, m:m + 1]


    if L1:
        mk = P.mark()
        cT_sb = P.sb("cT_sb", [128, 16], F32)
        sT_sb = P.sb("sT_sb", [128, 16], F32)
        bm_sb = P.sb("bm_sb", [128, 2, 48], F32)
        P.dma("sp", cT_sb[:], cT, writes=[cT_sb])
        P.dma("sp", bm_sb[:], b_modT, writes=[bm_sb])
        P.op("act", lambda e: e.activation(out=sT_sb[:], in_=cT_sb[:], func=AF.Silu),
             reads=[cT_sb], writes=[sT_sb])
        wm = [P.sb("wm%d" % i, [128, 8, 768], F32) for i in range(2)]
        for l in range(2):
            for blk in range(8):
                wt = wm[P.alt("wm", 2)]
                src = w_mod[l, :, blk * 768:(blk + 1) * 768].rearrange("(k p) n -> p k n", p=128)
                for kk in range(8):
                    P.dma("sp", wt[:, kk, :], src[:, kk, :], writes=[wt])
                for mc in range(6):
                    ps = psb[P.alt("ps", 8)]
                    for kk in range(8):
                        P.mm(ps, wt[:, kk, mc * 128:(mc + 1) * 128], sT_sb[:, kk * 2:kk * 2 + 2],
                             kk == 0, kk == 7, reads=[wt, sT_sb], out=ps[:, 0:2])
                    ch = blk * 6 + mc
                    P.op("dve", lambda e: e.tensor_tensor(
                        modT[:, l, ch, :], ps[:, 0:2], bm_sb[:, l, ch:ch + 1].to_broadcast([128, 2]), ALU.add),
                        reads=[ps, bm_sb], writes=[modT])
        P.op("dve", lambda e: e.tensor_scalar_add(mod1p[:], modT[:], 1.0), reads=[modT], writes=[mod1p])
        P.dma("sp", modT_d, modT[:], reads=[modT], writes=[modT_db])
        P.release(mk)
        if dbg == "M":
            P.barrier(engines=["sp"])
            P.close()
            return P
    else:
        P.dma("sp", modT[:], modT_d, reads=[modT_db], writes=[modT])
        P.op("dve", lambda e: e.tensor_scalar_add(mod1p[:], modT[:], 1.0), reads=[modT], writes=[mod1p])

    def modulate_tile(xt, ub, l, idx_sh, idx_sc, m, ncols):
        for kk in range(8):
            if kk % 2 == 0:
                P.op("act", lambda e: e.activation(out=ub[:, kk, 0:ncols], in_=xt[:, kk, 0:ncols],
                                                   func=AF.Identity,
                                                   bias=MOD(l, idx_sh, kk, m),
                                                   scale=MOD(l, idx_sc, kk, m, True)),
                     reads=[xt, mod1p, modT], writes=[ub])
            else:
                P.op("dve", lambda e: e.tensor_scalar(ub[:, kk, 0:ncols], xt[:, kk, 0:ncols],
                                                      MOD(l, idx_sc, kk, m, True), MOD(l, idx_sh, kk, m),
                                                      ALU.mult, ALU.add),
                     reads=[xt, mod1p, modT], writes=[ub])

    def ln_stats(src_chunks, nch, ncols, inv_n, tag):
        pm = psb[P.alt("ps", 8)]
        pq = psb[P.alt("ps", 8)]
        for i, (ap, tl) in enumerate(src_chunks):
            xb = st[tag + "_xb"][P.alt(tag + "xb", 2)]
            x2b = st[tag + "_x2b"][P.alt(tag + "x2b", 2)]
            P.op("act", lambda e: e.activation(out=xb[:, 0:ncols], in_=ap, func=AF.Copy),
                 reads=[tl], writes=[xb])
            P.op("act", lambda e: e.activation(out=x2b[:, 0:ncols], in_=ap, func=AF.Square),
                 reads=[tl], writes=[x2b])
            P.mm(pm, ones_bf[:], xb[:, 0:ncols], i == 0, i == nch - 1, reads=[ones_bf, xb], out=pm[:, 0:ncols], last=True)
            P.mm(pq, ones_bf[:], x2b[:, 0:ncols], i == 0, i == nch - 1, reads=[ones_bf, x2b], out=pq[:, 0:ncols], last=True)
        mean = st[tag + "_mean"]
        rstd = st[tag + "_rstd"]
        P.op("act", lambda e: e.activation(out=mean[:, 0:ncols], in_=pm[:, 0:ncols], func=AF.Identity, scale=inv_n),
             reads=[pm], writes=[mean])
        P.op("dve", lambda e: e.tensor_tensor(rstd[:, 0:ncols], mean[:, 0:ncols], mean[:, 0:ncols], ALU.mult),
             reads=[mean], writes=[rstd])
        P.op("dve", lambda e: e.scalar_tensor_tensor(rstd[:, 0:ncols], pq[:, 0:ncols], inv_n, rstd[:, 0:ncols],
                                                     ALU.mult, ALU.subtract),
             reads=[pq, rstd], writes=[rstd])
        P.op("dve", lambda e: e.tensor_scalar_add(rstd[:, 0:ncols], rstd[:, 0:ncols], EPS), reads=[], writes=[rstd])
        P.op("act", lambda e: e.sqrt(rstd[:, 0:ncols], rstd[:, 0:ncols]), reads=[], writes=[rstd])
        P.op("dve", lambda e: e.reciprocal(rstd[:, 0:ncols], rstd[:, 0:ncols]), reads=[], writes=[rstd])
        return mean, rstd

    st = {}

    def alloc_ln(tag, nch):
        st[tag + "_xb"] = [P.sb(tag + "_xb", [128, 512], BF16) for _ in range(2)]
        st[tag + "_x2b"] = [P.sb(tag + "_x2b", [128, 512], BF16) for _ in range(2)]
        st[tag + "_mean"] = P.sb(tag + "_mean", [128, 512], F32)
        st[tag + "_rstd"] = P.sb(tag + "_rstd", [128, 512], F32)


    if L1:
        mk = P.mark()
        wk = P.load_w_bf16("wk", ev_w_in[:, 512:1024], 512)
        wkp = P.load_w_bf16("wkp", w_qk_perm[:, 512:1024], 512)
        wv = P.load_w_bf16("wv", ev_w_in[:, 1024:1536], 512)
        xts = [P.sb("xtA%d" % i, [128, 8, 512], F32) for i in range(2)]
        ubs = [P.sb("ubA%d" % i, [128, 8, 512], BF16) for i in range(2)]
        css = [P.sb("csA%d" % i, [128, 2, 512], F32) for i in range(2)]
        t1s = [P.sb("t1A%d" % i, [128, 512], F32) for i in range(2)]
        t2s = [P.sb("t2A%d" % i, [128, 512], F32) for i in range(2)]
        krs = [P.sb("krA%d" % i, [128, 512], BF16) for i in range(3)]
        vbs = [P.sb("vbA%d" % i, [128, 512], BF16) for i in range(3)]
        for it in range(SEQ // 512 + 1):
            is_ctx = it == SEQ // 512
            ncols = CTX if is_ctx else 512
            c0 = it * 512
            xt = xts[it % 2]
            ub = ubs[it % 2]
            cs = css[it % 2]
            srcx = (ctxT if is_ctx else xT_full[:, c0:c0 + 512]).rearrange("(k p) t -> p k t", p=128)
            for kk in range(8):
                P.dma("sp", xt[:, kk, 0:ncols], srcx[:, kk, :], writes=[xt])
            if not is_ctx:
                P.dma("sp", cs[:], cs_full[:, :, c0:c0 + 512], writes=[cs])
            modulate_tile(xt, ub, 0, 0, 1, 1 if is_ctx else 0, ncols)
            for hc in range(4):
                pa = psb[P.alt("ps", 8)]
                P.proj_fm(pa, wk, hc * 128, ub, [wk, ub], ncols)
                kr = krs[P.alt("krA", 3)]
                if is_ctx:
                    P.op("act", lambda e: e.activation(out=kr[:, 0:ncols], in_=pa[:, 0:ncols], func=AF.Copy),
                         reads=[pa], writes=[kr])
                else:
                    pb = psb[P.alt("ps", 8)]
                    P.proj_fm(pb, wkp, hc * 128, ub, [wkp, ub], ncols)
                    t1 = t1s[hc % 2]
                    t2 = t2s[hc % 2]
                    P.op("dve", lambda e: e.tensor_tensor(t1[:], pa[:], cs[:, 0, :], ALU.mult),
                         reads=[pa, cs], writes=[t1])
                    P.op("dve", lambda e: e.tensor_tensor(t2[:], pb[:], cs[:, 1, :], ALU.mult),
                         reads=[pb, cs], writes=[t2])
                    P.op("pool", lambda e: e.tensor_tensor(kr[:], t1[:], t2[:], ALU.add),
                         reads=[t1, t2], writes=[kr])
                P.dma("sp", kT_d[hc, :, c0:c0 + ncols], kr[:, 0:ncols], reads=[kr], writes=[kT_db])
            for s in range(ncols // 128):
                pv = psb[P.alt("ps", 8)]
                for kk in range(8):
                    P.mm(pv, ub[:, kk, s * 128:(s + 1) * 128], wv[:, kk, :], kk == 0, kk == 7, reads=[ub, wv])
                vb = vbs[P.alt("vbA", 3)]
                P.op("act", lambda e: e.activation(out=vb[:], in_=pv[:], func=AF.Copy), reads=[pv], writes=[vb])
                P.dma("sp", v_d[c0 + s * 128:c0 + (s + 1) * 128, :], vb[:], reads=[vb], writes=[v_db])
        P.release(mk)

        if dbg == "A":
            P.barrier(engines=["sp"])
            P.close()
            return P
        mk = P.mark()
        wq = P.load_w_bf16("wq", ev_w_in[:, 0:512], 512)
        wqp = P.load_w_bf16("wqp", w_qk_perm[:, 0:512], 512)
        wval = P.load_w_bf16("wval", ev_w_in[:, 1536:2048], 512)
        wgat = P.load_w_bf16("wgat", ev_w_in[:, 2048:2560], 512)
        aT = P.sb("aT", [128, 4, TOK + 30], F32)
        hm = P.sb("hm", [128, 30], F32)
        P.dma("sp", hm[:], halo_mask, writes=[hm])
        cw = P.sb("cw", [128, 4, 31], F32)
        P.dma("sp", cw[:], conv_wT, writes=[cw])
        cv = P.sb("cv", [128, 3, 4], F32)
        P.dma("sp", cv[:], conv_vecs, writes=[cv])
        xts = [P.sb("xtB%d" % i, [128, 8, 512], F32) for i in range(2)]
        ubs = [P.sb("ubB%d" % i, [128, 8, 512], BF16) for i in range(2)]
        css = [P.sb("csB%d" % i, [128, 2, 512], F32) for i in range(2)]
        t1s = [P.sb("t1B%d" % i, [128, 512], F32) for i in range(2)]
        t2s = [P.sb("t2B%d" % i, [128, 512], F32) for i in range(2)]
        qrs = [P.sb("qrB%d" % i, [128, 512], BF16) for i in range(3)]
        sgs = [P.sb("sgB%d" % i, [128, 512], F32) for i in range(2)]
        for it in range(NT + 1):
            is_halo = it == NT
            ncols = 30 if is_halo else 512
            c0 = it * 512
            xt = xts[it % 2]
            ub = ubs[it % 2]
            cs = css[it % 2]
            srcx = xT_own[:, c0:c0 + ncols].rearrange("(k p) t -> p k t", p=128)
            for kk in range(8):
                P.dma("sp", xt[:, kk, 0:ncols], srcx[:, kk, :], writes=[xt])
            modulate_tile(xt, ub, 0, 0, 1, 0, ncols)
            if not is_halo:
                P.dma("sp", cs[:], cs_own[:, :, c0:c0 + 512], writes=[cs])
                for hc in range(4):
                    pa = psb[P.alt("ps", 8)]
                    P.proj_fm(pa, wq, hc * 128, ub, [wq, ub])
                    pb = psb[P.alt("ps", 8)]
                    P.proj_fm(pb, wqp, hc * 128, ub, [wqp, ub])
                    t1 = t1s[hc % 2]
                    t2 = t2s[hc % 2]
                    qr = qrs[P.alt("qrB", 3)]
                    P.op("dve", lambda e: e.tensor_tensor(t1[:], pa[:], cs[:, 0, :], ALU.mult),
                         reads=[pa, cs], writes=[t1])
                    P.op("dve", lambda e: e.tensor_tensor(t2[:], pb[:], cs[:, 1, :], ALU.mult),
                         reads=[pb, cs], writes=[t2])
                    P.op("pool", lambda e: e.tensor_tensor(qr[:], t1[:], t2[:], ALU.add),
                         reads=[t1, t2], writes=[qr])
                    P.dma("sp", qT_d[hc, :, c0:c0 + 512], qr[:], reads=[qr], writes=[qT_db])
            for ch in range(4):
                pvv = psb[P.alt("ps", 8)]
                P.proj_fm(pvv, wval, ch * 128, ub, [wval, ub], ncols)
                pg = psb[P.alt("ps", 8)]
                P.proj_fm(pg, wgat, ch * 128, ub, [wgat, ub], ncols)
                sg = sgs[ch % 2]
                P.op("act", lambda e: e.activation(out=sg[:, 0:ncols], in_=pg[:, 0:ncols], func=AF.Sigmoid),
                     reads=[pg], writes=[sg])
                if not is_halo:
                    P.op("dve", lambda e: e.tensor_tensor(aT[:, ch, 15 + c0:15 + c0 + 512], pvv[:], sg[:], ALU.mult),
                         reads=[pvv, sg], writes=[aT])
                else:
                    P.op("dve", lambda e: e.tensor_tensor(sg[:, 0:30], pvv[:, 0:30], sg[:, 0:30], ALU.mult),
                         reads=[pvv, sg], writes=[sg])
                    P.op("dve", lambda e: e.tensor_tensor(aT[:, ch, 0:15], sg[:, 0:15], hm[:, 0:15], ALU.mult),
                         reads=[sg, hm], writes=[aT])
                    P.op("dve", lambda e: e.tensor_tensor(aT[:, ch, 15 + TOK:30 + TOK], sg[:, 15:30], hm[:, 15:30],
                                                          ALU.mult),
                         reads=[sg, hm], writes=[aT])
        if dbg == "B1":
            P.barrier(engines=["sp"])
            P.close()
            return P
        alloc_ln("cln", 4)
        accs = [P.sb("cacc%d" % i, [128, 512], F32) for i in range(4)]
        cvo = [P.sb("cvo%d" % i, [128, 4, 512], BF16) for i in range(2)]
        xcs = [P.sb("cxc%d" % i, [128, 512], F32) for i in range(2)]
        for it in range(NT):
            c0 = it * 512
            for ch in range(4):
                eng = "dve"
                acc = accs[ch]
                P.op(eng, lambda e: e.tensor_scalar(acc[:], aT[:, ch, c0:c0 + 512], cw[:, ch, 0:1], cv[:, 0, ch:ch + 1],
                                                    ALU.mult, ALU.add),
                     reads=[aT, cw, cv], writes=[acc])
                for j in range(1, 31):
                    P.op(eng, lambda e: e.scalar_tensor_tensor(acc[:], aT[:, ch, c0 + j:c0 + j + 512], cw[:, ch, j:j + 1],
                                                               acc[:], ALU.mult, ALU.add),
                         reads=[aT, cw], writes=[acc])
            mean, rstd = ln_stats([(accs[ch][:], accs[ch]) for ch in range(4)], 4, 512, 1.0 / 512, "cln")
            co = cvo[it % 2]
            for ch in range(4):
                xc = xcs[ch % 2]
                P.op("dve", lambda e: e.tensor_tensor(xc[:], accs[ch][:], mean[:], ALU.subtract),
                     reads=[accs[ch], mean], writes=[xc])
                P.op("pool", lambda e: e.tensor_tensor(xc[:], xc[:], rstd[:], ALU.mult),
                     reads=[rstd], writes=[xc])
                P.op("dve", lambda e: e.tensor_scalar(xc[:], xc[:], cv[:, 1, ch:ch + 1], cv[:, 2, ch:ch + 1],
                                                      ALU.mult, ALU.add),
                     reads=[cv], writes=[xc])
                P.op("act", lambda e: e.activation(out=co[:, ch, :], in_=xc[:], func=AF.Silu),
                     reads=[xc], writes=[co])
            dst = catT_d[0:512, c0:c0 + 512].rearrange("(k p) t -> p k t", p=128)
            P.dma("sp", dst, co[:], reads=[co], writes=[catT_db])
        P.release(mk)

        if dbg == "B":
            P.barrier(engines=["sp"])
            P.close()
            return P
        mk = P.mark()
        lv = P.sb("lv", [1, 4, 64], F32)
        P.dma("sp", lv[:], lam_vecs, writes=[lv])
        lp = P.sb("lp", [1, 2, 64], F32)
        ls = P.sb("ls", [1, 4], F32)
        P.op("dve", lambda e: e.tensor_tensor(lp[:, 0, :], lv[:, 0, :], lv[:, 1, :], ALU.mult), reads=[lv], writes=[lp])
        P.op("dve", lambda e: e.tensor_tensor(lp[:, 1, :], lv[:, 2, :], lv[:, 3, :], ALU.mult), reads=[lv], writes=[lp])
        P.op("dve", lambda e: e.tensor_reduce(ls[:, 0:2], lp[:], AX.X, ALU.add), reads=[lp], writes=[ls])
        P.op("act", lambda e: e.activation(out=ls[:, 2:4], in_=ls[:, 0:2], func=AF.Exp), reads=[ls], writes=[ls])
        P.op("dve", lambda e: e.tensor_tensor(ls[:, 0:1], ls[:, 3:4], ls[:, 2:3], ALU.subtract), reads=[ls], writes=[ls])
        P.op("dve", lambda e: e.tensor_scalar_add(ls[:, 0:1], ls[:, 0:1], -LAM_INIT0), reads=[ls], writes=[ls])
        ones_f = P.sb("ones_f", [1, 128], F32)
        P.op("dve", lambda e: e.memset(ones_f[:], 1.0), writes=[ones_f])
        neglam = P.sb("neglam", [128, 1], F32)
        pl = psb[P.alt("ps", 8)]
        P.mm(pl, ones_f[:], ls[:, 0:1], True, True, reads=[ones_f, ls], out=pl[:, 0:1])
        P.op("dve", lambda e: e.tensor_copy(neglam[:], pl[:, 0:1]), reads=[pl], writes=[neglam])
        dg = P.sb("dg", [128, 1], F32)
        P.dma("sp", dg[:], diff_g, writes=[dg])
        P.op("dve", lambda e: e.tensor_scalar_mul(dg[:], dg[:], 1.0 - LAM_INIT0), reads=[dg], writes=[dg])

        kTh = P.sb("kTh", [128, NKEY], BF16)
        vh = P.sb("vh", [128, NKT, 128], BF16)
        qh = P.sb("qh", [128, TOK], BF16)
        pts = [P.sb("pt%d" % i, [128, 512], BF16) for i in range(4)]
        ocs = [P.sb("oc%d" % i, [128, 512], F32) for i in range(2)]
        rl = P.sb("rl", [128, 512], F32)
        osq = P.sb("osq", [128, 512], BF16)
        att_o = [P.sb("atto%d" % i, [128, 512], BF16) for i in range(2)]
        ps_s = psb[0:3]
        ps_o = psb[3:5]
        ps_l = psb[5:7]
        ps_r = psb[7]
        for h in range(4):
            for piece in range(5):
                a = piece * 3328
                P.dma("sp", kTh[:, a:a + 3328], kT_d[h, :, a:a + 3328], reads=[kT_db], writes=[kTh])
            vsrc = v_d[:, h * 128:(h + 1) * 128].rearrange("(kt p) d -> p kt d", p=128)
            for piece in range(10):
                P.dma("sp", vh[:, piece * 13:(piece + 1) * 13, :], vsrc[:, piece * 13:(piece + 1) * 13, :],
                      reads=[v_db], writes=[vh])
            P.dma("sp", qh[:], qT_d[h], reads=[qT_db], writes=[qh])
            for qt in range(NT):
                q0 = qt * 512
                for c in range(2):
                    po = ps_o[c]
                    pL = ps_l[c]
                    r0 = c * 64
                    for kt in range(NKT):
                        pss = ps_s[P.alt("pss", 3)]
                        P.mm(pss, kTh[r0:r0 + 64, kt * 128:(kt + 1) * 128], qh[r0:r0 + 64, q0:q0 + 512], True, True,
                             reads=[kTh, qh])
                        pt = pts[P.alt("pt", 4)]
                        P.op("act", lambda e: e.activation(out=pt[:], in_=pss[:], func=AF.Exp, scale=0.125),
                             reads=[pss], writes=[pt])
                        P.mm(po, vh[:, kt, :], pt[:], kt == 0, kt == NKT - 1, reads=[vh, pt], last=False)
                        P.mm(pL, ones_bf[:], pt[:], kt == 0, kt == NKT - 1, reads=[ones_bf, pt], last=True)
                    P.op("dve", lambda e: e.reciprocal(rl[:], pL[:]), reads=[pL], writes=[rl])
                    P.op("dve", lambda e: e.tensor_tensor(ocs[c][:], po[:], rl[:], ALU.mult),
                         reads=[po, rl], writes=[ocs[c]])
                o = ocs[0]
                P.op("dve", lambda e: e.scalar_tensor_tensor(o[:], ocs[1][:], neglam[:, 0:1], o[:], ALU.mult, ALU.add),
                     reads=[ocs[1], neglam], writes=[o])
                P.op("act", lambda e: e.activation(out=osq[:], in_=o[:], func=AF.Square), reads=[o], writes=[osq])
                P.mm(ps_r, ones_bf[:], osq[:], True, True, reads=[ones_bf, osq])
                P.op("dve", lambda e: e.tensor_scalar(rl[:], ps_r[:], 1.0 / 128, EPS, ALU.mult, ALU.add),
                     reads=[ps_r], writes=[rl])
                P.op("act", lambda e: e.sqrt(rl[:], rl[:]), reads=[], writes=[rl])
                P.op("dve", lambda e: e.reciprocal(rl[:], rl[:]), reads=[], writes=[rl])
                P.op("dve", lambda e: e.tensor_tensor(o[:], o[:], rl[:], ALU.mult), reads=[rl], writes=[o])
                ao = att_o[P.alt("atto", 2)]
                P.op("act", lambda e: e.activation(out=ao[:], in_=o[:], func=AF.Identity, scale=dg[:, 0:1]),
                     reads=[o, dg], writes=[ao])
                P.dma("sp", catT_d[512 + h * 128:512 + (h + 1) * 128, q0:q0 + 512], ao[:], reads=[ao], writes=[catT_db])
        P.release(mk)

    def post_mixer(l, w_out_ap, catA, catAb, catB, catBb, res_src, res_srcb):
        mk = P.mark()
        wo = P.load_w_bf16("wo", w_out_ap, D)
        rw = P.sb("rw", [128, 8, NE], F32)
        P.dma("sp", rw[:], router_w[l].rearrange("(k p) e -> p k e", p=128), writes=[rw])
        rb = P.sb("rb", [128, NE], F32)
        P.dma("sp", rb[:], router_b[l].partition_broadcast(128), writes=[rb])
        alloc_ln("ln1", 8)
        cats = [P.sb("cat%d" % i, [128, 8, 512], BF16) for i in range(2)]
        xts = [P.sb("xtD%d" % i, [128, 8, 512], F32) for i in range(2)]
        rts = [P.sb("rtD%d" % i, [128, 8, 512], F32) for i in range(2)]
        u2f = P.sb("u2f", [128, 8, 512], F32)
        u2b = [P.sb("u2b%d" % i, [128, 8, 512], BF16) for i in range(2)]
        lg = P.sb("lg", [128, NE], F32)
        mx = P.sb("mx", [128, 8], F32)
        msk = P.sb("msk", [128, NE], F32)
        ex = P.sb("ex", [128, NE], F32)
        ssum = P.sb("ssum", [128, 1], F32)
        gts = [P.sb("gts%d" % i, [128, NE], F32) for i in range(2)]
        gTt = P.sb("gTt", [NE, 512], F32)
        for it in range(NT):
            c0 = it * 512
            cat = cats[it % 2]
            xt = xts[it % 2]
            rt = rts[it % 2]
            srcA = catA[:, c0:c0 + 512].rearrange("(k p) t -> p k t", p=128)
            srcB = catB[:, c0:c0 + 512].rearrange("(k p) t -> p k t", p=128)
            srcx = res_src[:, c0:c0 + 512].rearrange("(k p) t -> p k t", p=128)
            for kk in range(8):
                if kk < 4:
                    P.dma("sp", cat[:, kk, :], srcA[:, kk, :], reads=[catAb], writes=[cat])
                else:
                    P.dma("sp", cat[:, kk, :], srcB[:, kk - 4, :], reads=[catBb], writes=[cat])
                P.dma("sp", xt[:, kk, :], srcx[:, kk, :], reads=[res_srcb], writes=[xt])
            for m in range(8):
                py = psb[P.alt("ps", 8)]
                P.proj_fm(py, wo, m * 128, cat, [wo, cat])
                P.op("act", lambda e: e.activation(out=xt[:, m, :], in_=xt[:, m, :], func=AF.Identity, scale=ALPHA),
                     reads=[], writes=[xt])
                P.op("dve", lambda e: e.scalar_tensor_tensor(rt[:, m, :], py[:], MOD(l, 2, m), xt[:, m, :],
                                                             ALU.mult, ALU.add),
                     reads=[py, modT, xt], writes=[rt])
            mean, rstd = ln_stats([(rt[:, m, :], rt) for m in range(8)], 8, 512, 1.0 / D, "ln1")
            ub = u2b[it % 2]
            for m in range(8):
                P.op("dve", lambda e: e.tensor_tensor(rt[:, m, :], rt[:, m, :], mean[:], ALU.subtract),
                     reads=[mean], writes=[rt])
                P.op("pool", lambda e: e.tensor_tensor(rt[:, m, :], rt[:, m, :], rstd[:], ALU.mult),
                     reads=[rstd], writes=[rt])
                P.op("dve", lambda e: e.tensor_scalar(rt[:, m, :], rt[:, m, :], lnv[:, l, 0, m:m + 1],
                                                      lnv[:, l, 1, m:m + 1], ALU.mult, ALU.add),
                     reads=[lnv], writes=[rt])
                P.op("act", lambda e: e.activation(out=u2f[:, m, :], in_=rt[:, m, :], func=AF.Identity,
                                                   bias=MOD(l, 3, m), scale=MOD(l, 4, m, 0, True)),
                     reads=[rt, modT, mod1p], writes=[u2f])
                P.op("pool", lambda e: e.tensor_copy(ub[:, m, :], u2f[:, m, :]), reads=[u2f], writes=[ub])
            dsth = h1T_d[:, c0:c0 + 512].rearrange("(k p) t -> p k t", p=128)
            dstu = u2T_d[:, c0:c0 + 512].rearrange("(k p) t -> p k t", p=128)
            for kk in range(8):
                P.dma("sp", dsth[:, kk, :], rt[:, kk, :], reads=[rt], writes=[h1T_db])
                P.dma("sp", dstu[:, kk, :], ub[:, kk, :], reads=[ub], writes=[u2T_db])
            for s in range(4):
                pr = psb[P.alt("ps", 8)]
                for kk in range(8):
                    P.mm(pr, u2f[:, kk, s * 128:(s + 1) * 128], rw[:, kk, :], kk == 0, kk == 7,
                         reads=[u2f, rw], out=pr[:, 0:NE])
                P.op("dve", lambda e: e.tensor_tensor(lg[:], pr[:, 0:NE], rb[:], ALU.add), reads=[pr, rb], writes=[lg])
                P.op("dve", lambda e: e.max(out=mx[:], in_=lg[:]), reads=[lg], writes=[mx])
                P.op("dve", lambda e: e.tensor_scalar(msk[:], lg[:], mx[:, 3:4], None, ALU.is_ge),
                     reads=[lg, mx], writes=[msk])
                P.op("dve", lambda e: e.tensor_scalar(ex[:], lg[:], mx[:, 0:1], None, ALU.subtract),
                     reads=[lg, mx], writes=[ex])
                P.op("act", lambda e: e.activation(out=ex[:], in_=ex[:], func=AF.Exp), reads=[], writes=[ex])
                P.op("dve", lambda e: e.tensor_tensor(ex[:], ex[:], msk[:], ALU.mult), reads=[msk], writes=[ex])
                P.op("dve", lambda e: e.tensor_reduce(ssum[:], ex[:], AX.X, ALU.add), reads=[ex], writes=[ssum])
                P.op("dve", lambda e: e.reciprocal(ssum[:], ssum[:]), reads=[], writes=[ssum])
                gt = gts[s % 2]
                P.op("dve", lambda e: e.tensor_scalar(gt[:], ex[:], ssum[:, 0:1], None, ALU.mult),
                     reads=[ex, ssum], writes=[gt])
                pg = psb[P.alt("ps", 8)]
                P.mm(pg, gt[:], ident_sb[:], True, True, reads=[gt, ident_sb], out=pg[0:NE, 0:128])
                P.op("act", lambda e: e.activation(out=gTt[:, s * 128:(s + 1) * 128], in_=pg[0:NE, 0:128], func=AF.Copy),
                     reads=[pg], writes=[gTt])
            P.dma("sp", gT_d[:, c0:c0 + 512], gTt[:], reads=[gTt], writes=[gT_db])
        P.release(mk)

    def sum_ln2(l, out_dst, out_dstb):
        mk = P.mark()
        alloc_ln("ln2", 8)
        hts = [P.sb("htS%d" % i, [128, 8, 512], F32) for i in range(2)]
        pbs = [P.sb("pbS%d" % i, [128, 8, 512], F32) for i in range(2)]
        acc = P.sb("accS", [128, 8, 512], F32)
        for it in range(NT):
            c0 = it * 512
            ht = hts[it % 2]
            srch = h1T_d[:, c0:c0 + 512].rearrange("(k p) t -> p k t", p=128)
            for kk in range(8):
                P.dma("sp", ht[:, kk, :], srch[:, kk, :], reads=[h1T_db], writes=[ht])
            for pr in range(2):
                for i in range(2):
                    srcp = y2p4[pr * 2 + i, :, c0:c0 + 512].rearrange("(k p) t -> p k t", p=128)
                    for kk in range(8):
                        P.dma("sp", pbs[i][:, kk, :], srcp[:, kk, :], reads=[y2p4b], writes=[pbs[i]])
                if pr == 0:
                    P.op("dve", lambda e: e.tensor_tensor(acc[:], pbs[0][:], pbs[1][:], ALU.add),
                         reads=[pbs[0], pbs[1]], writes=[acc])
                else:
                    P.op("pool", lambda e: e.tensor_tensor(acc[:], acc[:], pbs[0][:], ALU.add),
                         reads=[pbs[0]], writes=[acc])
                    P.op("dve", lambda e: e.tensor_tensor(acc[:], acc[:], pbs[1][:], ALU.add),
                         reads=[pbs[1]], writes=[acc])
            P.op("act", lambda e: e.activation(out=ht[:], in_=ht[:], func=AF.Identity, scale=ALPHA),
                 reads=[], writes=[ht])
            for m in range(8):
                P.op("dve", lambda e: e.scalar_tensor_tensor(ht[:, m, :], acc[:, m, :], MOD(l, 5, m), ht[:, m, :],
                                                             ALU.mult, ALU.add),
                     reads=[acc, modT], writes=[ht])
            mean, rstd = ln_stats([(ht[:, m, :], ht) for m in range(8)], 8, 512, 1.0 / D, "ln2")
            for m in range(8):
                P.op("dve", lambda e: e.tensor_tensor(ht[:, m, :], ht[:, m, :], mean[:], ALU.subtract),
                     reads=[mean], writes=[ht])
                P.op("pool", lambda e: e.tensor_tensor(ht[:, m, :], ht[:, m, :], rstd[:], ALU.mult),
                     reads=[rstd], writes=[ht])
                P.op("dve", lambda e: e.tensor_scalar(ht[:, m, :], ht[:, m, :], lnv[:, l, 2, m:m + 1],
                                                      lnv[:, l, 3, m:m + 1], ALU.mult, ALU.add),
                     reads=[lnv], writes=[ht])
            dsto = out_dst[:, c0:c0 + 512].rearrange("(k p) t -> p k t", p=128)
            for kk in range(8):
                P.dma("sp", dsto[:, kk, :], ht[:, kk, :], reads=[ht], writes=[out_dstb])
        P.release(mk)

    if L1:
        xres = xT_own[:, 0:TOK]
        post_mixer(0, ev_w_out, catT_d[0:512, :], catT_db, catT_d[512:1024, :], catT_db, xres, Buf("xres"))
    if L3:
        sum_ln2(0, h2T_d, h2T_db)
    if L6:
        sum_ln2(1, outT, outT_b)

    if L3:
        mk = P.mark()
        wu = P.load_w_bf16("wu", od_w_in[:, 0:512], 512)
        wv1 = P.load_w_bf16("wv1", od_w_in[:, 512:1024], 512)
        wf = P.load_w_bf16("wf", od_w_in[:, 1024:1536], 512)
        wsT = P.sb("wsT", [128, 4, 128], BF16)
        P.dma("pool", wsT[:], gmlp_wsT, writes=[wsT])
        bsb = P.sb("bsb", [128, 4, 128], F32)
        P.dma("sp", bsb[:], gmlp_bs.partition_broadcast(128), writes=[bsb])
        gln = P.sb("gln", [128, 2, 512], F32)
        P.dma("sp", gln[:], gmlp_ln.partition_broadcast(128), writes=[gln])
        fl = P.sb("fl", [128, 2, 4], F32)
        P.dma("sp", fl[:], four_ln, writes=[fl])
        dcs = P.sb("dcs", [128, 256], BF16)
        P.dma("sp", dcs[:], dft_cs, writes=[dcs])
        alloc_ln("fln", 1)
        xts = [P.sb("xtF%d" % i, [128, 8, 512], F32) for i in range(2)]
        ubs = [P.sb("ubF%d" % i, [128, 8, 512], BF16) for i in range(2)]
        ugs = [P.sb("ugF%d" % i, [128, 4, 512], F32) for i in range(2)]
        vg = P.sb("vgF", [128, 512], F32)
        bst = P.sb("bst", [128, 2, 6], F32)
        bag = P.sb("bag", [128, 2], F32)
        vrs = P.sb("vrs", [128, 1], F32)
        vln = [P.sb("vln%d" % i, [128, 512], BF16) for i in range(2)]
        svt = P.sb("svt", [128, 128], F32)
        spo = [P.sb("spo%d" % i, [128, 4, 512], BF16) for i in range(2)]
        fch = [P.sb("fch%d" % i, [128, 512], F32) for i in range(2)]
        flb = [P.sb("flb%d" % i, [128, 4, 512], BF16) for i in range(2)]
        abo = [P.sb("abo%d" % i, [128, 1024], BF16) for i in range(2)]

        def gelu_tanh(dst, dst_t, src, reads, tmp):
            c = 2.0 * math.sqrt(2.0 / math.pi)
            P.op("act", lambda e: e.activation(out=tmp[:], in_=src, func=AF.Square), reads=reads, writes=[tmp])
            P.op("dve", lambda e: e.tensor_scalar(tmp[:], tmp[:], 0.044715 * c, c, ALU.mult, ALU.add), reads=[], writes=[tmp])
            P.op("dve", lambda e: e.tensor_tensor(tmp[:], tmp[:], src, ALU.mult), reads=reads, writes=[tmp])
            P.op("act", lambda e: e.activation(out=tmp[:], in_=tmp[:], func=AF.Sigmoid), reads=[], writes=[tmp])
            P.op("dve", lambda e: e.tensor_tensor(dst, tmp[:], src, ALU.mult), reads=reads + [tmp], writes=[dst_t])

        gtmp = [P.sb("gtmp%d" % i, [128, 512], F32) for i in range(2)]
        for it in range(NT):
            c0 = it * 512
            xt = xts[it % 2]
            ub = ubs[it % 2]
            srcx = h2T_d[:, c0:c0 + 512].rearrange("(k p) t -> p k t", p=128)
            for kk in range(8):
                P.dma("sp", xt[:, kk, :], srcx[:, kk, :], reads=[h2T_db], writes=[xt])
            modulate_tile(xt, ub, 1, 0, 1, 0, 512)
            ug = ugs[it % 2]
            for ch in range(4):
                pu = psb[P.alt("ps", 8)]
                P.proj_fm(pu, wu, ch * 128, ub, [wu, ub])
                tmp = gtmp[ch % 2]
                gelu_tanh(ug[:, ch, :], ug, pu[:], [pu], tmp)
            sp = spo[it % 2]
            for s in range(4):
                pv = psb[P.alt("ps", 8)]
                for kk in range(8):
                    P.mm(pv, ub[:, kk, s * 128:(s + 1) * 128], wv1[:, kk, :], kk == 0, kk == 7, reads=[ub, wv1])
                tmp = gtmp[s % 2]
                gelu_tanh(vg[:], vg, pv[:], [pv], tmp)
                P.op("dve", lambda e: e.bn_stats(bst[:, 0, :], vg[:, 0:256]), reads=[vg], writes=[bst])
                P.op("dve", lambda e: e.bn_stats(bst[:, 1, :], vg[:, 256:512]), reads=[vg], writes=[bst])
                P.op("dve", lambda e: e.bn_aggr(bag[:], bst[:]), reads=[bst], writes=[bag])
                P.op("dve", lambda e: e.tensor_scalar_add(vrs[:], bag[:, 1:2], EPS), reads=[bag], writes=[vrs])
                P.op("act", lambda e: e.sqrt(vrs[:], vrs[:]), reads=[], writes=[vrs])
                P.op("dve", lambda e: e.reciprocal(vrs[:], vrs[:]), reads=[], writes=[vrs])
                P.op("dve", lambda e: e.tensor_scalar(vg[:], vg[:], bag[:, 0:1], vrs[:, 0:1], ALU.subtract, ALU.mult),
                     reads=[bag, vrs], writes=[vg])
                P.op("pool", lambda e: e.tensor_tensor(vg[:], vg[:], gln[:, 0, :], ALU.mult), reads=[gln], writes=[vg])
                vl = vln[s % 2]
                P.op("pool", lambda e: e.tensor_tensor(vl[:], vg[:], gln[:, 1, :], ALU.add), reads=[vg, gln], writes=[vl])
                for g in range(4):
                    pss = psb[P.alt("ps", 8)]
                    P.mm(pss, vl[:, g * 128:(g + 1) * 128], wsT[:, g, :], True, True, reads=[vl, wsT], out=pss[:, 0:128])
                    P.op("dve", lambda e: e.tensor_tensor(svt[:], pss[:, 0:128], bsb[:, g, :], ALU.add),
                         reads=[pss, bsb], writes=[svt])
                    P.op("dve", lambda e: e.tensor_tensor(sp[:, g, s * 128:(s + 1) * 128], svt[:],
                                                          ug[:, g, s * 128:(s + 1) * 128], ALU.mult),
                         reads=[svt, ug], writes=[sp])
            dsts = spT_d[:, c0:c0 + 512].rearrange("(k p) t -> p k t", p=128)
            P.dma("sp", dsts, sp[:], reads=[sp], writes=[spT_db])
            fb = flb[it % 2]
            for g in range(4):
                pf = psb[P.alt("ps", 8)]
                P.proj_fm(pf, wf, g * 128, ub, [wf, ub])
                fc = fch[g % 2]
                P.op("act", lambda e: e.activation(out=fc[:], in_=pf[:], func=AF.Copy), reads=[pf], writes=[fc])
                mean, rstd = ln_stats([(fc[:], fc)], 1, 512, 1.0 / 128, "fln")
                P.op("dve", lambda e: e.tensor_tensor(fc[:], fc[:], mean[:], ALU.subtract), reads=[mean], writes=[fc])
                P.op("pool", lambda e: e.tensor_tensor(fc[:], fc[:], rstd[:], ALU.mult), reads=[rstd], writes=[fc])
                P.op("dve", lambda e: e.tensor_scalar(fb[:, g, :], fc[:], fl[:, 0, g:g + 1], fl[:, 1, g:g + 1],
                                                      ALU.mult, ALU.add),
                     reads=[fc, fl], writes=[fb])
            for s in range(4):
                ab = abo[s % 2]
                for g in range(4):
                    pab = psb[P.alt("ps", 8)]
                    P.mm(pab, fb[:, g, s * 128:(s + 1) * 128], dcs[:], True, True, reads=[fb, dcs], out=pab[:, 0:256])
                    P.op("act", lambda e: e.activation(out=ab[:, g * 256:(g + 1) * 256], in_=pab[:, 0:256], func=AF.Copy),
                         reads=[pab], writes=[ab])
                P.dma("sp", ab_own[c0 + s * 128:c0 + (s + 1) * 128, :], ab[:], reads=[ab], writes=[ab_ownb])
        P.release(mk)

    if L4:
        mk = P.mark()
        c128s = P.sb("c128s", [128, 256], BF16)
        P.dma("sp", c128s[:], c128, writes=[c128s])
        tws = P.sb("tws", [128, 3, 128], F32)
        P.dma("sp", tws[:], tw_c, writes=[tws])
        ins_ = [P.sb("fin%d" % i, [128, 1024], BF16) for i in range(3)]
        p2s = [P.sb("p2s%d" % i, [128, 512], F32) for i in range(2)]
        yr = [P.sb("yr%d" % i, [128, 2, 128], F32) for i in range(2)]
        ym = [P.sb("ym%d" % i, [128, 2, 128], F32) for i in range(2)]
        t1 = [P.sb("tt1%d" % i, [128, 2, 128], F32) for i in range(2)]
        t2 = [P.sb("tt2%d" % i, [128, 2, 128], F32) for i in range(2)]
        zo = [P.sb("zo%d" % i, [128, 1024], BF16) for i in range(3)]
        abv = ab_full.rearrange("(n1 n2) c -> n1 n2 c", n2=128)
        for n2 in range(128):
            fin = ins_[n2 % 3]
            P.dma("sp", fin[:], abv[:, n2, :], reads=[ab_fullb], writes=[fin])
            z = zo[n2 % 3]
            for hf in range(2):
                p1 = psb[P.alt("ps", 8)]
                p2 = psb[P.alt("ps", 8)]
                P.mm(p1, c128s[:, 0:128], fin[:, hf * 512:(hf + 1) * 512], True, True, reads=[c128s, fin])
                P.mm(p2, c128s[:, 128:256], fin[:, hf * 512:(hf + 1) * 512], True, True, reads=[c128s, fin])
                s2 = p2s[hf]
                P.op("act", lambda e: e.activation(out=s2[:], in_=p2[:], func=AF.Copy), reads=[p2], writes=[s2])
                p1v = p1[:].rearrange("p (g a c) -> p g a c", g=2, a=2)
                s2v = s2[:].rearrange("p (g a c) -> p g a c", g=2, a=2)
                Yr, Ym, T1, T2 = yr[hf], ym[hf], t1[hf], t2[hf]
                P.op("dve", lambda e: e.tensor_tensor(Yr[:], p1v[:, :, 0, :], s2v[:, :, 1, :], ALU.subtract),
                     reads=[p1, s2], writes=[Yr])
                P.op("dve", lambda e: e.tensor_tensor(Ym[:], p1v[:, :, 1, :], s2v[:, :, 0, :], ALU.add),
                     reads=[p1, s2], writes=[Ym])
                P.op("act", lambda e: e.activation(out=T1[:], in_=Yr[:], func=AF.Identity, scale=tws[:, 0, n2:n2 + 1]),
                     reads=[Yr, tws], writes=[T1])
                P.op("act", lambda e: e.activation(out=T2[:], in_=Ym[:], func=AF.Identity, scale=tws[:, 0, n2:n2 + 1]),
                     reads=[Ym, tws], writes=[T2])
                zr = z[:, hf * 256:(hf + 1) * 256].rearrange("p (g c) -> p g c", g=2)
                zi = z[:, 512 + hf * 256:512 + (hf + 1) * 256].rearrange("p (g c) -> p g c", g=2)
                P.op("dve", lambda e: e.scalar_tensor_tensor(zr, Ym[:], tws[:, 2, n2:n2 + 1], T1[:], ALU.mult, ALU.add),
                     reads=[Ym, T1, tws], writes=[z])
                P.op("dve", lambda e: e.scalar_tensor_tensor(zi, Yr[:], tws[:, 1, n2:n2 + 1], T2[:], ALU.mult, ALU.add),
                     reads=[Yr, T2, tws], writes=[z])
            P.dma("sp", Zd[:, n2, :], z[:], reads=[z], writes=[Zdb])
        P.release(mk)

        mk = P.mark()
        cs32s = P.sb("cs32s", [128, 64], BF16)
        P.dma("sp", cs32s[:], cs32, writes=[cs32s])
        zts = [P.sb("zt%d" % i, [128, 1024], BF16) for i in range(3)]
        fo_sb = P.sb("fo_sb", [128, 4, 32, 128], BF16)
        for k1 in range(128):
            zt = zts[k1 % 3]
            P.dma("sp", zt[:], Zd[k1, :, :], reads=[Zdb], writes=[zt])
            px = psb[P.alt("ps", 8)]
            for g in range(4):
                P.mm(px, zt[:, g * 128:(g + 1) * 128], cs32s[:, 0:32], True, False, reads=[zt, cs32s],
                     out=px[:, g * 32:(g + 1) * 32], last=False)
                P.mm(px, zt[:, 512 + g * 128:512 + (g + 1) * 128], cs32s[:, 32:64], False, True, reads=[zt, cs32s],
                     out=px[:, g * 32:(g + 1) * 32], last=(g == 3))
            P.op("act", lambda e: e.activation(out=fo_sb[:, :, :, k1],
                                               in_=px[:, 0:128].rearrange("p (g k) -> p g k", g=4), func=AF.Copy),
                 reads=[px], writes=[fo_sb])
        dstf = fouT_d.rearrange("(g p) t -> p g t", p=128)
        for g in range(4):
            P.dma("sp", dstf[:, g, :], fo_sb[:, g, :, :].rearrange("p a b -> p (a b)"), reads=[fo_sb], writes=[fouT_db])
        P.release(mk)
        post_mixer(1, od_w_out, spT_d, spT_db, fouT_d, fouT_db, h2T_d, h2T_db)

    P.barrier(engines=["sp"])
    P.close()
    return P


def build_E():
    P = Prog("E")
    nc = P.nc
    NL = 8
    NTK = SEQ
    u2T_all = P.inp("u2T_all", [D, NTK], BF16)
    gT_loc = P.inp("gT_loc", [NL, NTK])
    wgu_d = P.inp("wgu", [NL, D, 2 * D])
    wd_d = P.inp("wd", [NL, D, D])
    bguT = P.inp("bguT", [128, NL, 16])
    bd_d = P.inp("bd", [NL, D])
    y2p = nc.dram_tensor("y2p", [D, NTK], F32, kind="ExternalOutput").ap()
    y2pb = Buf("y2p")
    psb = P.psb
    QT = 1024
    u2q = P.sb("u2q", [128, 8, QT], BF16)
    y2 = P.sb("y2", [128, 8, QT], F32)
    gTqs = [P.sb("gTq%d" % i, [NL, QT], F32) for i in range(2)]
    bd = P.sb("bd", [NL, D], F32)
    P.dma("sp", bd[:], bd_d, writes=[bd])
    bgu = P.sb("bgu", [128, NL, 16], F32)
    P.dma("sp", bgu[:], bguT, writes=[bgu])
    wgus = [P.sb("wgu%d" % i, [128, 8, 2 * D], BF16) for i in range(2)]
    wds = [P.sb("wd%d" % i, [128, 8, D], BF16) for i in range(1)]
    Ge = [P.sb("Ge%d" % i, [128, 512], F32) for i in range(2)]
    xg = [P.sb("xg%d" % i, [128, 512], F32) for i in range(2)]
    sg = [P.sb("sgm%d" % i, [128, 512], F32) for i in range(2)]
    xl = [P.sb("xl%d" % i, [128, 512], F32) for i in range(2)]
    actb = [P.sb("actb%d" % i, [128, 8, 512], BF16) for i in range(2)]
    for q in range(NTK // QT):
        q0 = q * QT
        gTq = gTqs[q % 2]
        srcu = u2T_all[:, q0:q0 + QT].rearrange("(k p) t -> p k t", p=128)
        for kk in range(8):
            P.dma("sp", u2q[:, kk, :], srcu[:, kk, :], writes=[u2q])
        P.dma("sp", gTq[:], gT_loc[:, q0:q0 + QT], writes=[gTq])
        for tt in range(QT // 512):
            for m in range(8):
                pb_ = psb[P.alt("ps", 8)]
                P.mm(pb_, bd[:, m * 128:(m + 1) * 128], gTq[:, tt * 512:(tt + 1) * 512], True, True, reads=[bd, gTq])
                P.op("act", lambda e: e.activation(out=y2[:, m, tt * 512:(tt + 1) * 512], in_=pb_[:], func=AF.Copy),
                     reads=[pb_], writes=[y2])
        for ei in range(NL):
            wgu = wgus[ei % 2]
            wd = wds[0]
            srcg = wgu_d[ei].rearrange("(k p) n -> p k n", p=128)
            srcd = wd_d[ei].rearrange("(k p) n -> p k n", p=128)
            for kk in range(8):
                P.dma("pool", wgu[:, kk, :], srcg[:, kk, :], writes=[wgu])
            for kk in range(8):
                P.dma("pool", wd[:, kk, :], srcd[:, kk, :], writes=[wd])
            for tt in range(QT // 512):
                t0 = tt * 512
                G = Ge[P.alt("Ge", 2)]
                P.dma("sp", G[:], gT_loc[ei:ei + 1, q0 + t0:q0 + t0 + 512].partition_broadcast(128), writes=[G])
                ab = actb[P.alt("actb", 2)]
                for m in range(8):
                    pgl = psb[P.alt("ps", 8)]
                    pll = psb[P.alt("ps", 8)]
                    for kk in range(8):
                        P.mm(pgl, wgu[:, kk, m * 128:(m + 1) * 128], u2q[:, kk, t0:t0 + 512], kk == 0, kk == 7,
                             reads=[wgu, u2q])
                    for kk in range(8):
                        P.mm(pll, wgu[:, kk, D + m * 128:D + (m + 1) * 128], u2q[:, kk, t0:t0 + 512], kk == 0, kk == 7,
                             reads=[wgu, u2q])
                    a1 = xg[m % 2]
                    a2 = sg[m % 2]
                    a3 = xl[m % 2]
                    P.op("dve", lambda e: e.tensor_scalar(a1[:], pgl[:], bgu[:, ei, m:m + 1], 7.0, ALU.add, ALU.min),
                         reads=[pgl, bgu], writes=[a1])
                    P.op("act", lambda e: e.activation(out=a2[:], in_=a1[:], func=AF.Sigmoid, scale=1.702),
                         reads=[a1], writes=[a2])
                    P.op("dve", lambda e: e.tensor_scalar(a3[:], pll[:], bgu[:, ei, 8 + m:9 + m], -7.0, ALU.add, ALU.max),
                         reads=[pll, bgu], writes=[a3])
                    P.op("dve", lambda e: e.tensor_scalar(a3[:], a3[:], 7.0, 1.0, ALU.min, ALU.add),
                         reads=[], writes=[a3])
                    P.op("pool", lambda e: e.tensor_tensor(a1[:], a1[:], a2[:], ALU.mult), reads=[a2], writes=[a1])
                    P.op("pool", lambda e: e.tensor_tensor(a1[:], a1[:], a3[:], ALU.mult), reads=[a3], writes=[a1])
                    P.op("pool", lambda e: e.tensor_tensor(ab[:, m, :], a1[:], G[:], ALU.mult), reads=[a1, G], writes=[ab])
                for m in range(8):
                    pd = psb[P.alt("ps", 8)]
                    for kk in range(8):
                        P.mm(pd, wd[:, kk, m * 128:(m + 1) * 128], ab[:, kk, :], kk == 0, kk == 7, reads=[wd, ab])
                    P.op("dve", lambda e: e.tensor_tensor(y2[:, m, t0:t0 + 512], y2[:, m, t0:t0 + 512], pd[:], ALU.add),
                         reads=[pd], writes=[y2])
        dsty = y2p[:, q0:q0 + QT].rearrange("(k p) t -> p k t", p=128)
        for kk in range(8):
            P.dma("sp", dsty[:, kk, :], y2[:, kk, :], reads=[y2], writes=[y2pb])
    P.barrier(engines=["sp"])
    P.close()
    return P


def _fm(v, nch):
    return np.ascontiguousarray(np.asarray(v, np.float32).reshape(nch, 128).T)


def _rope_tables():
    t = np.arange(SEQ)
    row = (t // 64).astype(np.float64)
    col = (t % 64).astype(np.float64)
    inv = 10000.0 ** (-np.arange(16, dtype=np.float64) / 16)
    cos = np.zeros((64, SEQ))
    ssin = np.zeros((64, SEQ))
    for half, pos in enumerate((row, col)):
        ang = inv[:, None] * pos[None, :]
        c, s = np.cos(ang), np.sin(ang)
        b = half * 32
        cos[b:b + 16] = c
        cos[b + 16:b + 32] = c
        ssin[b:b + 16] = -s
        ssin[b + 16:b + 32] = s
    cos = np.concatenate([cos, cos], 0)
    ssin = np.concatenate([ssin, ssin], 0)
    return np.stack([cos, ssin], 1).astype(np.float32)


def _perm64():
    p = np.arange(64)
    out = p.copy()
    for b in (0, 32):
        out[b:b + 16] = p[b + 16:b + 32]
        out[b + 16:b + 32] = p[b:b + 16]
    return out


_CACHE = {}


def _get_prog(mode):
    if mode not in _CACHE:
        _CACHE[mode] = build_E() if mode == "E" else build(mode)
    return _CACHE[mode]


def _run(mode, maps):
    p = _get_prog(mode)
    maps = [{k: v for k, v in m.items() if k in p.ins} for m in maps]
    for m in maps:
        missing = [k for k in p.ins if k not in m]
        assert not missing, (mode, missing)
    return run_bass_kernel_spmd(p.nc, maps, core_ids=list(range(len(maps)))).results


def _common(inp):
    f32 = np.float32
    ln_vecs = np.zeros((128, 2, 4, 8), f32)
    for l in range(2):
        for i, nm in enumerate(("ln1_g", "ln1_b", "ln2_g", "ln2_b")):
            ln_vecs[:, l, i, :] = _fm(inp[nm][l], 8)
    return dict(ln_vecs=ln_vecs, router_w=np.asarray(inp["router_w"], f32),
                router_b=np.asarray(inp["router_b"], f32).reshape(2, 1, NE), ident=np.eye(128, dtype=f32))


def _maps_L1(inp):
    f32 = np.float32
    x = np.asarray(inp["x"], f32)
    ctx = np.asarray(inp["ctx"], f32)
    c = np.asarray(inp["c"], f32)
    c_ctx = np.asarray(inp["c_ctx"], f32)
    cs = _rope_tables()
    perm = np.concatenate([_perm64() + 64 * i for i in range(16)])
    w_in = np.asarray(inp["ev_w_in"][0], f32)
    w_qk_perm = np.ascontiguousarray(w_in[:, :1024][:, perm])
    b_modT = np.ascontiguousarray(np.asarray(inp["b_mod"], f32).reshape(2, 48, 128).transpose(2, 0, 1))
    common = _common(inp)
    xT = [np.ascontiguousarray(x[b].T) for b in range(NB)]
    maps = []
    for core in range(8):
        b, j = core // 4, core % 4
        t0 = j * TOK
        own = np.zeros((D, TOK + 30), f32)
        own[:, :TOK] = xT[b][:, t0:t0 + TOK]
        hmask = np.zeros((128, 30), f32)
        if t0 > 0:
            own[:, TOK:TOK + 15] = xT[b][:, t0 - 15:t0]
            hmask[:, 0:15] = 1.0
        if t0 + TOK < SEQ:
            own[:, TOK + 15:TOK + 30] = xT[b][:, t0 + TOK:t0 + TOK + 15]
            hmask[:, 15:30] = 1.0
        cT = np.zeros((128, 8, 2), f32)
        cT[:, :, 0] = _fm(c[b], 8)
        cT[:, :, 1] = _fm(c_ctx, 8)
        m = dict(common)
        m.update(
            xT_full=xT[b], xT_own=own, halo_mask=hmask, ctxT=np.ascontiguousarray(ctx[b].T),
            cT=cT.reshape(128, 16), w_mod=np.asarray(inp["w_mod"], f32), b_modT=b_modT,
            ev_w_in=w_in, w_qk_perm=w_qk_perm, cs_full=cs, cs_own=np.ascontiguousarray(cs[:, :, t0:t0 + TOK]),
            conv_wT=np.ascontiguousarray(np.asarray(inp["conv_w"][0], f32).T.reshape(4, 128, 31).transpose(1, 0, 2)),
            conv_vecs=np.ascontiguousarray(np.stack([_fm(inp["conv_b"][0], 4), _fm(inp["conv_ln_g"][0], 4),
                                                     _fm(inp["conv_ln_b"][0], 4)], 1)),
            lam_vecs=np.stack([inp["lam_q1"][0], inp["lam_k1"][0], inp["lam_q2"][0], inp["lam_k2"][0]], 0
                              ).astype(f32).reshape(1, 4, 64),
            diff_g=np.asarray(inp["diff_norm_g"][0], f32).reshape(128, 1),
            ev_w_out=np.asarray(inp["ev_w_out"][0], f32))
        maps.append(m)
    return maps


def _maps_E(inp, l, prev):
    f32 = np.float32
    maps = []
    for core in range(8):
        b, j = core // 4, core % 4
        e0 = 8 * j
        u2 = np.concatenate([np.asarray(prev[b * 4 + jj]["u2T_d"]) for jj in range(4)], 1)
        g = np.concatenate([np.asarray(prev[b * 4 + jj]["gT_d"], f32)[e0:e0 + 8] for jj in range(4)], 1)
        bgu = np.asarray(inp["b_gate_up"][l, e0:e0 + 8], f32).reshape(8, 16, 128).transpose(2, 0, 1)
        maps.append(dict(u2T_all=np.ascontiguousarray(u2), gT_loc=np.ascontiguousarray(g),
                         wgu=np.ascontiguousarray(inp["w_gate_up"][l, e0:e0 + 8], dtype=f32),
                         wd=np.ascontiguousarray(inp["w_down"][l, e0:e0 + 8], dtype=f32),
                         bguT=np.ascontiguousarray(bgu), bd=np.ascontiguousarray(inp["b_down"][l, e0:e0 + 8], dtype=f32)))
    return maps


def _partials(rE, core):
    b, j = core // 4, core % 4
    return np.ascontiguousarray(np.stack([np.asarray(rE[b * 4 + jj]["y2p"], np.float32)[:, j * TOK:(j + 1) * TOK]
                                          for jj in range(4)], 0))


def _maps_L3(inp, r1, rE):
    f32 = np.float32
    common = _common(inp)
    cidx = np.arange(128)
    ang = 2 * np.pi * np.outer(cidx, cidx) / 128.0
    sc = 1.0 / math.sqrt(SEQ * 128.0)
    dft_cs = np.concatenate([np.cos(ang) * sc, np.sin(ang) * sc], 1).astype(ml_dtypes.bfloat16)
    maps = []
    for core in range(8):
        m = dict(common)
        m.update(
            y2p4=_partials(rE, core), h1T_d=np.asarray(r1[core]["h1T_d"], f32), modT_d=np.asarray(r1[core]["modT_d"], f32),
            od_w_in=np.asarray(inp["od_w_in"][0], f32),
            gmlp_ln=np.stack([inp["gmlp_ln_g"][0], inp["gmlp_ln_b"][0]], 0).astype(f32).reshape(1, 2, 512),
            gmlp_wsT=np.ascontiguousarray(np.asarray(inp["gmlp_ws"][0], f32).transpose(2, 0, 1)),
            gmlp_bs=np.asarray(inp["gmlp_bs"][0], f32).reshape(1, 4, 128),
            four_ln=np.ascontiguousarray(np.stack([_fm(inp["four_ln_g"][0], 4), _fm(inp["four_ln_b"][0], 4)], 1)),
            dft_cs=dft_cs)
        maps.append(m)
    return maps


def _maps_L4(inp, r1, r3):
    f32 = np.float32
    common = _common(inp)
    idx = np.arange(128)
    ang = 2 * np.pi * np.outer(idx, idx) / 128.0
    c128 = np.concatenate([np.cos(ang), np.sin(ang)], 1).astype(ml_dtypes.bfloat16)
    angt = 2 * np.pi * np.outer(idx, idx) / float(SEQ)
    tw = np.ascontiguousarray(np.stack([np.cos(angt), np.sin(angt), -np.sin(angt)], 1).astype(f32))
    maps = []
    for core in range(8):
        b, j = core // 4, core % 4
        k2 = 32 * j + np.arange(32)
        a2 = 2 * np.pi * np.outer(idx, k2) / 128.0
        cs32 = np.concatenate([np.cos(a2), -np.sin(a2)], 1).astype(ml_dtypes.bfloat16)
        ab_full = np.concatenate([np.asarray(r3[b * 4 + jj]["ab_own"]) for jj in range(4)], 0)
        m = dict(common)
        m.update(od_w_out=np.asarray(inp["od_w_out"][0], f32), c128=c128, tw_c=tw, cs32=cs32,
                 ab_full=np.ascontiguousarray(ab_full), spT_d=np.asarray(r3[core]["spT_d"]),
                 h2T_d=np.asarray(r3[core]["h2T_d"], f32), modT_d=np.asarray(r1[core]["modT_d"], f32))
        maps.append(m)
    return maps


def _maps_L6(inp, r1, r4, rE):
    f32 = np.float32
    common = _common(inp)
    maps = []
    for core in range(8):
        m = dict(common)
        m.update(y2p4=_partials(rE, core), h1T_d=np.asarray(r4[core]["h1T_d"], f32),
                 modT_d=np.asarray(r1[core]["modT_d"], f32))
        maps.append(m)
    return maps


def kernel(**inputs):
    r1 = _run("L1", _maps_L1(inputs))
    rE0 = _run("E", _maps_E(inputs, 0, r1))
    r3 = _run("L3", _maps_L3(inputs, r1, rE0))
    del rE0
    r4 = _run("L4", _maps_L4(inputs, r1, r3))
    rE1 = _run("E", _maps_E(inputs, 1, r4))
    r6 = _run("L6", _maps_L6(inputs, r1, r4, rE1))
    out = np.zeros((NB, SEQ, D), np.float32)
    for core in range(8):
        b, j = core // 4, core % 4
        out[b, j * TOK:(j + 1) * TOK, :] = np.asarray(r6[core]["outT"], np.float32).T
    return out
```

```python
import math
import numpy as np
import ml_dtypes
import concourse.bass as bass
import concourse.mybir as mybir
from concourse.bass_utils import run_bass_kernel_spmd

F32 = mybir.dt.float32
BF16 = mybir.dt.bfloat16
AF = mybir.ActivationFunctionType
ALU = mybir.AluOpType
AX = mybir.AxisListType

D = 1024
SEQ = 16384
NB = 2
TOK = 4096
NT = TOK // 512
CTX = 256
NKEY = SEQ + CTX
NKT = NKEY // 128
NE = 32
ALPHA = 4.0 ** 0.25
EPS = 1e-5
LAM_INIT0 = 0.8 - 0.6 * math.exp(-0.3 * 0)


class Buf:
    __slots__ = ("name", "w", "r")

    def __init__(self, name=""):
        self.name = name
        self.w = None
        self.r = []


class T:
    def __init__(self, t, name):
        self.t = t
        self.b = Buf(name)

    def __getitem__(self, idx):
        return self.t[idx]


class KB:
    ENG = ("pe", "act", "dve", "pool", "sp")

    def __init__(self, n_dma_sems=32):
        self.nc = nc = bass.Bass("TRN2", target_bir_lowering=False)
        self.e = dict(pe=nc.tensor, act=nc.scalar, dve=nc.vector, pool=nc.gpsimd, sp=nc.sync)
        self._ctx = []
        self._semctx = []
        self.gen = {}
        self.tot = {}
        self.sem = {}
        self.cnt = {}
        for n in self.ENG:
            self.sem[n] = self._enter(nc.semaphore("s_" + n))
            self.cnt[n] = 0
        self.dsem = [self._enter(nc.semaphore("d%d" % i)) for i in range(n_dma_sems)]
        self.dcnt = [0] * n_dma_sems
        self.dnext = 0
        self.semobj = {}
        for i, s in enumerate(self.dsem):
            self.semobj[("d", i)] = s
        self.waited = {n: {} for n in self.ENG}
        self.n_inst = 0

    SEM_ROT = 4000

    def _enter(self, cm):
        v = cm.__enter__()
        self._ctx.append(cm)
        return v

    def _sem_new(self, name):
        cm = self.nc.semaphore(name)
        v = cm.__enter__()
        self._semctx.append(cm)
        return v

    def mark(self):
        return len(self._ctx)

    def release(self, mark):
        self.barrier()
        while len(self._ctx) > mark:
            self._ctx.pop().__exit__(None, None, None)

    def close(self):
        while self._ctx:
            self._ctx.pop().__exit__(None, None, None)
        while self._semctx:
            self._semctx.pop().__exit__(None, None, None)

    def sb(self, name, shape, dtype):
        self._uid = getattr(self, "_uid", 0) + 1
        name = "%s_%d" % (name, self._uid)
        return T(self._enter(self.nc.sbuf_tensor(name, list(shape), dtype)), name)

    def ps(self, name, shape, dtype=F32):
        return T(self._enter(self.nc.psum_tensor(name, list(shape), dtype)), name)

    def _wait(self, eng, ev):
        if ev is None:
            return
        key, val = ev
        if key[0] == "e" and key[1] == "pe" and eng == "pe":
            return
        w = self.waited[eng]
        if w.get(key, 0) >= val:
            return
        w[key] = val
        self.e[eng].wait_ge(self.semobj[key], val)

    @staticmethod
    def _b(x):
        return x.b if isinstance(x, T) else x

    def _deps(self, eng, reads, writes):
        for b in reads:
            self._wait(eng, self._b(b).w)
        for b in writes:
            b = self._b(b)
            self._wait(eng, b.w)
            for ev in b.r:
                self._wait(eng, ev)

    def _commit(self, ev, reads, writes):
        for b in reads:
            b = self._b(b)
            b.r.append(ev)
            if len(b.r) > 16:
                d = {}
                for k, v in b.r:
                    if d.get(k, 0) < v:
                        d[k] = v
                b.r = list(d.items())
        for b in writes:
            b = self._b(b)
            b.w = ev
            b.r = []

    def op(self, eng, fn, reads=(), writes=(), inc=True):
        self._deps(eng, reads, writes)
        ins = fn(self.e[eng])
        self.n_inst += 1
        key = ("e", eng, self.gen.get(eng, 0))
        if key not in self.semobj:
            self.semobj[key] = self.sem[eng]
        ev = (key, self.cnt[eng] + 1)
        if inc:
            ins.then_inc(self.sem[eng], 1)
            self.cnt[eng] += 1
        self._commit(ev, reads, writes)
        if inc and self.cnt[eng] >= self.SEM_ROT:
            self.gen[eng] = self.gen.get(eng, 0) + 1
            self.sem[eng] = self._sem_new("s_%s_%d" % (eng, self.gen[eng]))
            self.cnt[eng] = 0
            self.tot[eng] = self.tot.get(eng, 0) + self.SEM_ROT
        return ins

    def dma(self, q, out, in_, reads=(), writes=(), **kw):
        j = self.dnext
        self.dnext = (self.dnext + 1) % len(self.dsem)
        self._deps(q, reads, writes)
        if self.dcnt[j]:
            self._wait(q, (("d", j), self.dcnt[j]))
        ins = self.e[q].dma_start(out=out, in_=in_, **kw)
        self.n_inst += 1
        self.dcnt[j] += 16
        ins.then_inc(self.dsem[j], 16)
        ev = (("d", j), self.dcnt[j])
        self._commit(ev, reads, writes)
        return ev

    def barrier(self, engines=None):
        for E in (engines or self.ENG):
            for F in self.ENG:
                if F != E and self.cnt[F] > 0:
                    self._wait(E, (("e", F, self.gen.get(F, 0)), self.cnt[F]))
            for j in range(len(self.dsem)):
                if self.dcnt[j]:
                    self._wait(E, (("d", j), self.dcnt[j]))


class Prog(KB):
    def __init__(self, mode, dbg=None):
        super().__init__()
        self.dbg = dbg
        self.mode = mode
        self.ins = {}
        self.psb = [self.ps("psb%d" % i, [128, 512], F32) for i in range(8)]
        self.rr = {}

    def inp(self, name, shape, dtype=F32):
        if False and name in ("w_gate_up", "w_down", "b_guT", "b_down", "od_w_in",
                                                            "gmlp_ln", "gmlp_wsT", "gmlp_bs", "four_ln", "dft_cs"):
            return None
        t = self.nc.dram_tensor(name, list(shape), dtype, kind="ExternalInput")
        self.ins[name] = (tuple(shape), dtype)
        return t.ap()

    def scratch(self, name, shape, dtype, boundary=None):
        kind = "Internal"
        if boundary is not None and self.mode != "ALL":
            kind = "ExternalOutput" if self.mode == "L1" else "ExternalInput"
            if kind == "ExternalInput":
                self.ins[name] = (tuple(shape), dtype)
        t = self.nc.dram_tensor(name, list(shape), dtype, kind=kind)
        return t.ap(), Buf(name)

    def alt(self, key, n):
        v = self.rr.get(key, 0)
        self.rr[key] = v + 1
        return v % n

    def mm(self, ps, lhsT, rhs, start, stop, reads, last=None, out=None):
        if last is None:
            last = stop
        o = ps[:] if out is None else out
        return self.op("pe", lambda e: e.matmul(o, lhsT, rhs, start=start, stop=stop),
                       reads=reads, writes=[ps], inc=last)

    def proj_fm(self, ps, w, col0, ub, ubuf_reads, ncols=512, nk=8):
        for k in range(nk):
            self.mm(ps, w[:, k, col0:col0 + 128], ub[:, k, 0:ncols], k == 0, k == nk - 1,
                    reads=ubuf_reads, out=ps[:, 0:ncols])

    def load_w_bf16(self, name, dram_ap, ncols, nk=8):
        t = self.sb(name, [128, nk, ncols], BF16)
        src = dram_ap.rearrange("(k p) n -> p k n", p=128)
        for k in range(nk):
            self.dma("pool", t[:, k, :], src[:, k, :], writes=[t])
        return t


def _f(v):
    return float(v)


def build(mode, dbg=None):
    P = Prog(mode, dbg)
    nc = P.nc
    L1 = mode == "L1"
    L3 = mode == "L3"
    L4 = mode == "L4"
    L6 = mode == "L6"

    def ext_out(name, shape, dtype):
        return nc.dram_tensor(name, list(shape), dtype, kind="ExternalOutput").ap(), Buf(name)

    def ext_in(name, shape, dtype=F32):
        return P.inp(name, shape, dtype), Buf(name)

    def internal(name, shape, dtype):
        return nc.dram_tensor(name, list(shape), dtype, kind="Internal").ap(), Buf(name)

    if L1:
        xT_full = P.inp("xT_full", [D, SEQ])
        xT_own = P.inp("xT_own", [D, TOK + 30])
        halo_mask = P.inp("halo_mask", [128, 30])
        ctxT = P.inp("ctxT", [D, CTX])
        cT = P.inp("cT", [128, 16])
        w_mod = P.inp("w_mod", [2, D, 6 * D])
        b_modT = P.inp("b_modT", [128, 2, 48])
        ev_w_in = P.inp("ev_w_in", [D, 2560])
        w_qk_perm = P.inp("w_qk_perm", [D, 1024])
        cs_full = P.inp("cs_full", [128, 2, SEQ])
        cs_own = P.inp("cs_own", [128, 2, TOK])
        conv_wT = P.inp("conv_wT", [128, 4, 31])
        conv_vecs = P.inp("conv_vecs", [128, 3, 4])
        lam_vecs = P.inp("lam_vecs", [1, 4, 64])
        diff_g = P.inp("diff_g", [128, 1])
        ev_w_out = P.inp("ev_w_out", [D, D])
    if L3:
        od_w_in = P.inp("od_w_in", [D, 1536])
        gmlp_ln = P.inp("gmlp_ln", [1, 2, 512])
        gmlp_wsT = P.inp("gmlp_wsT", [128, 4, 128])
        gmlp_bs = P.inp("gmlp_bs", [1, 4, 128])
        four_ln = P.inp("four_ln", [128, 2, 4])
        dft_cs = P.inp("dft_cs", [128, 256], BF16)
    if L4:
        od_w_out = P.inp("od_w_out", [D, D])
        c128 = P.inp("c128", [128, 256], BF16)
        tw_c = P.inp("tw_c", [128, 3, 128])
        cs32 = P.inp("cs32", [128, 64], BF16)
    ln_vecs = P.inp("ln_vecs", [128, 2, 4, 8])
    if L1 or L4:
        router_w = P.inp("router_w", [2, D, NE])
        router_b = P.inp("router_b", [2, 1, NE])
    ident = P.inp("ident", [128, 128])

    if L1:
        modT_d, modT_db = ext_out("modT_d", [128, 2, 48, 2], F32)
        kT_d, kT_db = internal("kT_d", [4, 128, NKEY], BF16)
        v_d, v_db = internal("v_d", [NKEY, 512], BF16)
        qT_d, qT_db = internal("qT_d", [4, 128, TOK], BF16)
        catT_d, catT_db = internal("catT_d", [D, TOK], BF16)
    else:
        modT_d, modT_db = ext_in("modT_d", [128, 2, 48, 2], F32)
    if L1 or L4:
        h1T_d, h1T_db = ext_out("h1T_d", [D, TOK], F32)
        u2T_d, u2T_db = ext_out("u2T_d", [D, TOK], BF16)
        gT_d, gT_db = ext_out("gT_d", [NE, TOK], F32)
    if L3 or L6:
        h1T_d, h1T_db = ext_in("h1T_d", [D, TOK], F32)
        y2p4, y2p4b = ext_in("y2p4", [4, D, TOK], F32)
    if L3:
        h2T_d, h2T_db = ext_out("h2T_d", [D, TOK], F32)
        spT_d, spT_db = ext_out("spT_d", [512, TOK], BF16)
        ab_own, ab_ownb = ext_out("ab_own", [TOK, 1024], BF16)
    if L4:
        h2T_d, h2T_db = ext_in("h2T_d", [D, TOK], F32)
        spT_d, spT_db = ext_in("spT_d", [512, TOK], BF16)
        ab_full, ab_fullb = ext_in("ab_full", [SEQ, 1024], BF16)
        Zd, Zdb = internal("Zd", [128, 128, 1024], BF16)
        fouT_d, fouT_db = internal("fouT_d", [512, TOK], BF16)
    if L6:
        outT, outT_b = ext_out("outT", [D, TOK], F32)

    psb = P.psb

    psb = P.psb

    ones_bf = P.sb("ones_bf", [128, 128], BF16)
    P.op("dve", lambda e: e.memset(ones_bf[:], 1.0), writes=[ones_bf])
    modT = P.sb("modT", [128, 2, 48, 2], F32)
    mod1p = P.sb("mod1p", [128, 2, 48, 2], F32)
    lnv = P.sb("lnv", [128, 2, 4, 8], F32)
    P.dma("sp", lnv[:], ln_vecs, writes=[lnv])
    ident_sb = P.sb("ident_sb", [128, 128], F32)
    P.dma("sp", ident_sb[:], ident, writes=[ident_sb])

    def MOD(l, idx, ch, m=0, plus1=False):
        t = mod1p if plus1 else modT
        return t[:, l, idx * 8 + ch, m:m + 1]


    if L1:
        mk = P.mark()
        cT_sb = P.sb("cT_sb", [128, 16], F32)
        sT_sb = P.sb("sT_sb", [128, 16], F32)
        bm_sb = P.sb("bm_sb", [128, 2, 48], F32)
        P.dma("sp", cT_sb[:], cT, writes=[cT_sb])
        P.dma("sp", bm_sb[:], b_modT, writes=[bm_sb])
        P.op("act", lambda e: e.activation(out=sT_sb[:], in_=cT_sb[:], func=AF.Silu),
             reads=[cT_sb], writes=[sT_sb])
        wm = [P.sb("wm%d" % i, [128, 8, 768], F32) for i in range(2)]
        for l in range(2):
            for blk in range(8):
                wt = wm[P.alt("wm", 2)]
                src = w_mod[l, :, blk * 768:(blk + 1) * 768].rearrange("(k p) n -> p k n", p=128)
                for kk in range(8):
                    P.dma("sp", wt[:, kk, :], src[:, kk, :], writes=[wt])
                for mc in range(6):
                    ps = psb[P.alt("ps", 8)]
                    for kk in range(8):
                        P.mm(ps, wt[:, kk, mc * 128:(mc + 1) * 128], sT_sb[:, kk * 2:kk * 2 + 2],
                             kk == 0, kk == 7, reads=[wt, sT_sb], out=ps[:, 0:2])
                    ch = blk * 6 + mc
                    P.op("dve", lambda e: e.tensor_tensor(
                        modT[:, l, ch, :], ps[:, 0:2], bm_sb[:, l, ch:ch + 1].to_broadcast([128, 2]), ALU.add),
                        reads=[ps, bm_sb], writes=[modT])
        P.op("dve", lambda e: e.tensor_scalar_add(mod1p[:], modT[:], 1.0), reads=[modT], writes=[mod1p])
        P.dma("sp", modT_d, modT[:], reads=[modT], writes=[modT_db])
        P.release(mk)
        if dbg == "M":
            P.barrier(engines=["sp"])
            P.close()
            return P
    else:
        P.dma("sp", modT[:], modT_d, reads=[modT_db], writes=[modT])
        P.op("dve", lambda e: e.tensor_scalar_add(mod1p[:], modT[:], 1.0), reads=[modT], writes=[mod1p])

    def modulate_tile(xt, ub, l, idx_sh, idx_sc, m, ncols):
        for kk in range(8):
            if kk % 2 == 0:
                P.op("act", lambda e: e.activation(out=ub[:, kk, 0:ncols], in_=xt[:, kk, 0:ncols],
                                                   func=AF.Identity,
                                                   bias=MOD(l, idx_sh, kk, m),
                                                   scale=MOD(l, idx_sc, kk, m, True)),
                     reads=[xt, mod1p, modT], writes=[ub])
            else:
                P.op("dve", lambda e: e.tensor_scalar(ub[:, kk, 0:ncols], xt[:, kk, 0:ncols],
                                                      MOD(l, idx_sc, kk, m, True), MOD(l, idx_sh, kk, m),
                                                      ALU.mult, ALU.add),
                     reads=[xt, mod1p, modT], writes=[ub])

    def ln_stats(src_chunks, nch, ncols, inv_n, tag):
        pm = psb[P.alt("ps", 8)]
        pq = psb[P.alt("ps", 8)]
        for i, (ap, tl) in enumerate(src_chunks):
            xb = st[tag + "_xb"][P.alt(tag + "xb", 2)]
            x2b = st[tag + "_x2b"][P.alt(tag + "x2b", 2)]
            P.op("act", lambda e: e.activation(out=xb[:, 0:ncols], in_=ap, func=AF.Copy),
                 reads=[tl], writes=[xb])
            P.op("act", lambda e: e.activation(out=x2b[:, 0:ncols], in_=ap, func=AF.Square),
                 reads=[tl], writes=[x2b])
            P.mm(pm, ones_bf[:], xb[:, 0:ncols], i == 0, i == nch - 1, reads=[ones_bf, xb], out=pm[:, 0:ncols], last=True)
            P.mm(pq, ones_bf[:], x2b[:, 0:ncols], i == 0, i == nch - 1, reads=[ones_bf, x2b], out=pq[:, 0:ncols], last=True)
        mean = st[tag + "_mean"]
        rstd = st[tag + "_rstd"]
        P.op("act", lambda e: e.activation(out=mean[:, 0:ncols], in_=pm[:, 0:ncols], func=AF.Identity, scale=inv_n),
             reads=[pm], writes=[mean])
        P.op("dve", lambda e: e.tensor_tensor(rstd[:, 0:ncols], mean[:, 0:ncols], mean[:, 0:ncols], ALU.mult),
             reads=[mean], writes=[rstd])
        P.op("dve", lambda e: e.scalar_tensor_tensor(rstd[:, 0:ncols], pq[:, 0:ncols], inv_n, rstd[:, 0:ncols],
                                                     ALU.mult, ALU.subtract),
             reads=[pq, rstd], writes=[rstd])
        P.op("dve", lambda e: e.tensor_scalar_add(rstd[:, 0:ncols], rstd[:, 0:ncols], EPS), reads=[], writes=[rstd])
        P.op("act", lambda e: e.sqrt(rstd[:, 0:ncols], rstd[:, 0:ncols]), reads=[], writes=[rstd])
        P.op("dve", lambda e: e.reciprocal(rstd[:, 0:ncols], rstd[:, 0:ncols]), reads=[], writes=[rstd])
        return mean, rstd

    st = {}

    def alloc_ln(tag, nch):
        st[tag + "_xb"] = [P.sb(tag + "_xb", [128, 512], BF16) for _ in range(2)]
        st[tag + "_x2b"] = [P.sb(tag + "_x2b", [128, 512], BF16) for _ in range(2)]
        st[tag + "_mean"] = P.sb(tag + "_mean", [128, 512], F32)
        st[tag + "_rstd"] = P.sb(tag + "_rstd", [128, 512], F32)


    if L1:
        mk = P.mark()
        wk = P.load_w_bf16("wk", ev_w_in[:, 512:1024], 512)
        wkp = P.load_w_bf16("wkp", w_qk_perm[:, 512:1024], 512)
        wv = P.load_w_bf16("wv", ev_w_in[:, 1024:1536], 512)
        xts = [P.sb("xtA%d" % i, [128, 8, 512], F32) for i in range(2)]
        ubs = [P.sb("ubA%d" % i, [128, 8, 512], BF16) for i in range(2)]
        css = [P.sb("csA%d" % i, [128, 2, 512], F32) for i in range(2)]
        t1s = [P.sb("t1A%d" % i, [128, 512], F32) for i in range(2)]
        t2s = [P.sb("t2A%d" % i, [128, 512], F32) for i in range(2)]
        krs = [P.sb("krA%d" % i, [128, 512], BF16) for i in range(3)]
        vbs = [P.sb("vbA%d" % i, [128, 512], BF16) for i in range(3)]
        for it in range(SEQ // 512 + 1):
            is_ctx = it == SEQ // 512
            ncols = CTX if is_ctx else 512
            c0 = it * 512
            xt = xts[it % 2]
            ub = ubs[it % 2]
            cs = css[it % 2]
            srcx = (ctxT if is_ctx else xT_full[:, c0:c0 + 512]).rearrange("(k p) t -> p k t", p=128)
            for kk in range(8):
                P.dma("sp", xt[:, kk, 0:ncols], srcx[:, kk, :], writes=[xt])
            if not is_ctx:
                P.dma("sp", cs[:], cs_full[:, :, c0:c0 + 512], writes=[cs])
            modulate_tile(xt, ub, 0, 0, 1, 1 if is_ctx else 0, ncols)
            for hc in range(4):
                pa = psb[P.alt("ps", 8)]
                P.proj_fm(pa, wk, hc * 128, ub, [wk, ub], ncols)
                kr = krs[P.alt("krA", 3)]
                if is_ctx:
                    P.op("act", lambda e: e.activation(out=kr[:, 0:ncols], in_=pa[:, 0:ncols], func=AF.Copy),
                         reads=[pa], writes=[kr])
                else:
                    pb = psb[P.alt("ps", 8)]
                    P.proj_fm(pb, wkp, hc * 128, ub, [wkp, ub], ncols)
                    t1 = t1s[hc % 2]
                    t2 = t2s[hc % 2]
                    P.op("dve", lambda e: e.tensor_tensor(t1[:], pa[:], cs[:, 0, :], ALU.mult),
                         reads=[pa, cs], writes=[t1])
                    P.op("dve", lambda e: e.tensor_tensor(t2[:], pb[:], cs[:, 1, :], ALU.mult),
                         reads=[pb, cs], writes=[t2])
                    P.op("pool", lambda e: e.tensor_tensor(kr[:], t1[:], t2[:], ALU.add),
                         reads=[t1, t2], writes=[kr])
                P.dma("sp", kT_d[hc, :, c0:c0 + ncols], kr[:, 0:ncols], reads=[kr], writes=[kT_db])
            for s in range(ncols // 128):
                pv = psb[P.alt("ps", 8)]
                for kk in range(8):
                    P.mm(pv, ub[:, kk, s * 128:(s + 1) * 128], wv[:, kk, :], kk == 0, kk == 7, reads=[ub, wv])
                vb = vbs[P.alt("vbA", 3)]
                P.op("act", lambda e: e.activation(out=vb[:], in_=pv[:], func=AF.Copy), reads=[pv], writes=[vb])
                P.dma("sp", v_d[c0 + s * 128:c0 + (s + 1) * 128, :], vb[:], reads=[vb], writes=[v_db])
        P.release(mk)

        if dbg == "A":
            P.barrier(engines=["sp"])
            P.close()
            return P
        mk = P.mark()
        wq = P.load_w_bf16("wq", ev_w_in[:, 0:512], 512)
        wqp = P.load_w_bf16("wqp", w_qk_perm[:, 0:512], 512)
        wval = P.load_w_bf16("wval", ev_w_in[:, 1536:2048], 512)
        wgat = P.load_w_bf16("wgat", ev_w_in[:, 2048:2560], 512)
        aT = P.sb("aT", [128, 4, TOK + 30], F32)
        hm = P.sb("hm", [128, 30], F32)
        P.dma("sp", hm[:], halo_mask, writes=[hm])
        cw = P.sb("cw", [128, 4, 31], F32)
        P.dma("sp", cw[:], conv_wT, writes=[cw])
        cv = P.sb("cv", [128, 3, 4], F32)
        P.dma("sp", cv[:], conv_vecs, writes=[cv])
        xts = [P.sb("xtB%d" % i, [128, 8, 512], F32) for i in range(2)]
        ubs = [P.sb("ubB%d" % i, [128, 8, 512], BF16) for i in range(2)]
        css = [P.sb("csB%d" % i, [128, 2, 512], F32) for i in range(2)]
        t1s = [P.sb("t1B%d" % i, [128, 512], F32) for i in range(2)]
        t2s = [P.sb("t2B%d" % i, [128, 512], F32) for i in range(2)]
        qrs = [P.sb("qrB%d" % i, [128, 512], BF16) for i in range(3)]
        sgs = [P.sb("sgB%d" % i, [128, 512], F32) for i in range(2)]
        for it in range(NT + 1):
            is_halo = it == NT
            ncols = 30 if is_halo else 512
            c0 = it * 512
            xt = xts[it % 2]
            ub = ubs[it % 2]
            cs = css[it % 2]
            srcx = xT_own[:, c0:c0 + ncols].rearrange("(k p) t -> p k t", p=128)
            for kk in range(8):
                P.dma("sp", xt[:, kk, 0:ncols], srcx[:, kk, :], writes=[xt])
            modulate_tile(xt, ub, 0, 0, 1, 0, ncols)
            if not is_halo:
                P.dma("sp", cs[:], cs_own[:, :, c0:c0 + 512], writes=[cs])
                for hc in range(4):
                    pa = psb[P.alt("ps", 8)]
                    P.proj_fm(pa, wq, hc * 128, ub, [wq, ub])
                    pb = psb[P.alt("ps", 8)]
                    P.proj_fm(pb, wqp, hc * 128, ub, [wqp, ub])
                    t1 = t1s[hc % 2]
                    t2 = t2s[hc % 2]
                    qr = qrs[P.alt("qrB", 3)]
                    P.op("dve", lambda e: e.tensor_tensor(t1[:], pa[:], cs[:, 0, :], ALU.mult),
                         reads=[pa, cs], writes=[t1])
                    P.op("dve", lambda e: e.tensor_tensor(t2[:], pb[:], cs[:, 1, :], ALU.mult),
                         reads=[pb, cs], writes=[t2])
                    P.op("pool", lambda e: e.tensor_tensor(qr[:], t1[:], t2[:], ALU.add),
                         reads=[t1, t2], writes=[qr])
                    P.dma("sp", qT_d[hc, :, c0:c0 + 512], qr[:], reads=[qr], writes=[qT_db])
            for ch in range(4):
                pvv = psb[P.alt("ps", 8)]
                P.proj_fm(pvv, wval, ch * 128, ub, [wval, ub], ncols)
                pg = psb[P.alt("ps", 8)]
                P.proj_fm(pg, wgat, ch * 128, ub, [wgat, ub], ncols)
                sg = sgs[ch % 2]
                P.op("act", lambda e: e.activation(out=sg[:, 0:ncols], in_=pg[:, 0:ncols], func=AF.Sigmoid),
                     reads=[pg], writes=[sg])
                if not is_halo:
                    P.op("dve", lambda e: e.tensor_tensor(aT[:, ch, 15 + c0:15 + c0 + 512], pvv[:], sg[:], ALU.mult),
                         reads=[pvv, sg], writes=[aT])
                else:
                    P.op("dve", lambda e: e.tensor_tensor(sg[:, 0:30], pvv[:, 0:30], sg[:, 0:30], ALU.mult),
                         reads=[pvv, sg], writes=[sg])
                    P.op("dve", lambda e: e.tensor_tensor(aT[:, ch, 0:15], sg[:, 0:15], hm[:, 0:15], ALU.mult),
                         reads=[sg, hm], writes=[aT])
                    P.op("dve", lambda e: e.tensor_tensor(aT[:, ch, 15 + TOK:30 + TOK], sg[:, 15:30], hm[:, 15:30],
                                                          ALU.mult),
                         reads=[sg, hm], writes=[aT])
        if dbg == "B1":
            P.barrier(engines=["sp"])
            P.close()
            return P
        alloc_ln("cln", 4)
        accs = [P.sb("cacc%d" % i, [128, 512], F32) for i in range(4)]
        cvo = [P.sb("cvo%d" % i, [128, 4, 512], BF16) for i in range(2)]
        xcs = [P.sb("cxc%d" % i, [128, 512], F32) for i in range(2)]
        for it in range(NT):
            c0 = it * 512
            for ch in range(4):
                eng = "dve"
                acc = accs[ch]
                P.op(eng, lambda e: e.tensor_scalar(acc[:], aT[:, ch, c0:c0 + 512], cw[:, ch, 0:1], cv[:, 0, ch:ch + 1],
                                                    ALU.mult, ALU.add),
                     reads=[aT, cw, cv], writes=[acc])
                for j in range(1, 31):
                    P.op(eng, lambda e: e.scalar_tensor_tensor(acc[:], aT[:, ch, c0 + j:c0 + j + 512], cw[:, ch, j:j + 1],
                                                               acc[:], ALU.mult, ALU.add),
                         reads=[aT, cw], writes=[acc])
            mean, rstd = ln_stats([(accs[ch][:], accs[ch]) for ch in range(4)], 4, 512, 1.0 / 512, "cln")
            co = cvo[it % 2]
            for ch in range(4):
                xc = xcs[ch % 2]
                P.op("dve", lambda e: e.tensor_tensor(xc[:], accs[ch][:], mean[:], ALU.subtract),
                     reads=[accs[ch], mean], writes=[xc])
                P.op("pool", lambda e: e.tensor_tensor(xc[:], xc[:], rstd[:], ALU.mult),
                     reads=[rstd], writes=[xc])
                P.op("dve", lambda e: e.tensor_scalar(xc[:], xc[:], cv[:, 1, ch:ch + 1], cv[:, 2, ch:ch + 1],
                                                      ALU.mult, ALU.add),
                     reads=[cv], writes=[xc])
                P.op("act", lambda e: e.activation(out=co[:, ch, :], in_=xc[:], func=AF.Silu),
                     reads=[xc], writes=[co])
            dst = catT_d[0:512, c0:c0 + 512].rearrange("(k p) t -> p k t", p=128)
            P.dma("sp", dst, co[:], reads=[co], writes=[catT_db])
        P.release(mk)

        if dbg == "B":
            P.barrier(engines=["sp"])
            P.close()
            return P
        mk = P.mark()
        lv = P.sb("lv", [1, 4, 64], F32)
        P.dma("sp", lv[:], lam_vecs, writes=[lv])
        lp = P.sb("lp", [1, 2, 64], F32)
        ls = P.sb("ls", [1, 4], F32)
        P.op("dve", lambda e: e.tensor_tensor(lp[:, 0, :], lv[:, 0, :], lv[:, 1, :], ALU.mult), reads=[lv], writes=[lp])
        P.op("dve", lambda e: e.tensor_tensor(lp[:, 1, :], lv[:, 2, :], lv[:, 3, :], ALU.mult), reads=[lv], writes=[lp])
        P.op("dve", lambda e: e.tensor_reduce(ls[:, 0:2], lp[:], AX.X, ALU.add), reads=[lp], writes=[ls])
        P.op("act", lambda e: e.activation(out=ls[:, 2:4], in_=ls[:, 0:2], func=AF.Exp), reads=[ls], writes=[ls])
        P.op("dve", lambda e: e.tensor_tensor(ls[:, 0:1], ls[:, 3:4], ls[:, 2:3], ALU.subtract), reads=[ls], writes=[ls])
        P.op("dve", lambda e: e.tensor_scalar_add(ls[:, 0:1], ls[:, 0:1], -LAM_INIT0), reads=[ls], writes=[ls])
        ones_f = P.sb("ones_f", [1, 128], F32)
        P.op("dve", lambda e: e.memset(ones_f[:], 1.0), writes=[ones_f])
        neglam = P.sb("neglam", [128, 1], F32)
        pl = psb[P.alt("ps", 8)]
        P.mm(pl, ones_f[:], ls[:, 0:1], True, True, reads=[ones_f, ls], out=pl[:, 0:1])
        P.op("dve", lambda e: e.tensor_copy(neglam[:], pl[:, 0:1]), reads=[pl], writes=[neglam])
        dg = P.sb("dg", [128, 1], F32)
        P.dma("sp", dg[:], diff_g, writes=[dg])
        P.op("dve", lambda e: e.tensor_scalar_mul(dg[:], dg[:], 1.0 - LAM_INIT0), reads=[dg], writes=[dg])

        kTh = P.sb("kTh", [128, NKEY], BF16)
        vh = P.sb("vh", [128, NKT, 128], BF16)
        qh = P.sb("qh", [128, TOK], BF16)
        pts = [P.sb("pt%d" % i, [128, 512], BF16) for i in range(6)]
        accD = P.sb("accD", [128, 512], F32)
        accP = P.sb("accP", [128, 512], F32)
        ones_f32m = P.sb("ones_f32m", [128, 128], F32)
        P.op("dve", lambda e: e.memset(ones_f32m[:], 1.0), writes=[ones_f32m])
        ocs = [P.sb("oc%d" % i, [128, 512], F32) for i in range(2)]
        rl = P.sb("rl", [128, 512], F32)
        osq = P.sb("osq", [128, 512], BF16)
        att_o = [P.sb("atto%d" % i, [128, 512], BF16) for i in range(2)]
        ps_s = psb[0:3]
        ps_o = psb[3:5]
        ps_l = psb[5:7]
        ps_r = psb[7]
        for h in range(4):
            for piece in range(5):
                a = piece * 3328
                P.dma("sp", kTh[:, a:a + 3328], kT_d[h, :, a:a + 3328], reads=[kT_db], writes=[kTh])
            vsrc = v_d[:, h * 128:(h + 1) * 128].rearrange("(kt p) d -> p kt d", p=128)
            for piece in range(10):
                P.dma("sp", vh[:, piece * 13:(piece + 1) * 13, :], vsrc[:, piece * 13:(piece + 1) * 13, :],
                      reads=[v_db], writes=[vh])
            P.dma("sp", qh[:], qT_d[h], reads=[qT_db], writes=[qh])
            for qt in range(NT):
                q0 = qt * 512
                for c in range(2):
                    po = ps_o[c]
                    pL = ps_l[c]
                    r0 = c * 64
                    def qk_exp(kt):
                        pss = ps_s[P.alt("pss", 3)]
                        P.mm(pss, kTh[r0:r0 + 64, kt * 128:(kt + 1) * 128], qh[r0:r0 + 64, q0:q0 + 512], True, True,
                             reads=[kTh, qh])
                        pt = pts[P.alt("pt", 6)]
                        P.op("act", lambda e: e.activation(out=pt[:], in_=pss[:], func=AF.Exp, scale=0.125),
                             reads=[pss], writes=[pt])
                        return pt
                    pt_next = qk_exp(0)
                    for kt in range(NKT):
                        pt = pt_next
                        if kt + 1 < NKT:
                            pt_next = qk_exp(kt + 1)
                        P.mm(po, vh[:, kt, :], pt[:], kt == 0, kt == NKT - 1, reads=[vh, pt], last=True)
                        if kt % 3 == 2:
                            eng, acc, first = "pool", accP, kt == 2
                        else:
                            eng, acc, first = "dve", accD, kt == 0
                        if first:
                            P.op(eng, lambda e: e.tensor_copy(acc[:], pt[:]), reads=[pt], writes=[acc])
                        else:
                            P.op(eng, lambda e: e.tensor_tensor(acc[:], acc[:], pt[:], ALU.add), reads=[pt], writes=[acc])
                    P.mm(pL, ones_f32m[:], accD[:], True, False, reads=[ones_f32m, accD], last=False)
                    P.mm(pL, ones_f32m[:], accP[:], False, True, reads=[ones_f32m, accP], last=True)
                    P.op("dve", lambda e: e.reciprocal(rl[:], pL[:]), reads=[pL], writes=[rl])
                    P.op("dve", lambda e: e.tensor_tensor(ocs[c][:], po[:], rl[:], ALU.mult),
                         reads=[po, rl], writes=[ocs[c]])
                o = ocs[0]
                P.op("dve", lambda e: e.scalar_tensor_tensor(o[:], ocs[1][:], neglam[:, 0:1], o[:], ALU.mult, ALU.add),
                     reads=[ocs[1], neglam], writes=[o])
                P.op("act", lambda e: e.activation(out=osq[:], in_=o[:], func=AF.Square), reads=[o], writes=[osq])
                P.mm(ps_r, ones_bf[:], osq[:], True, True, reads=[ones_bf, osq])
                P.op("dve", lambda e: e.tensor_scalar(rl[:], ps_r[:], 1.0 / 128, EPS, ALU.mult, ALU.add),
                     reads=[ps_r], writes=[rl])
                P.op("act", lambda e: e.sqrt(rl[:], rl[:]), reads=[], writes=[rl])
                P.op("dve", lambda e: e.reciprocal(rl[:], rl[:]), reads=[], writes=[rl])
                P.op("dve", lambda e: e.tensor_tensor(o[:], o[:], rl[:], ALU.mult), reads=[rl], writes=[o])
                ao = att_o[P.alt("atto", 2)]
                P.op("act", lambda e: e.activation(out=ao[:], in_=o[:], func=AF.Identity, scale=dg[:, 0:1]),
                     reads=[o, dg], writes=[ao])
                P.dma("sp", catT_d[512 + h * 128:512 + (h + 1) * 128, q0:q0 + 512], ao[:], reads=[ao], writes=[catT_db])
        P.release(mk)

    def post_mixer(l, w_out_ap, catA, catAb, catB, catBb, res_src, res_srcb):
        mk = P.mark()
        wo = P.load_w_bf16("wo", w_out_ap, D)
        rw = P.sb("rw", [128, 8, NE], F32)
        P.dma("sp", rw[:], router_w[l].rearrange("(k p) e -> p k e", p=128), writes=[rw])
        rb = P.sb("rb", [128, NE], F32)
        P.dma("sp", rb[:], router_b[l].partition_broadcast(128), writes=[rb])
        alloc_ln("ln1", 8)
        cats = [P.sb("cat%d" % i, [128, 8, 512], BF16) for i in range(2)]
        xts = [P.sb("xtD%d" % i, [128, 8, 512], F32) for i in range(2)]
        rts = [P.sb("rtD%d" % i, [128, 8, 512], F32) for i in range(2)]
        u2f = P.sb("u2f", [128, 8, 512], F32)
        u2b = [P.sb("u2b%d" % i, [128, 8, 512], BF16) for i in range(2)]
        lg = P.sb("lg", [128, NE], F32)
        mx = P.sb("mx", [128, 8], F32)
        msk = P.sb("msk", [128, NE], F32)
        ex = P.sb("ex", [128, NE], F32)
        ssum = P.sb("ssum", [128, 1], F32)
        gts = [P.sb("gts%d" % i, [128, NE], F32) for i in range(2)]
        gTt = P.sb("gTt", [NE, 512], F32)
        for it in range(NT):
            c0 = it * 512
            cat = cats[it % 2]
            xt = xts[it % 2]
            rt = rts[it % 2]
            srcA = catA[:, c0:c0 + 512].rearrange("(k p) t -> p k t", p=128)
            srcB = catB[:, c0:c0 + 512].rearrange("(k p) t -> p k t", p=128)
            srcx = res_src[:, c0:c0 + 512].rearrange("(k p) t -> p k t", p=128)
            for kk in range(8):
                if kk < 4:
                    P.dma("sp", cat[:, kk, :], srcA[:, kk, :], reads=[catAb], writes=[cat])
                else:
                    P.dma("sp", cat[:, kk, :], srcB[:, kk - 4, :], reads=[catBb], writes=[cat])
                P.dma("sp", xt[:, kk, :], srcx[:, kk, :], reads=[res_srcb], writes=[xt])
            for m in range(8):
                py = psb[P.alt("ps", 8)]
                P.proj_fm(py, wo, m * 128, cat, [wo, cat])
                P.op("act", lambda e: e.activation(out=xt[:, m, :], in_=xt[:, m, :], func=AF.Identity, scale=ALPHA),
                     reads=[], writes=[xt])
                P.op("dve", lambda e: e.scalar_tensor_tensor(rt[:, m, :], py[:], MOD(l, 2, m), xt[:, m, :],
                                                             ALU.mult, ALU.add),
                     reads=[py, modT, xt], writes=[rt])
            mean, rstd = ln_stats([(rt[:, m, :], rt) for m in range(8)], 8, 512, 1.0 / D, "ln1")
            ub = u2b[it % 2]
            for m in range(8):
                P.op("dve", lambda e: e.tensor_tensor(rt[:, m, :], rt[:, m, :], mean[:], ALU.subtract),
                     reads=[mean], writes=[rt])
                P.op("pool", lambda e: e.tensor_tensor(rt[:, m, :], rt[:, m, :], rstd[:], ALU.mult),
                     reads=[rstd], writes=[rt])
                P.op("dve", lambda e: e.tensor_scalar(rt[:, m, :], rt[:, m, :], lnv[:, l, 0, m:m + 1],
                                                      lnv[:, l, 1, m:m + 1], ALU.mult, ALU.add),
                     reads=[lnv], writes=[rt])
                P.op("act", lambda e: e.activation(out=u2f[:, m, :], in_=rt[:, m, :], func=AF.Identity,
                                                   bias=MOD(l, 3, m), scale=MOD(l, 4, m, 0, True)),
                     reads=[rt, modT, mod1p], writes=[u2f])
                P.op("pool", lambda e: e.tensor_copy(ub[:, m, :], u2f[:, m, :]), reads=[u2f], writes=[ub])
            dsth = h1T_d[:, c0:c0 + 512].rearrange("(k p) t -> p k t", p=128)
            dstu = u2T_d[:, c0:c0 + 512].rearrange("(k p) t -> p k t", p=128)
            for kk in range(8):
                P.dma("sp", dsth[:, kk, :], rt[:, kk, :], reads=[rt], writes=[h1T_db])
                P.dma("sp", dstu[:, kk, :], ub[:, kk, :], reads=[ub], writes=[u2T_db])
            for s in range(4):
                pr = psb[P.alt("ps", 8)]
                for kk in range(8):
                    P.mm(pr, u2f[:, kk, s * 128:(s + 1) * 128], rw[:, kk, :], kk == 0, kk == 7,
                         reads=[u2f, rw], out=pr[:, 0:NE])
                P.op("dve", lambda e: e.tensor_tensor(lg[:], pr[:, 0:NE], rb[:], ALU.add), reads=[pr, rb], writes=[lg])
                P.op("dve", lambda e: e.max(out=mx[:], in_=lg[:]), reads=[lg], writes=[mx])
                P.op("dve", lambda e: e.tensor_scalar(msk[:], lg[:], mx[:, 3:4], None, ALU.is_ge),
                     reads=[lg, mx], writes=[msk])
                P.op("dve", lambda e: e.tensor_scalar(ex[:], lg[:], mx[:, 0:1], None, ALU.subtract),
                     reads=[lg, mx], writes=[ex])
                P.op("act", lambda e: e.activation(out=ex[:], in_=ex[:], func=AF.Exp), reads=[], writes=[ex])
                P.op("dve", lambda e: e.tensor_tensor(ex[:], ex[:], msk[:], ALU.mult), reads=[msk], writes=[ex])
                P.op("dve", lambda e: e.tensor_reduce(ssum[:], ex[:], AX.X, ALU.add), reads=[ex], writes=[ssum])
                P.op("dve", lambda e: e.reciprocal(ssum[:], ssum[:]), reads=[], writes=[ssum])
                gt = gts[s % 2]
                P.op("dve", lambda e: e.tensor_scalar(gt[:], ex[:], ssum[:, 0:1], None, ALU.mult),
                     reads=[ex, ssum], writes=[gt])
                pg = psb[P.alt("ps", 8)]
                P.mm(pg, gt[:], ident_sb[:], True, True, reads=[gt, ident_sb], out=pg[0:NE, 0:128])
                P.op("act", lambda e: e.activation(out=gTt[:, s * 128:(s + 1) * 128], in_=pg[0:NE, 0:128], func=AF.Copy),
                     reads=[pg], writes=[gTt])
            P.dma("sp", gT_d[:, c0:c0 + 512], gTt[:], reads=[gTt], writes=[gT_db])
        P.release(mk)

    def sum_ln2(l, out_dst, out_dstb):
        mk = P.mark()
        alloc_ln("ln2", 8)
        hts = [P.sb("htS%d" % i, [128, 8, 512], F32) for i in range(2)]
        pbs = [P.sb("pbS%d" % i, [128, 8, 512], F32) for i in range(2)]
        acc = P.sb("accS", [128, 8, 512], F32)
        for it in range(NT):
            c0 = it * 512
            ht = hts[it % 2]
            srch = h1T_d[:, c0:c0 + 512].rearrange("(k p) t -> p k t", p=128)
            for kk in range(8):
                P.dma("sp", ht[:, kk, :], srch[:, kk, :], reads=[h1T_db], writes=[ht])
            for pr in range(2):
                for i in range(2):
                    srcp = y2p4[pr * 2 + i, :, c0:c0 + 512].rearrange("(k p) t -> p k t", p=128)
                    for kk in range(8):
                        P.dma("sp", pbs[i][:, kk, :], srcp[:, kk, :], reads=[y2p4b], writes=[pbs[i]])
                if pr == 0:
                    P.op("dve", lambda e: e.tensor_tensor(acc[:], pbs[0][:], pbs[1][:], ALU.add),
                         reads=[pbs[0], pbs[1]], writes=[acc])
                else:
                    P.op("pool", lambda e: e.tensor_tensor(acc[:], acc[:], pbs[0][:], ALU.add),
                         reads=[pbs[0]], writes=[acc])
                    P.op("dve", lambda e: e.tensor_tensor(acc[:], acc[:], pbs[1][:], ALU.add),
                         reads=[pbs[1]], writes=[acc])
            P.op("act", lambda e: e.activation(out=ht[:], in_=ht[:], func=AF.Identity, scale=ALPHA),
                 reads=[], writes=[ht])
            for m in range(8):
                P.op("dve", lambda e: e.scalar_tensor_tensor(ht[:, m, :], acc[:, m, :], MOD(l, 5, m), ht[:, m, :],
                                                             ALU.mult, ALU.add),
                     reads=[acc, modT], writes=[ht])
            mean, rstd = ln_stats([(ht[:, m, :], ht) for m in range(8)], 8, 512, 1.0 / D, "ln2")
            for m in range(8):
                P.op("dve", lambda e: e.tensor_tensor(ht[:, m, :], ht[:, m, :], mean[:], ALU.subtract),
                     reads=[mean], writes=[ht])
                P.op("pool", lambda e: e.tensor_tensor(ht[:, m, :], ht[:, m, :], rstd[:], ALU.mult),
                     reads=[rstd], writes=[ht])
                P.op("dve", lambda e: e.tensor_scalar(ht[:, m, :], ht[:, m, :], lnv[:, l, 2, m:m + 1],
                                                      lnv[:, l, 3, m:m + 1], ALU.mult, ALU.add),
                     reads=[lnv], writes=[ht])
            dsto = out_dst[:, c0:c0 + 512].rearrange("(k p) t -> p k t", p=128)
            for kk in range(8):
                P.dma("sp", dsto[:, kk, :], ht[:, kk, :], reads=[ht], writes=[out_dstb])
        P.release(mk)

    if L1:
        xres = xT_own[:, 0:TOK]
        post_mixer(0, ev_w_out, catT_d[0:512, :], catT_db, catT_d[512:1024, :], catT_db, xres, Buf("xres"))
    if L3:
        sum_ln2(0, h2T_d, h2T_db)
    if L6:
        sum_ln2(1, outT, outT_b)

    if L3:
        mk = P.mark()
        wu = P.load_w_bf16("wu", od_w_in[:, 0:512], 512)
        wv1 = P.load_w_bf16("wv1", od_w_in[:, 512:1024], 512)
        wf = P.load_w_bf16("wf", od_w_in[:, 1024:1536], 512)
        wsT = P.sb("wsT", [128, 4, 128], BF16)
        P.dma("pool", wsT[:], gmlp_wsT, writes=[wsT])
        bsb = P.sb("bsb", [128, 4, 128], F32)
        P.dma("sp", bsb[:], gmlp_bs.partition_broadcast(128), writes=[bsb])
        gln = P.sb("gln", [128, 2, 512], F32)
        P.dma("sp", gln[:], gmlp_ln.partition_broadcast(128), writes=[gln])
        fl = P.sb("fl", [128, 2, 4], F32)
        P.dma("sp", fl[:], four_ln, writes=[fl])
        dcs = P.sb("dcs", [128, 256], BF16)
        P.dma("sp", dcs[:], dft_cs, writes=[dcs])
        alloc_ln("fln", 1)
        xts = [P.sb("xtF%d" % i, [128, 8, 512], F32) for i in range(2)]
        ubs = [P.sb("ubF%d" % i, [128, 8, 512], BF16) for i in range(2)]
        ugs = [P.sb("ugF%d" % i, [128, 4, 512], F32) for i in range(2)]
        vg = P.sb("vgF", [128, 512], F32)
        bst = P.sb("bst", [128, 2, 6], F32)
        bag = P.sb("bag", [128, 2], F32)
        vrs = P.sb("vrs", [128, 1], F32)
        vln = [P.sb("vln%d" % i, [128, 512], BF16) for i in range(2)]
        svt = P.sb("svt", [128, 128], F32)
        spo = [P.sb("spo%d" % i, [128, 4, 512], BF16) for i in range(2)]
        fch = [P.sb("fch%d" % i, [128, 512], F32) for i in range(2)]
        flb = [P.sb("flb%d" % i, [128, 4, 512], BF16) for i in range(2)]
        abo = [P.sb("abo%d" % i, [128, 1024], BF16) for i in range(2)]

        def gelu_tanh(dst, dst_t, src, reads, tmp):
            c = 2.0 * math.sqrt(2.0 / math.pi)
            P.op("act", lambda e: e.activation(out=tmp[:], in_=src, func=AF.Square), reads=reads, writes=[tmp])
            P.op("dve", lambda e: e.tensor_scalar(tmp[:], tmp[:], 0.044715 * c, c, ALU.mult, ALU.add), reads=[], writes=[tmp])
            P.op("dve", lambda e: e.tensor_tensor(tmp[:], tmp[:], src, ALU.mult), reads=reads, writes=[tmp])
            P.op("act", lambda e: e.activation(out=tmp[:], in_=tmp[:], func=AF.Sigmoid), reads=[], writes=[tmp])
            P.op("dve", lambda e: e.tensor_tensor(dst, tmp[:], src, ALU.mult), reads=reads + [tmp], writes=[dst_t])

        gtmp = [P.sb("gtmp%d" % i, [128, 512], F32) for i in range(2)]
        for it in range(NT):
            c0 = it * 512
            xt = xts[it % 2]
            ub = ubs[it % 2]
            srcx = h2T_d[:, c0:c0 + 512].rearrange("(k p) t -> p k t", p=128)
            for kk in range(8):
                P.dma("sp", xt[:, kk, :], srcx[:, kk, :], reads=[h2T_db], writes=[xt])
            modulate_tile(xt, ub, 1, 0, 1, 0, 512)
            ug = ugs[it % 2]
            for ch in range(4):
                pu = psb[P.alt("ps", 8)]
                P.proj_fm(pu, wu, ch * 128, ub, [wu, ub])
                tmp = gtmp[ch % 2]
                gelu_tanh(ug[:, ch, :], ug, pu[:], [pu], tmp)
            sp = spo[it % 2]
            for s in range(4):
                pv = psb[P.alt("ps", 8)]
                for kk in range(8):
                    P.mm(pv, ub[:, kk, s * 128:(s + 1) * 128], wv1[:, kk, :], kk == 0, kk == 7, reads=[ub, wv1])
                tmp = gtmp[s % 2]
                gelu_tanh(vg[:], vg, pv[:], [pv], tmp)
                P.op("dve", lambda e: e.bn_stats(bst[:, 0, :], vg[:, 0:256]), reads=[vg], writes=[bst])
                P.op("dve", lambda e: e.bn_stats(bst[:, 1, :], vg[:, 256:512]), reads=[vg], writes=[bst])
                P.op("dve", lambda e: e.bn_aggr(bag[:], bst[:]), reads=[bst], writes=[bag])
                P.op("dve", lambda e: e.tensor_scalar_add(vrs[:], bag[:, 1:2], EPS), reads=[bag], writes=[vrs])
                P.op("act", lambda e: e.sqrt(vrs[:], vrs[:]), reads=[], writes=[vrs])
                P.op("dve", lambda e: e.reciprocal(vrs[:], vrs[:]), reads=[], writes=[vrs])
                P.op("dve", lambda e: e.tensor_scalar(vg[:], vg[:], bag[:, 0:1], vrs[:, 0:1], ALU.subtract, ALU.mult),
                     reads=[bag, vrs], writes=[vg])
                P.op("pool", lambda e: e.tensor_tensor(vg[:], vg[:], gln[:, 0, :], ALU.mult), reads=[gln], writes=[vg])
                vl = vln[s % 2]
                P.op("pool", lambda e: e.tensor_tensor(vl[:], vg[:], gln[:, 1, :], ALU.add), reads=[vg, gln], writes=[vl])
                for g in range(4):
                    pss = psb[P.alt("ps", 8)]
                    P.mm(pss, vl[:, g * 128:(g + 1) * 128], wsT[:, g, :], True, True, reads=[vl, wsT], out=pss[:, 0:128])
                    P.op("dve", lambda e: e.tensor_tensor(svt[:], pss[:, 0:128], bsb[:, g, :], ALU.add),
                         reads=[pss, bsb], writes=[svt])
                    P.op("dve", lambda e: e.tensor_tensor(sp[:, g, s * 128:(s + 1) * 128], svt[:],
                                                          ug[:, g, s * 128:(s + 1) * 128], ALU.mult),
                         reads=[svt, ug], writes=[sp])
            dsts = spT_d[:, c0:c0 + 512].rearrange("(k p) t -> p k t", p=128)
            P.dma("sp", dsts, sp[:], reads=[sp], writes=[spT_db])
            fb = flb[it % 2]
            for g in range(4):
                pf = psb[P.alt("ps", 8)]
                P.proj_fm(pf, wf, g * 128, ub, [wf, ub])
                fc = fch[g % 2]
                P.op("act", lambda e: e.activation(out=fc[:], in_=pf[:], func=AF.Copy), reads=[pf], writes=[fc])
                mean, rstd = ln_stats([(fc[:], fc)], 1, 512, 1.0 / 128, "fln")
                P.op("dve", lambda e: e.tensor_tensor(fc[:], fc[:], mean[:], ALU.subtract), reads=[mean], writes=[fc])
                P.op("pool", lambda e: e.tensor_tensor(fc[:], fc[:], rstd[:], ALU.mult), reads=[rstd], writes=[fc])
                P.op("dve", lambda e: e.tensor_scalar(fb[:, g, :], fc[:], fl[:, 0, g:g + 1], fl[:, 1, g:g + 1],
                                                      ALU.mult, ALU.add),
                     reads=[fc, fl], writes=[fb])
            for s in range(4):
                ab = abo[s % 2]
                for g in range(4):
                    pab = psb[P.alt("ps", 8)]
                    P.mm(pab, fb[:, g, s * 128:(s + 1) * 128], dcs[:], True, True, reads=[fb, dcs], out=pab[:, 0:256])
                    P.op("act", lambda e: e.activation(out=ab[:, g * 256:(g + 1) * 256], in_=pab[:, 0:256], func=AF.Copy),
                         reads=[pab], writes=[ab])
                P.dma("sp", ab_own[c0 + s * 128:c0 + (s + 1) * 128, :], ab[:], reads=[ab], writes=[ab_ownb])
        P.release(mk)

    if L4:
        mk = P.mark()
        c128s = P.sb("c128s", [128, 256], BF16)
        P.dma("sp", c128s[:], c128, writes=[c128s])
        tws = P.sb("tws", [128, 3, 128], F32)
        P.dma("sp", tws[:], tw_c, writes=[tws])
        ins_ = [P.sb("fin%d" % i, [128, 1024], BF16) for i in range(3)]
        p2s = [P.sb("p2s%d" % i, [128, 512], F32) for i in range(2)]
        yr = [P.sb("yr%d" % i, [128, 2, 128], F32) for i in range(2)]
        ym = [P.sb("ym%d" % i, [128, 2, 128], F32) for i in range(2)]
        t1 = [P.sb("tt1%d" % i, [128, 2, 128], F32) for i in range(2)]
        t2 = [P.sb("tt2%d" % i, [128, 2, 128], F32) for i in range(2)]
        zo = [P.sb("zo%d" % i, [128, 1024], BF16) for i in range(3)]
        abv = ab_full.rearrange("(n1 n2) c -> n1 n2 c", n2=128)
        for n2 in range(128):
            fin = ins_[n2 % 3]
            P.dma("sp", fin[:], abv[:, n2, :], reads=[ab_fullb], writes=[fin])
            z = zo[n2 % 3]
            for hf in range(2):
                p1 = psb[P.alt("ps", 8)]
                p2 = psb[P.alt("ps", 8)]
                P.mm(p1, c128s[:, 0:128], fin[:, hf * 512:(hf + 1) * 512], True, True, reads=[c128s, fin])
                P.mm(p2, c128s[:, 128:256], fin[:, hf * 512:(hf + 1) * 512], True, True, reads=[c128s, fin])
                s2 = p2s[hf]
                P.op("act", lambda e: e.activation(out=s2[:], in_=p2[:], func=AF.Copy), reads=[p2], writes=[s2])
                p1v = p1[:].rearrange("p (g a c) -> p g a c", g=2, a=2)
                s2v = s2[:].rearrange("p (g a c) -> p g a c", g=2, a=2)
                Yr, Ym, T1, T2 = yr[hf], ym[hf], t1[hf], t2[hf]
                P.op("dve", lambda e: e.tensor_tensor(Yr[:], p1v[:, :, 0, :], s2v[:, :, 1, :], ALU.subtract),
                     reads=[p1, s2], writes=[Yr])
                P.op("dve", lambda e: e.tensor_tensor(Ym[:], p1v[:, :, 1, :], s2v[:, :, 0, :], ALU.add),
                     reads=[p1, s2], writes=[Ym])
                P.op("act", lambda e: e.activation(out=T1[:], in_=Yr[:], func=AF.Identity, scale=tws[:, 0, n2:n2 + 1]),
                     reads=[Yr, tws], writes=[T1])
                P.op("act", lambda e: e.activation(out=T2[:], in_=Ym[:], func=AF.Identity, scale=tws[:, 0, n2:n2 + 1]),
                     reads=[Ym, tws], writes=[T2])
                zr = z[:, hf * 256:(hf + 1) * 256].rearrange("p (g c) -> p g c", g=2)
                zi = z[:, 512 + hf * 256:512 + (hf + 1) * 256].rearrange("p (g c) -> p g c", g=2)
                P.op("dve", lambda e: e.scalar_tensor_tensor(zr, Ym[:], tws[:, 2, n2:n2 + 1], T1[:], ALU.mult, ALU.add),
                     reads=[Ym, T1, tws], writes=[z])
                P.op("dve", lambda e: e.scalar_tensor_tensor(zi, Yr[:], tws[:, 1, n2:n2 + 1], T2[:], ALU.mult, ALU.add),
                     reads=[Yr, T2, tws], writes=[z])
            P.dma("sp", Zd[:, n2, :], z[:], reads=[z], writes=[Zdb])
        P.release(mk)

        mk = P.mark()
        cs32s = P.sb("cs32s", [128, 64], BF16)
        P.dma("sp", cs32s[:], cs32, writes=[cs32s])
        zts = [P.sb("zt%d" % i, [128, 1024], BF16) for i in range(3)]
        fo_sb = P.sb("fo_sb", [128, 4, 32, 128], BF16)
        for k1 in range(128):
            zt = zts[k1 % 3]
            P.dma("sp", zt[:], Zd[k1, :, :], reads=[Zdb], writes=[zt])
            px = psb[P.alt("ps", 8)]
            for g in range(4):
                P.mm(px, zt[:, g * 128:(g + 1) * 128], cs32s[:, 0:32], True, False, reads=[zt, cs32s],
                     out=px[:, g * 32:(g + 1) * 32], last=False)
                P.mm(px, zt[:, 512 + g * 128:512 + (g + 1) * 128], cs32s[:, 32:64], False, True, reads=[zt, cs32s],
                     out=px[:, g * 32:(g + 1) * 32], last=(g == 3))
            P.op("act", lambda e: e.activation(out=fo_sb[:, :, :, k1],
                                               in_=px[:, 0:128].rearrange("p (g k) -> p g k", g=4), func=AF.Copy),
                 reads=[px], writes=[fo_sb])
        dstf = fouT_d.rearrange("(g p) t -> p g t", p=128)
        for g in range(4):
            P.dma("sp", dstf[:, g, :], fo_sb[:, g, :, :].rearrange("p a b -> p (a b)"), reads=[fo_sb], writes=[fouT_db])
        P.release(mk)
        post_mixer(1, od_w_out, spT_d, spT_db, fouT_d, fouT_db, h2T_d, h2T_db)

    P.barrier(engines=["sp"])
    P.close()
    return P


def build_E():
    P = Prog("E")
    nc = P.nc
    NL = 8
    NTK = SEQ
    u2T_all = P.inp("u2T_all", [D, NTK], BF16)
    gT_loc = P.inp("gT_loc", [NL, NTK])
    wgu_d = P.inp("wgu", [NL, D, 2 * D])
    wd_d = P.inp("wd", [NL, D, D])
    bguT = P.inp("bguT", [128, NL, 16])
    bd_d = P.inp("bd", [NL, D])
    y2p = nc.dram_tensor("y2p", [D, NTK], F32, kind="ExternalOutput").ap()
    y2pb = Buf("y2p")
    psb = P.psb
    QT = 1024
    u2q = P.sb("u2q", [128, 8, QT], BF16)
    y2 = P.sb("y2", [128, 8, QT], F32)
    gTqs = [P.sb("gTq%d" % i, [NL, QT], F32) for i in range(2)]
    bd = P.sb("bd", [NL, D], F32)
    P.dma("sp", bd[:], bd_d, writes=[bd])
    bgu = P.sb("bgu", [128, NL, 16], F32)
    P.dma("sp", bgu[:], bguT, writes=[bgu])
    wgus = [P.sb("wgu%d" % i, [128, 8, 2 * D], BF16) for i in range(2)]
    wds = [P.sb("wd%d" % i, [128, 8, D], BF16) for i in range(1)]
    Ge = [P.sb("Ge%d" % i, [128, 512], F32) for i in range(2)]
    xg = [P.sb("xg%d" % i, [128, 512], F32) for i in range(2)]
    sg = [P.sb("sgm%d" % i, [128, 512], F32) for i in range(2)]
    xl = [P.sb("xl%d" % i, [128, 512], F32) for i in range(2)]
    actb = [P.sb("actb%d" % i, [128, 8, 512], BF16) for i in range(2)]
    wgu_bf = nc.dram_tensor("wgu_bf", [NL, D, 2 * D], BF16, kind="Internal").ap()
    wd_bf = nc.dram_tensor("wd_bf", [NL, D, D], BF16, kind="Internal").ap()
    wgu_bfb = [Buf("wgu_bf%d" % i) for i in range(NL)]
    wd_bfb = [Buf("wd_bf%d" % i) for i in range(NL)]
    for ei in range(NL):
        stg = wgus[ei % 2]
        std = wds[0]
        srcg = wgu_d[ei].rearrange("(k p) n -> p k n", p=128)
        srcd = wd_d[ei].rearrange("(k p) n -> p k n", p=128)
        dstg = wgu_bf[ei].rearrange("(k p) n -> p k n", p=128)
        dstd = wd_bf[ei].rearrange("(k p) n -> p k n", p=128)
        for kk in range(8):
            P.dma("pool", stg[:, kk, :], srcg[:, kk, :], writes=[stg])
        for kk in range(8):
            P.dma("sp", dstg[:, kk, :], stg[:, kk, :], reads=[stg], writes=[wgu_bfb[ei]])
        for kk in range(8):
            P.dma("pool", std[:, kk, :], srcd[:, kk, :], writes=[std])
        for kk in range(8):
            P.dma("act", dstd[:, kk, :], std[:, kk, :], reads=[std], writes=[wd_bfb[ei]])
    for q in range(NTK // QT):
        q0 = q * QT
        gTq = gTqs[q % 2]
        srcu = u2T_all[:, q0:q0 + QT].rearrange("(k p) t -> p k t", p=128)
        for kk in range(8):
            P.dma("sp", u2q[:, kk, :], srcu[:, kk, :], writes=[u2q])
        P.dma("sp", gTq[:], gT_loc[:, q0:q0 + QT], writes=[gTq])
        for tt in range(QT // 512):
            for m in range(8):
                pb_ = psb[P.alt("ps", 8)]
                P.mm(pb_, bd[:, m * 128:(m + 1) * 128], gTq[:, tt * 512:(tt + 1) * 512], True, True, reads=[bd, gTq])
                P.op("act", lambda e: e.activation(out=y2[:, m, tt * 512:(tt + 1) * 512], in_=pb_[:], func=AF.Copy),
                     reads=[pb_], writes=[y2])
        for ei in range(NL):
            wgu = wgus[ei % 2]
            wd = wds[0]
            srcg = wgu_bf[ei].rearrange("(k p) n -> p k n", p=128)
            srcd = wd_bf[ei].rearrange("(k p) n -> p k n", p=128)
            for kk in range(8):
                P.dma("act" if kk % 2 else "sp", wgu[:, kk, :], srcg[:, kk, :], reads=[wgu_bfb[ei]], writes=[wgu])
            for kk in range(8):
                P.dma("act" if kk % 2 else "sp", wd[:, kk, :], srcd[:, kk, :], reads=[wd_bfb[ei]], writes=[wd])
            abs_ = []
            for tt in range(QT // 512):
                t0 = tt * 512
                G = Ge[P.alt("Ge", 2)]
                P.dma("sp", G[:], gT_loc[ei:ei + 1, q0 + t0:q0 + t0 + 512].partition_broadcast(128), writes=[G])
                ab = actb[P.alt("actb", 2)]
                for m in range(8):
                    pgl = psb[P.alt("ps", 8)]
                    pll = psb[P.alt("ps", 8)]
                    for kk in range(8):
                        P.mm(pgl, wgu[:, kk, m * 128:(m + 1) * 128], u2q[:, kk, t0:t0 + 512], kk == 0, kk == 7,
                             reads=[wgu, u2q])
                    for kk in range(8):
                        P.mm(pll, wgu[:, kk, D + m * 128:D + (m + 1) * 128], u2q[:, kk, t0:t0 + 512], kk == 0, kk == 7,
                             reads=[wgu, u2q])
                    a1 = xg[m % 2]
                    a2 = sg[m % 2]
                    a3 = xl[m % 2]
                    P.op("dve", lambda e: e.tensor_scalar(a1[:], pgl[:], bgu[:, ei, m:m + 1], 7.0, ALU.add, ALU.min),
                         reads=[pgl, bgu], writes=[a1])
                    P.op("act", lambda e: e.activation(out=a2[:], in_=a1[:], func=AF.Sigmoid, scale=1.702),
                         reads=[a1], writes=[a2])
                    P.op("dve", lambda e: e.tensor_scalar(a3[:], pll[:], bgu[:, ei, 8 + m:9 + m], -7.0, ALU.add, ALU.max),
                         reads=[pll, bgu], writes=[a3])
                    P.op("dve", lambda e: e.tensor_scalar(a3[:], a3[:], 7.0, 1.0, ALU.min, ALU.add),
                         reads=[], writes=[a3])
                    P.op("pool", lambda e: e.tensor_tensor(a1[:], a1[:], a2[:], ALU.mult), reads=[a2], writes=[a1])
                    P.op("pool", lambda e: e.tensor_tensor(a1[:], a1[:], a3[:], ALU.mult), reads=[a3], writes=[a1])
                    P.op("pool", lambda e: e.tensor_tensor(ab[:, m, :], a1[:], G[:], ALU.mult), reads=[a1, G], writes=[ab])
                abs_.append((ab, t0))
            for ab, t0 in abs_:
                for m in range(8):
                    pd = psb[P.alt("ps", 8)]
                    for kk in range(8):
                        P.mm(pd, wd[:, kk, m * 128:(m + 1) * 128], ab[:, kk, :], kk == 0, kk == 7, reads=[wd, ab])
                    P.op("dve", lambda e: e.tensor_tensor(y2[:, m, t0:t0 + 512], y2[:, m, t0:t0 + 512], pd[:], ALU.add),
                         reads=[pd], writes=[y2])
        dsty = y2p[:, q0:q0 + QT].rearrange("(k p) t -> p k t", p=128)
        for kk in range(8):
            P.dma("sp", dsty[:, kk, :], y2[:, kk, :], reads=[y2], writes=[y2pb])
    P.barrier(engines=["sp"])
    P.close()
    return P


def _fm(v, nch):
    return np.ascontiguousarray(np.asarray(v, np.float32).reshape(nch, 128).T)


def _rope_tables():
    t = np.arange(SEQ)
    row = (t // 64).astype(np.float64)
    col = (t % 64).astype(np.float64)
    inv = 10000.0 ** (-np.arange(16, dtype=np.float64) / 16)
    cos = np.zeros((64, SEQ))
    ssin = np.zeros((64, SEQ))
    for half, pos in enumerate((row, col)):
        ang = inv[:, None] * pos[None, :]
        c, s = np.cos(ang), np.sin(ang)
        b = half * 32
        cos[b:b + 16] = c
        cos[b + 16:b + 32] = c
        ssin[b:b + 16] = -s
        ssin[b + 16:b + 32] = s
    cos = np.concatenate([cos, cos], 0)
    ssin = np.concatenate([ssin, ssin], 0)
    return np.stack([cos, ssin], 1).astype(np.float32)


def _perm64():
    p = np.arange(64)
    out = p.copy()
    for b in (0, 32):
        out[b:b + 16] = p[b + 16:b + 32]
        out[b + 16:b + 32] = p[b:b + 16]
    return out


_CACHE = {}


def _get_prog(mode):
    if mode not in _CACHE:
        _CACHE[mode] = build_E() if mode == "E" else build(mode)
    return _CACHE[mode]


def _run(mode, maps):
    p = _get_prog(mode)
    maps = [{k: v for k, v in m.items() if k in p.ins} for m in maps]
    for m in maps:
        missing = [k for k in p.ins if k not in m]
        assert not missing, (mode, missing)
    return run_bass_kernel_spmd(p.nc, maps, core_ids=list(range(len(maps)))).results


def _common(inp):
    f32 = np.float32
    ln_vecs = np.zeros((128, 2, 4, 8), f32)
    for l in range(2):
        for i, nm in enumerate(("ln1_g", "ln1_b", "ln2_g", "ln2_b")):
            ln_vecs[:, l, i, :] = _fm(inp[nm][l], 8)
    return dict(ln_vecs=ln_vecs, router_w=np.asarray(inp["router_w"], f32),
                router_b=np.asarray(inp["router_b"], f32).reshape(2, 1, NE), ident=np.eye(128, dtype=f32))


def _maps_L1(inp):
    f32 = np.float32
    x = np.asarray(inp["x"], f32)
    ctx = np.asarray(inp["ctx"], f32)
    c = np.asarray(inp["c"], f32)
    c_ctx = np.asarray(inp["c_ctx"], f32)
    cs = _rope_tables()
    perm = np.concatenate([_perm64() + 64 * i for i in range(16)])
    w_in = np.asarray(inp["ev_w_in"][0], f32)
    w_qk_perm = np.ascontiguousarray(w_in[:, :1024][:, perm])
    b_modT = np.ascontiguousarray(np.asarray(inp["b_mod"], f32).reshape(2, 48, 128).transpose(2, 0, 1))
    common = _common(inp)
    xT = [np.ascontiguousarray(x[b].T) for b in range(NB)]
    maps = []
    for core in range(8):
        b, j = core // 4, core % 4
        t0 = j * TOK
        own = np.zeros((D, TOK + 30), f32)
        own[:, :TOK] = xT[b][:, t0:t0 + TOK]
        hmask = np.zeros((128, 30), f32)
        if t0 > 0:
            own[:, TOK:TOK + 15] = xT[b][:, t0 - 15:t0]
            hmask[:, 0:15] = 1.0
        if t0 + TOK < SEQ:
            own[:, TOK + 15:TOK + 30] = xT[b][:, t0 + TOK:t0 + TOK + 15]
            hmask[:, 15:30] = 1.0
        cT = np.zeros((128, 8, 2), f32)
        cT[:, :, 0] = _fm(c[b], 8)
        cT[:, :, 1] = _fm(c_ctx, 8)
        m = dict(common)
        m.update(
            xT_full=xT[b], xT_own=own, halo_mask=hmask, ctxT=np.ascontiguousarray(ctx[b].T),
            cT=cT.reshape(128, 16), w_mod=np.asarray(inp["w_mod"], f32), b_modT=b_modT,
            ev_w_in=w_in, w_qk_perm=w_qk_perm, cs_full=cs, cs_own=np.ascontiguousarray(cs[:, :, t0:t0 + TOK]),
            conv_wT=np.ascontiguousarray(np.asarray(inp["conv_w"][0], f32).T.reshape(4, 128, 31).transpose(1, 0, 2)),
            conv_vecs=np.ascontiguousarray(np.stack([_fm(inp["conv_b"][0], 4), _fm(inp["conv_ln_g"][0], 4),
                                                     _fm(inp["conv_ln_b"][0], 4)], 1)),
            lam_vecs=np.stack([inp["lam_q1"][0], inp["lam_k1"][0], inp["lam_q2"][0], inp["lam_k2"][0]], 0
                              ).astype(f32).reshape(1, 4, 64),
            diff_g=np.asarray(inp["diff_norm_g"][0], f32).reshape(128, 1),
            ev_w_out=np.asarray(inp["ev_w_out"][0], f32))
        maps.append(m)
    return maps


def _maps_E(inp, l, prev):
    f32 = np.float32
    maps = []
    for core in range(8):
        b, j = core // 4, core % 4
        e0 = 8 * j
        u2 = np.concatenate([np.asarray(prev[b * 4 + jj]["u2T_d"]) for jj in range(4)], 1)
        g = np.concatenate([np.asarray(prev[b * 4 + jj]["gT_d"], f32)[e0:e0 + 8] for jj in range(4)], 1)
        bgu = np.asarray(inp["b_gate_up"][l, e0:e0 + 8], f32).reshape(8, 16, 128).transpose(2, 0, 1)
        maps.append(dict(u2T_all=np.ascontiguousarray(u2), gT_loc=np.ascontiguousarray(g),
                         wgu=np.ascontiguousarray(inp["w_gate_up"][l, e0:e0 + 8], dtype=f32),
                         wd=np.ascontiguousarray(inp["w_down"][l, e0:e0 + 8], dtype=f32),
                         bguT=np.ascontiguousarray(bgu), bd=np.ascontiguousarray(inp["b_down"][l, e0:e0 + 8], dtype=f32)))
    return maps


def _partials(rE, core):
    b, j = core // 4, core % 4
    return np.ascontiguousarray(np.stack([np.asarray(rE[b * 4 + jj]["y2p"], np.float32)[:, j * TOK:(j + 1) * TOK]
                                          for jj in range(4)], 0))


def _maps_L3(inp, r1, rE):
    f32 = np.float32
    common = _common(inp)
    cidx = np.arange(128)
    ang = 2 * np.pi * np.outer(cidx, cidx) / 128.0
    sc = 1.0 / math.sqrt(SEQ * 128.0)
    dft_cs = np.concatenate([np.cos(ang) * sc, np.sin(ang) * sc], 1).astype(ml_dtypes.bfloat16)
    maps = []
    for core in range(8):
        m = dict(common)
        m.update(
            y2p4=_partials(rE, core), h1T_d=np.asarray(r1[core]["h1T_d"], f32), modT_d=np.asarray(r1[core]["modT_d"], f32),
            od_w_in=np.asarray(inp["od_w_in"][0], f32),
            gmlp_ln=np.stack([inp["gmlp_ln_g"][0], inp["gmlp_ln_b"][0]], 0).astype(f32).reshape(1, 2, 512),
            gmlp_wsT=np.ascontiguousarray(np.asarray(inp["gmlp_ws"][0], f32).transpose(2, 0, 1)),
            gmlp_bs=np.asarray(inp["gmlp_bs"][0], f32).reshape(1, 4, 128),
            four_ln=np.ascontiguousarray(np.stack([_fm(inp["four_ln_g"][0], 4), _fm(inp["four_ln_b"][0], 4)], 1)),
            dft_cs=dft_cs)
        maps.append(m)
    return maps


def _maps_L4(inp, r1, r3):
    f32 = np.float32
    common = _common(inp)
    idx = np.arange(128)
    ang = 2 * np.pi * np.outer(idx, idx) / 128.0
    c128 = np.concatenate([np.cos(ang), np.sin(ang)], 1).astype(ml_dtypes.bfloat16)
    angt = 2 * np.pi * np.outer(idx, idx) / float(SEQ)
    tw = np.ascontiguousarray(np.stack([np.cos(angt), np.sin(angt), -np.sin(angt)], 1).astype(f32))
    maps = []
    for core in range(8):
        b, j = core // 4, core % 4
        k2 = 32 * j + np.arange(32)
        a2 = 2 * np.pi * np.outer(idx, k2) / 128.0
        cs32 = np.concatenate([np.cos(a2), -np.sin(a2)], 1).astype(ml_dtypes.bfloat16)
        ab_full = np.concatenate([np.asarray(r3[b * 4 + jj]["ab_own"]) for jj in range(4)], 0)
        m = dict(common)
        m.update(od_w_out=np.asarray(inp["od_w_out"][0], f32), c128=c128, tw_c=tw, cs32=cs32,
                 ab_full=np.ascontiguousarray(ab_full), spT_d=np.asarray(r3[core]["spT_d"]),
                 h2T_d=np.asarray(r3[core]["h2T_d"], f32), modT_d=np.asarray(r1[core]["modT_d"], f32))
        maps.append(m)
    return maps


def _maps_L6(inp, r1, r4, rE):
    f32 = np.float32
    common = _common(inp)
    maps = []
    for core in range(8):
        m = dict(common)
        m.update(y2p4=_partials(rE, core), h1T_d=np.asarray(r4[core]["h1T_d"], f32),
                 modT_d=np.asarray(r1[core]["modT_d"], f32))
        maps.append(m)
    return maps


def kernel(**inputs):
    r1 = _run("L1", _maps_L1(inputs))
    rE0 = _run("E", _maps_E(inputs, 0, r1))
    r3 = _run("L3", _maps_L3(inputs, r1, rE0))
    del rE0
    r4 = _run("L4", _maps_L4(inputs, r1, r3))
    rE1 = _run("E", _maps_E(inputs, 1, r4))
    r6 = _run("L6", _maps_L6(inputs, r1, r4, rE1))
    out = np.zeros((NB, SEQ, D), np.float32)
    for core in range(8):
        b, j = core // 4, core % 4
        out[b, j * TOK:(j + 1) * TOK, :] = np.asarray(r6[core]["outT"], np.float32).T
    return out
```

```python
import math
import numpy as np
import ml_dtypes
import concourse.bass as bass
import concourse.mybir as mybir
from concourse.bass_utils import run_bass_kernel_spmd

F32 = mybir.dt.float32
BF16 = mybir.dt.bfloat16
AF = mybir.ActivationFunctionType
ALU = mybir.AluOpType
AX = mybir.AxisListType

D = 1024
SEQ = 16384
NB = 2
TOK = 4096
NT = TOK // 512
CTX = 256
NKEY = SEQ + CTX
NKT = NKEY // 128
NE = 32
ALPHA = 4.0 ** 0.25
EPS = 1e-5
LAM_INIT0 = 0.8 - 0.6 * math.exp(-0.3 * 0)


class Buf:
    __slots__ = ("name", "w", "r")

    def __init__(self, name=""):
        self.name = name
        self.w = None
        self.r = []


class T:
    def __init__(self, t, name):
        self.t = t
        self.b = Buf(name)

    def __getitem__(self, idx):
        return self.t[idx]


class KB:
    ENG = ("pe", "act", "dve", "pool", "sp")

    def __init__(self, n_dma_sems=32):
        self.nc = nc = bass.Bass("TRN2", target_bir_lowering=False)
        self.e = dict(pe=nc.tensor, act=nc.scalar, dve=nc.vector, pool=nc.gpsimd, sp=nc.sync)
        self._ctx = []
        self._semctx = []
        self.gen = {}
        self.tot = {}
        self.sem = {}
        self.cnt = {}
        for n in self.ENG:
            self.sem[n] = self._enter(nc.semaphore("s_" + n))
            self.cnt[n] = 0
        self.dsem = [self._enter(nc.semaphore("d%d" % i)) for i in range(n_dma_sems)]
        self.dcnt = [0] * n_dma_sems
        self.dnext = 0
        self.semobj = {}
        for i, s in enumerate(self.dsem):
            self.semobj[("d", i)] = s
        self.waited = {n: {} for n in self.ENG}
        self.n_inst = 0

    SEM_ROT = 4000

    def _enter(self, cm):
        v = cm.__enter__()
        self._ctx.append(cm)
        return v

    def _sem_new(self, name):
        cm = self.nc.semaphore(name)
        v = cm.__enter__()
        self._semctx.append(cm)
        return v

    def mark(self):
        return len(self._ctx)

    def release(self, mark):
        self.barrier()
        while len(self._ctx) > mark:
            self._ctx.pop().__exit__(None, None, None)

    def close(self):
        while self._ctx:
            self._ctx.pop().__exit__(None, None, None)
        while self._semctx:
            self._semctx.pop().__exit__(None, None, None)

    def sb(self, name, shape, dtype):
        self._uid = getattr(self, "_uid", 0) + 1
        name = "%s_%d" % (name, self._uid)
        return T(self._enter(self.nc.sbuf_tensor(name, list(shape), dtype)), name)

    def ps(self, name, shape, dtype=F32):
        return T(self._enter(self.nc.psum_tensor(name, list(shape), dtype)), name)

    def _wait(self, eng, ev):
        if ev is None:
            return
        key, val = ev
        if key[0] == "e" and key[1] == "pe" and eng == "pe":
            return
        w = self.waited[eng]
        if w.get(key, 0) >= val:
            return
        w[key] = val
        self.e[eng].wait_ge(self.semobj[key], val)

    @staticmethod
    def _b(x):
        return x.b if isinstance(x, T) else x

    def _deps(self, eng, reads, writes):
        for b in reads:
            self._wait(eng, self._b(b).w)
        for b in writes:
            b = self._b(b)
            self._wait(eng, b.w)
            for ev in b.r:
                self._wait(eng, ev)

    def _commit(self, ev, reads, writes):
        for b in reads:
            b = self._b(b)
            b.r.append(ev)
            if len(b.r) > 16:
                d = {}
                for k, v in b.r:
                    if d.get(k, 0) < v:
                        d[k] = v
                b.r = list(d.items())
        for b in writes:
            b = self._b(b)
            b.w = ev
            b.r = []

    def op(self, eng, fn, reads=(), writes=(), inc=True):
        self._deps(eng, reads, writes)
        ins = fn(self.e[eng])
        self.n_inst += 1
        key = ("e", eng, self.gen.get(eng, 0))
        if key not in self.semobj:
            self.semobj[key] = self.sem[eng]
        ev = (key, self.cnt[eng] + 1)
        if inc:
            ins.then_inc(self.sem[eng], 1)
            self.cnt[eng] += 1
        self._commit(ev, reads, writes)
        if inc and self.cnt[eng] >= self.SEM_ROT:
            self.gen[eng] = self.gen.get(eng, 0) + 1
            self.sem[eng] = self._sem_new("s_%s_%d" % (eng, self.gen[eng]))
            self.cnt[eng] = 0
            self.tot[eng] = self.tot.get(eng, 0) + self.SEM_ROT
        return ins

    def dma(self, q, out, in_, reads=(), writes=(), **kw):
        j = self.dnext
        self.dnext = (self.dnext + 1) % len(self.dsem)
        self._deps(q, reads, writes)
        if self.dcnt[j]:
            self._wait(q, (("d", j), self.dcnt[j]))
        ins = self.e[q].dma_start(out=out, in_=in_, **kw)
        self.n_inst += 1
        self.dcnt[j] += 16
        ins.then_inc(self.dsem[j], 16)
        ev = (("d", j), self.dcnt[j])
        self._commit(ev, reads, writes)
        return ev

    def barrier(self, engines=None):
        for E in (engines or self.ENG):
            for F in self.ENG:
                if F != E and self.cnt[F] > 0:
                    self._wait(E, (("e", F, self.gen.get(F, 0)), self.cnt[F]))
            for j in range(len(self.dsem)):
                if self.dcnt[j]:
                    self._wait(E, (("d", j), self.dcnt[j]))


class Prog(KB):
    def __init__(self, mode, dbg=None):
        super().__init__()
        self.dbg = dbg
        self.mode = mode
        self.ins = {}
        self.psb = [self.ps("psb%d" % i, [128, 512], F32) for i in range(8)]
        self.rr = {}

    def inp(self, name, shape, dtype=F32):
        if False and name in ("w_gate_up", "w_down", "b_guT", "b_down", "od_w_in",
                                                            "gmlp_ln", "gmlp_wsT", "gmlp_bs", "four_ln", "dft_cs"):
            return None
        t = self.nc.dram_tensor(name, list(shape), dtype, kind="ExternalInput")
        self.ins[name] = (tuple(shape), dtype)
        return t.ap()

    def scratch(self, name, shape, dtype, boundary=None):
        kind = "Internal"
        if boundary is not None and self.mode != "ALL":
            kind = "ExternalOutput" if self.mode == "L1" else "ExternalInput"
            if kind == "ExternalInput":
                self.ins[name] = (tuple(shape), dtype)
        t = self.nc.dram_tensor(name, list(shape), dtype, kind=kind)
        return t.ap(), Buf(name)

    def alt(self, key, n):
        v = self.rr.get(key, 0)
        self.rr[key] = v + 1
        return v % n

    def mm(self, ps, lhsT, rhs, start, stop, reads, last=None, out=None):
        if last is None:
            last = stop
        o = ps[:] if out is None else out
        return self.op("pe", lambda e: e.matmul(o, lhsT, rhs, start=start, stop=stop),
                       reads=reads, writes=[ps], inc=last)

    def proj_fm(self, ps, w, col0, ub, ubuf_reads, ncols=512, nk=8):
        for k in range(nk):
            self.mm(ps, w[:, k, col0:col0 + 128], ub[:, k, 0:ncols], k == 0, k == nk - 1,
                    reads=ubuf_reads, out=ps[:, 0:ncols])

    def load_w_bf16(self, name, dram_ap, ncols, nk=8):
        t = self.sb(name, [128, nk, ncols], BF16)
        src = dram_ap.rearrange("(k p) n -> p k n", p=128)
        for k in range(nk):
            self.dma("pool", t[:, k, :], src[:, k, :], writes=[t])
        return t


def _f(v):
    return float(v)


def build(mode, dbg=None):
    P = Prog(mode, dbg)
    nc = P.nc
    L1 = mode == "L1"
    L3 = mode == "L3"
    L4 = mode == "L4"
    L6 = mode == "L6"

    def ext_out(name, shape, dtype):
        return nc.dram_tensor(name, list(shape), dtype, kind="ExternalOutput").ap(), Buf(name)

    def ext_in(name, shape, dtype=F32):
        return P.inp(name, shape, dtype), Buf(name)

    def internal(name, shape, dtype):
        return nc.dram_tensor(name, list(shape), dtype, kind="Internal").ap(), Buf(name)

    if L1:
        xT_full = P.inp("xT_full", [D, SEQ])
        xT_own = P.inp("xT_own", [D, TOK + 30])
        halo_mask = P.inp("halo_mask", [128, 30])
        ctxT = P.inp("ctxT", [D, CTX])
        cT = P.inp("cT", [128, 16])
        w_mod = P.inp("w_mod", [2, D, 6 * D])
        b_modT = P.inp("b_modT", [128, 2, 48])
        ev_w_in = P.inp("ev_w_in", [D, 2560])
        w_qk_perm = P.inp("w_qk_perm", [D, 1024])
        cs_full = P.inp("cs_full", [128, 2, SEQ])
        cs_own = P.inp("cs_own", [128, 2, TOK])
        conv_wT = P.inp("conv_wT", [128, 4, 31])
        conv_vecs = P.inp("conv_vecs", [128, 3, 4])
        lam_vecs = P.inp("lam_vecs", [1, 4, 64])
        diff_g = P.inp("diff_g", [128, 1])
        ev_w_out = P.inp("ev_w_out", [D, D])
    if L3:
        od_w_in = P.inp("od_w_in", [D, 1536])
        gmlp_ln = P.inp("gmlp_ln", [1, 2, 512])
        gmlp_wsT = P.inp("gmlp_wsT", [128, 4, 128])
        gmlp_bs = P.inp("gmlp_bs", [1, 4, 128])
        four_ln = P.inp("four_ln", [128, 2, 4])
        dft_cs = P.inp("dft_cs", [128, 256], BF16)
    if L4:
        od_w_out = P.inp("od_w_out", [D, D])
        c128 = P.inp("c128", [128, 256], BF16)
        tw_c = P.inp("tw_c", [128, 3, 128])
        cs32 = P.inp("cs32", [128, 64], BF16)
    ln_vecs = P.inp("ln_vecs", [128, 2, 4, 8])
    if L1 or L4:
        router_w = P.inp("router_w", [2, D, NE])
        router_b = P.inp("router_b", [2, 1, NE])
    ident = P.inp("ident", [128, 128])

    if L1:
        modT_d, modT_db = ext_out("modT_d", [128, 2, 48, 2], F32)
        kT_d, kT_db = internal("kT_d", [4, 128, NKEY], BF16)
        v_d, v_db = internal("v_d", [NKEY, 512], BF16)
        qT_d, qT_db = internal("qT_d", [4, 128, TOK], BF16)
        catT_d, catT_db = internal("catT_d", [D, TOK], BF16)
    else:
        modT_d, modT_db = ext_in("modT_d", [128, 2, 48, 2], F32)
    if L1 or L4:
        h1T_d, h1T_db = ext_out("h1T_d", [D, TOK], F32)
        u2T_d, u2T_db = ext_out("u2T_d", [D, TOK], BF16)
        gT_d, gT_db = ext_out("gT_d", [NE, TOK], F32)
    if L3 or L6:
        h1T_d, h1T_db = ext_in("h1T_d", [D, TOK], F32)
        y2p4, y2p4b = ext_in("y2p4", [4, D, TOK], F32)
    if L3:
        h2T_d, h2T_db = ext_out("h2T_d", [D, TOK], F32)
        spT_d, spT_db = ext_out("spT_d", [512, TOK], BF16)
        ab_own, ab_ownb = ext_out("ab_own", [TOK, 1024], BF16)
    if L4:
        h2T_d, h2T_db = ext_in("h2T_d", [D, TOK], F32)
        spT_d, spT_db = ext_in("spT_d", [512, TOK], BF16)
        ab_full, ab_fullb = ext_in("ab_full", [SEQ, 1024], BF16)
        Zd, Zdb = internal("Zd", [128, 128, 1024], BF16)
        fouT_d, fouT_db = internal("fouT_d", [512, TOK], BF16)
    if L6:
        outT, outT_b = ext_out("outT", [D, TOK], F32)

    psb = P.psb

    psb = P.psb

    ones_bf = P.sb("ones_bf", [128, 128], BF16)
    P.op("dve", lambda e: e.memset(ones_bf[:], 1.0), writes=[ones_bf])
    modT = P.sb("modT", [128, 2, 48, 2], F32)
    mod1p = P.sb("mod1p", [128, 2, 48, 2], F32)
    lnv = P.sb("lnv", [128, 2, 4, 8], F32)
    P.dma("sp", lnv[:], ln_vecs, writes=[lnv])
    ident_sb = P.sb("ident_sb", [128, 128], F32)
    P.dma("sp", ident_sb[:], ident, writes=[ident_sb])

    def MOD(l, idx, ch, m=0, plus1=False):
        t = mod1p if plus1 else modT
        return t[:, l, idx * 8 + ch, m:m + 1]


    if L1:
        mk = P.mark()
        cT_sb = P.sb("cT_sb", [128, 16], F32)
        sT_sb = P.sb("sT_sb", [128, 16], F32)
        bm_sb = P.sb("bm_sb", [128, 2, 48], F32)
        P.dma("sp", cT_sb[:], cT, writes=[cT_sb])
        P.dma("sp", bm_sb[:], b_modT, writes=[bm_sb])
        P.op("act", lambda e: e.activation(out=sT_sb[:], in_=cT_sb[:], func=AF.Silu),
             reads=[cT_sb], writes=[sT_sb])
        wm = [P.sb("wm%d" % i, [128, 8, 768], F32) for i in range(2)]
        for l in range(2):
            for blk in range(8):
                wt = wm[P.alt("wm", 2)]
                src = w_mod[l, :, blk * 768:(blk + 1) * 768].rearrange("(k p) n -> p k n", p=128)
                for kk in range(8):
                    P.dma("sp", wt[:, kk, :], src[:, kk, :], writes=[wt])
                for mc in range(6):
                    ps = psb[P.alt("ps", 8)]
                    for kk in range(8):
                        P.mm(ps, wt[:, kk, mc * 128:(mc + 1) * 128], sT_sb[:, kk * 2:kk * 2 + 2],
                             kk == 0, kk == 7, reads=[wt, sT_sb], out=ps[:, 0:2])
                    ch = blk * 6 + mc
                    P.op("dve", lambda e: e.tensor_tensor(
                        modT[:, l, ch, :], ps[:, 0:2], bm_sb[:, l, ch:ch + 1].to_broadcast([128, 2]), ALU.add),
                        reads=[ps, bm_sb], writes=[modT])
        P.op("dve", lambda e: e.tensor_scalar_add(mod1p[:], modT[:], 1.0), reads=[modT], writes=[mod1p])
        P.dma("sp", modT_d, modT[:], reads=[modT], writes=[modT_db])
        P.release(mk)
        if dbg == "M":
            P.barrier(engines=["sp"])
            P.close()
            return P
    else:
        P.dma("sp", modT[:], modT_d, reads=[modT_db], writes=[modT])
        P.op("dve", lambda e: e.tensor_scalar_add(mod1p[:], modT[:], 1.0), reads=[modT], writes=[mod1p])

    def modulate_tile(xt, ub, l, idx_sh, idx_sc, m, ncols):
        for kk in range(8):
            if kk % 2 == 0:
                P.op("act", lambda e: e.activation(out=ub[:, kk, 0:ncols], in_=xt[:, kk, 0:ncols],
                                                   func=AF.Identity,
                                                   bias=MOD(l, idx_sh, kk, m),
                                                   scale=MOD(l, idx_sc, kk, m, True)),
                     reads=[xt, mod1p, modT], writes=[ub])
            else:
                P.op("dve", lambda e: e.tensor_scalar(ub[:, kk, 0:ncols], xt[:, kk, 0:ncols],
                                                      MOD(l, idx_sc, kk, m, True), MOD(l, idx_sh, kk, m),
                                                      ALU.mult, ALU.add),
                     reads=[xt, mod1p, modT], writes=[ub])

    def ln_stats(src_chunks, nch, ncols, inv_n, tag):
        pm = psb[P.alt("ps", 8)]
        pq = psb[P.alt("ps", 8)]
        for i, (ap, tl) in enumerate(src_chunks):
            xb = st[tag + "_xb"][P.alt(tag + "xb", 2)]
            x2b = st[tag + "_x2b"][P.alt(tag + "x2b", 2)]
            P.op("act", lambda e: e.activation(out=xb[:, 0:ncols], in_=ap, func=AF.Copy),
                 reads=[tl], writes=[xb])
            P.op("act", lambda e: e.activation(out=x2b[:, 0:ncols], in_=ap, func=AF.Square),
                 reads=[tl], writes=[x2b])
            P.mm(pm, ones_bf[:], xb[:, 0:ncols], i == 0, i == nch - 1, reads=[ones_bf, xb], out=pm[:, 0:ncols], last=True)
            P.mm(pq, ones_bf[:], x2b[:, 0:ncols], i == 0, i == nch - 1, reads=[ones_bf, x2b], out=pq[:, 0:ncols], last=True)
        mean = st[tag + "_mean"]
        rstd = st[tag + "_rstd"]
        P.op("act", lambda e: e.activation(out=mean[:, 0:ncols], in_=pm[:, 0:ncols], func=AF.Identity, scale=inv_n),
             reads=[pm], writes=[mean])
        P.op("dve", lambda e: e.tensor_tensor(rstd[:, 0:ncols], mean[:, 0:ncols], mean[:, 0:ncols], ALU.mult),
             reads=[mean], writes=[rstd])
        P.op("dve", lambda e: e.scalar_tensor_tensor(rstd[:, 0:ncols], pq[:, 0:ncols], inv_n, rstd[:, 0:ncols],
                                                     ALU.mult, ALU.subtract),
             reads=[pq, rstd], writes=[rstd])
        P.op("dve", lambda e: e.tensor_scalar_add(rstd[:, 0:ncols], rstd[:, 0:ncols], EPS), reads=[], writes=[rstd])
        P.op("act", lambda e: e.sqrt(rstd[:, 0:ncols], rstd[:, 0:ncols]), reads=[], writes=[rstd])
        P.op("dve", lambda e: e.reciprocal(rstd[:, 0:ncols], rstd[:, 0:ncols]), reads=[], writes=[rstd])
        return mean, rstd

    st = {}

    def alloc_ln(tag, nch):
        st[tag + "_xb"] = [P.sb(tag + "_xb", [128, 512], BF16) for _ in range(2)]
        st[tag + "_x2b"] = [P.sb(tag + "_x2b", [128, 512], BF16) for _ in range(2)]
        st[tag + "_mean"] = P.sb(tag + "_mean", [128, 512], F32)
        st[tag + "_rstd"] = P.sb(tag + "_rstd", [128, 512], F32)


    if L1:
        mk = P.mark()
        wk = P.load_w_bf16("wk", ev_w_in[:, 512:1024], 512)
        wkp = P.load_w_bf16("wkp", w_qk_perm[:, 512:1024], 512)
        wv = P.load_w_bf16("wv", ev_w_in[:, 1024:1536], 512)
        xts = [P.sb("xtA%d" % i, [128, 8, 512], F32) for i in range(2)]
        ubs = [P.sb("ubA%d" % i, [128, 8, 512], BF16) for i in range(2)]
        css = [P.sb("csA%d" % i, [128, 2, 512], F32) for i in range(2)]
        t1s = [P.sb("t1A%d" % i, [128, 512], F32) for i in range(2)]
        t2s = [P.sb("t2A%d" % i, [128, 512], F32) for i in range(2)]
        krs = [P.sb("krA%d" % i, [128, 512], BF16) for i in range(3)]
        vbs = [P.sb("vbA%d" % i, [128, 512], BF16) for i in range(3)]
        for it in range(SEQ // 512 + 1):
            is_ctx = it == SEQ // 512
            ncols = CTX if is_ctx else 512
            c0 = it * 512
            xt = xts[it % 2]
            ub = ubs[it % 2]
            cs = css[it % 2]
            srcx = (ctxT if is_ctx else xT_full[:, c0:c0 + 512]).rearrange("(k p) t -> p k t", p=128)
            for kk in range(8):
                P.dma("sp", xt[:, kk, 0:ncols], srcx[:, kk, :], writes=[xt])
            if not is_ctx:
                P.dma("sp", cs[:], cs_full[:, :, c0:c0 + 512], writes=[cs])
            modulate_tile(xt, ub, 0, 0, 1, 1 if is_ctx else 0, ncols)
            for hc in range(4):
                pa = psb[P.alt("ps", 8)]
                P.proj_fm(pa, wk, hc * 128, ub, [wk, ub], ncols)
                kr = krs[P.alt("krA", 3)]
                if is_ctx:
                    P.op("act", lambda e: e.activation(out=kr[:, 0:ncols], in_=pa[:, 0:ncols], func=AF.Copy),
                         reads=[pa], writes=[kr])
                else:
                    pb = psb[P.alt("ps", 8)]
                    P.proj_fm(pb, wkp, hc * 128, ub, [wkp, ub], ncols)
                    t1 = t1s[hc % 2]
                    t2 = t2s[hc % 2]
                    P.op("dve", lambda e: e.tensor_tensor(t1[:], pa[:], cs[:, 0, :], ALU.mult),
                         reads=[pa, cs], writes=[t1])
                    P.op("dve", lambda e: e.tensor_tensor(t2[:], pb[:], cs[:, 1, :], ALU.mult),
                         reads=[pb, cs], writes=[t2])
                    P.op("pool", lambda e: e.tensor_tensor(kr[:], t1[:], t2[:], ALU.add),
                         reads=[t1, t2], writes=[kr])
                P.dma("sp", kT_d[hc, :, c0:c0 + ncols], kr[:, 0:ncols], reads=[kr], writes=[kT_db])
            for s in range(ncols // 128):
                pv = psb[P.alt("ps", 8)]
                for kk in range(8):
                    P.mm(pv, ub[:, kk, s * 128:(s + 1) * 128], wv[:, kk, :], kk == 0, kk == 7, reads=[ub, wv])
                vb = vbs[P.alt("vbA", 3)]
                P.op("act", lambda e: e.activation(out=vb[:], in_=pv[:], func=AF.Copy), reads=[pv], writes=[vb])
                P.dma("sp", v_d[c0 + s * 128:c0 + (s + 1) * 128, :], vb[:], reads=[vb], writes=[v_db])
        P.release(mk)

        if dbg == "A":
            P.barrier(engines=["sp"])
            P.close()
            return P
        mk = P.mark()
        wq = P.load_w_bf16("wq", ev_w_in[:, 0:512], 512)
        wqp = P.load_w_bf16("wqp", w_qk_perm[:, 0:512], 512)
        wval = P.load_w_bf16("wval", ev_w_in[:, 1536:2048], 512)
        wgat = P.load_w_bf16("wgat", ev_w_in[:, 2048:2560], 512)
        aT = P.sb("aT", [128, 4, TOK + 30], F32)
        hm = P.sb("hm", [128, 30], F32)
        P.dma("sp", hm[:], halo_mask, writes=[hm])
        cw = P.sb("cw", [128, 4, 31], F32)
        P.dma("sp", cw[:], conv_wT, writes=[cw])
        cv = P.sb("cv", [128, 3, 4], F32)
        P.dma("sp", cv[:], conv_vecs, writes=[cv])
        xts = [P.sb("xtB%d" % i, [128, 8, 512], F32) for i in range(2)]
        ubs = [P.sb("ubB%d" % i, [128, 8, 512], BF16) for i in range(2)]
        css = [P.sb("csB%d" % i, [128, 2, 512], F32) for i in range(2)]
        t1s = [P.sb("t1B%d" % i, [128, 512], F32) for i in range(2)]
        t2s = [P.sb("t2B%d" % i, [128, 512], F32) for i in range(2)]
        qrs = [P.sb("qrB%d" % i, [128, 512], BF16) for i in range(3)]
        sgs = [P.sb("sgB%d" % i, [128, 512], F32) for i in range(2)]
        for it in range(NT + 1):
            is_halo = it == NT
            ncols = 30 if is_halo else 512
            c0 = it * 512
            xt = xts[it % 2]
            ub = ubs[it % 2]
            cs = css[it % 2]
            srcx = xT_own[:, c0:c0 + ncols].rearrange("(k p) t -> p k t", p=128)
            for kk in range(8):
                P.dma("sp", xt[:, kk, 0:ncols], srcx[:, kk, :], writes=[xt])
            modulate_tile(xt, ub, 0, 0, 1, 0, ncols)
            if not is_halo:
                P.dma("sp", cs[:], cs_own[:, :, c0:c0 + 512], writes=[cs])
                for hc in range(4):
                    pa = psb[P.alt("ps", 8)]
                    P.proj_fm(pa, wq, hc * 128, ub, [wq, ub])
                    pb = psb[P.alt("ps", 8)]
                    P.proj_fm(pb, wqp, hc * 128, ub, [wqp, ub])
                    t1 = t1s[hc % 2]
                    t2 = t2s[hc % 2]
                    qr = qrs[P.alt("qrB", 3)]
                    P.op("dve", lambda e: e.tensor_tensor(t1[:], pa[:], cs[:, 0, :], ALU.mult),
                         reads=[pa, cs], writes=[t1])
                    P.op("dve", lambda e: e.tensor_tensor(t2[:], pb[:], cs[:, 1, :], ALU.mult),
                         reads=[pb, cs], writes=[t2])
                    P.op("pool", lambda e: e.tensor_tensor(qr[:], t1[:], t2[:], ALU.add),
                         reads=[t1, t2], writes=[qr])
                    P.dma("sp", qT_d[hc, :, c0:c0 + 512], qr[:], reads=[qr], writes=[qT_db])
            for ch in range(4):
                pvv = psb[P.alt("ps", 8)]
                P.proj_fm(pvv, wval, ch * 128, ub, [wval, ub], ncols)
                pg = psb[P.alt("ps", 8)]
                P.proj_fm(pg, wgat, ch * 128, ub, [wgat, ub], ncols)
                sg = sgs[ch % 2]
                P.op("act", lambda e: e.activation(out=sg[:, 0:ncols], in_=pg[:, 0:ncols], func=AF.Sigmoid),
                     reads=[pg], writes=[sg])
                if not is_halo:
                    P.op("dve", lambda e: e.tensor_tensor(aT[:, ch, 15 + c0:15 + c0 + 512], pvv[:], sg[:], ALU.mult),
                         reads=[pvv, sg], writes=[aT])
                else:
                    P.op("dve", lambda e: e.tensor_tensor(sg[:, 0:30], pvv[:, 0:30], sg[:, 0:30], ALU.mult),
                         reads=[pvv, sg], writes=[sg])
                    P.op("dve", lambda e: e.tensor_tensor(aT[:, ch, 0:15], sg[:, 0:15], hm[:, 0:15], ALU.mult),
                         reads=[sg, hm], writes=[aT])
                    P.op("dve", lambda e: e.tensor_tensor(aT[:, ch, 15 + TOK:30 + TOK], sg[:, 15:30], hm[:, 15:30],
                                                          ALU.mult),
                         reads=[sg, hm], writes=[aT])
        if dbg == "B1":
            P.barrier(engines=["sp"])
            P.close()
            return P
        alloc_ln("cln", 4)
        accs = [P.sb("cacc%d" % i, [128, 512], F32) for i in range(4)]
        cvo = [P.sb("cvo%d" % i, [128, 4, 512], BF16) for i in range(2)]
        xcs = [P.sb("cxc%d" % i, [128, 512], F32) for i in range(2)]
        for it in range(NT):
            c0 = it * 512
            for ch in range(4):
                eng = "dve"
                acc = accs[ch]
                P.op(eng, lambda e: e.tensor_scalar(acc[:], aT[:, ch, c0:c0 + 512], cw[:, ch, 0:1], cv[:, 0, ch:ch + 1],
                                                    ALU.mult, ALU.add),
                     reads=[aT, cw, cv], writes=[acc])
                for j in range(1, 31):
                    P.op(eng, lambda e: e.scalar_tensor_tensor(acc[:], aT[:, ch, c0 + j:c0 + j + 512], cw[:, ch, j:j + 1],
                                                               acc[:], ALU.mult, ALU.add),
                         reads=[aT, cw], writes=[acc])
            mean, rstd = ln_stats([(accs[ch][:], accs[ch]) for ch in range(4)], 4, 512, 1.0 / 512, "cln")
            co = cvo[it % 2]
            for ch in range(4):
                xc = xcs[ch % 2]
                P.op("dve", lambda e: e.tensor_tensor(xc[:], accs[ch][:], mean[:], ALU.subtract),
                     reads=[accs[ch], mean], writes=[xc])
                P.op("pool", lambda e: e.tensor_tensor(xc[:], xc[:], rstd[:], ALU.mult),
                     reads=[rstd], writes=[xc])
                P.op("dve", lambda e: e.tensor_scalar(xc[:], xc[:], cv[:, 1, ch:ch + 1], cv[:, 2, ch:ch + 1],
                                                      ALU.mult, ALU.add),
                     reads=[cv], writes=[xc])
                P.op("act", lambda e: e.activation(out=co[:, ch, :], in_=xc[:], func=AF.Silu),
                     reads=[xc], writes=[co])
            dst = catT_d[0:512, c0:c0 + 512].rearrange("(k p) t -> p k t", p=128)
            P.dma("sp", dst, co[:], reads=[co], writes=[catT_db])
        P.release(mk)

        if dbg == "B":
            P.barrier(engines=["sp"])
            P.close()
            return P
        mk = P.mark()
        lv = P.sb("lv", [1, 4, 64], F32)
        P.dma("sp", lv[:], lam_vecs, writes=[lv])
        lp = P.sb("lp", [1, 2, 64], F32)
        ls = P.sb("ls", [1, 4], F32)
        P.op("dve", lambda e: e.tensor_tensor(lp[:, 0, :], lv[:, 0, :], lv[:, 1, :], ALU.mult), reads=[lv], writes=[lp])
        P.op("dve", lambda e: e.tensor_tensor(lp[:, 1, :], lv[:, 2, :], lv[:, 3, :], ALU.mult), reads=[lv], writes=[lp])
        P.op("dve", lambda e: e.tensor_reduce(ls[:, 0:2], lp[:], AX.X, ALU.add), reads=[lp], writes=[ls])
        P.op("act", lambda e: e.activation(out=ls[:, 2:4], in_=ls[:, 0:2], func=AF.Exp), reads=[ls], writes=[ls])
        P.op("dve", lambda e: e.tensor_tensor(ls[:, 0:1], ls[:, 3:4], ls[:, 2:3], ALU.subtract), reads=[ls], writes=[ls])
        P.op("dve", lambda e: e.tensor_scalar_add(ls[:, 0:1], ls[:, 0:1], -LAM_INIT0), reads=[ls], writes=[ls])
        ones_f = P.sb("ones_f", [1, 128], F32)
        P.op("dve", lambda e: e.memset(ones_f[:], 1.0), writes=[ones_f])
        neglam = P.sb("neglam", [128, 1], F32)
        pl = psb[P.alt("ps", 8)]
        P.mm(pl, ones_f[:], ls[:, 0:1], True, True, reads=[ones_f, ls], out=pl[:, 0:1])
        P.op("dve", lambda e: e.tensor_copy(neglam[:], pl[:, 0:1]), reads=[pl], writes=[neglam])
        dg = P.sb("dg", [128, 1], F32)
        P.dma("sp", dg[:], diff_g, writes=[dg])
        P.op("dve", lambda e: e.tensor_scalar_mul(dg[:], dg[:], 1.0 - LAM_INIT0), reads=[dg], writes=[dg])

        kTh = P.sb("kTh", [128, NKEY], BF16)
        vh = P.sb("vh", [128, NKT, 128], BF16)
        qz = [P.sb("qz%d" % i, [128, TOK], BF16) for i in range(2)]
        for c in range(2):
            P.op("pool", lambda e: e.memset(qz[c][:], 0.0), writes=[qz[c]])
        pts = [P.sb("pt%d" % i, [128, 512], BF16) for i in range(6)]
        accD = P.sb("accD", [128, 512], F32)
        accP = P.sb("accP", [128, 512], F32)
        ones_f32m = P.sb("ones_f32m", [128, 128], F32)
        P.op("dve", lambda e: e.memset(ones_f32m[:], 1.0), writes=[ones_f32m])
        ocs = [P.sb("oc%d" % i, [128, 512], F32) for i in range(2)]
        rl = P.sb("rl", [128, 512], F32)
        osq = P.sb("osq", [128, 512], BF16)
        att_o = [P.sb("atto%d" % i, [128, 512], BF16) for i in range(2)]
        ps_s = psb[0:3]
        ps_o = psb[3:5]
        ps_l = psb[5:7]
        ps_r = psb[7]
        for h in range(4):
            for piece in range(5):
                a = piece * 3328
                P.dma("sp", kTh[:, a:a + 3328], kT_d[h, :, a:a + 3328], reads=[kT_db], writes=[kTh])
            vsrc = v_d[:, h * 128:(h + 1) * 128].rearrange("(kt p) d -> p kt d", p=128)
            for piece in range(10):
                P.dma("sp", vh[:, piece * 13:(piece + 1) * 13, :], vsrc[:, piece * 13:(piece + 1) * 13, :],
                      reads=[v_db], writes=[vh])
            for c in range(2):
                P.dma("sp", qz[c][c * 64:(c + 1) * 64, :], qT_d[h, c * 64:(c + 1) * 64, :], reads=[qT_db], writes=[qz[c]])
            for qt in range(NT):
                q0 = qt * 512
                for c in range(2):
                    po = ps_o[c]
                    pL = ps_l[c]
                    r0 = c * 64
                    def qk_exp(kt):
                        pss = ps_s[P.alt("pss", 3)]
                        P.mm(pss, kTh[:, kt * 128:(kt + 1) * 128], qz[c][:, q0:q0 + 512], True, True,
                             reads=[kTh, qz[c]])
                        pt = pts[P.alt("pt", 6)]
                        P.op("act", lambda e: e.activation(out=pt[:], in_=pss[:], func=AF.Exp, scale=0.125),
                             reads=[pss], writes=[pt])
                        return pt
                    pt_next = qk_exp(0)
                    for kt in range(NKT):
                        pt = pt_next
                        if kt + 1 < NKT:
                            pt_next = qk_exp(kt + 1)
                        P.mm(po, vh[:, kt, :], pt[:], kt == 0, kt == NKT - 1, reads=[vh, pt], last=True)
                        if kt % 3 == 2:
                            eng, acc, first = "pool", accP, kt == 2
                        else:
                            eng, acc, first = "dve", accD, kt == 0
                        if first:
                            P.op(eng, lambda e: e.tensor_copy(acc[:], pt[:]), reads=[pt], writes=[acc])
                        else:
                            P.op(eng, lambda e: e.tensor_tensor(acc[:], acc[:], pt[:], ALU.add), reads=[pt], writes=[acc])
                    P.mm(pL, ones_f32m[:], accD[:], True, False, reads=[ones_f32m, accD], last=False)
                    P.mm(pL, ones_f32m[:], accP[:], False, True, reads=[ones_f32m, accP], last=True)
                    P.op("dve", lambda e: e.reciprocal(rl[:], pL[:]), reads=[pL], writes=[rl])
                    P.op("dve", lambda e: e.tensor_tensor(ocs[c][:], po[:], rl[:], ALU.mult),
                         reads=[po, rl], writes=[ocs[c]])
                o = ocs[0]
                P.op("dve", lambda e: e.scalar_tensor_tensor(o[:], ocs[1][:], neglam[:, 0:1], o[:], ALU.mult, ALU.add),
                     reads=[ocs[1], neglam], writes=[o])
                P.op("act", lambda e: e.activation(out=osq[:], in_=o[:], func=AF.Square), reads=[o], writes=[osq])
                P.mm(ps_r, ones_bf[:], osq[:], True, True, reads=[ones_bf, osq])
                P.op("dve", lambda e: e.tensor_scalar(rl[:], ps_r[:], 1.0 / 128, EPS, ALU.mult, ALU.add),
                     reads=[ps_r], writes=[rl])
                P.op("act", lambda e: e.sqrt(rl[:], rl[:]), reads=[], writes=[rl])
                P.op("dve", lambda e: e.reciprocal(rl[:], rl[:]), reads=[], writes=[rl])
                P.op("dve", lambda e: e.tensor_tensor(o[:], o[:], rl[:], ALU.mult), reads=[rl], writes=[o])
                ao = att_o[P.alt("atto", 2)]
                P.op("act", lambda e: e.activation(out=ao[:], in_=o[:], func=AF.Identity, scale=dg[:, 0:1]),
                     reads=[o, dg], writes=[ao])
                P.dma("sp", catT_d[512 + h * 128:512 + (h + 1) * 128, q0:q0 + 512], ao[:], reads=[ao], writes=[catT_db])
        P.release(mk)

    def post_mixer(l, w_out_ap, catA, catAb, catB, catBb, res_src, res_srcb):
        mk = P.mark()
        wo = P.load_w_bf16("wo", w_out_ap, D)
        rw = P.sb("rw", [128, 8, NE], F32)
        P.dma("sp", rw[:], router_w[l].rearrange("(k p) e -> p k e", p=128), writes=[rw])
        rb = P.sb("rb", [128, NE], F32)
        P.dma("sp", rb[:], router_b[l].partition_broadcast(128), writes=[rb])
        alloc_ln("ln1", 8)
        cats = [P.sb("cat%d" % i, [128, 8, 512], BF16) for i in range(2)]
        xts = [P.sb("xtD%d" % i, [128, 8, 512], F32) for i in range(2)]
        rts = [P.sb("rtD%d" % i, [128, 8, 512], F32) for i in range(2)]
        u2f = P.sb("u2f", [128, 8, 512], F32)
        u2b = [P.sb("u2b%d" % i, [128, 8, 512], BF16) for i in range(2)]
        lg = P.sb("lg", [128, NE], F32)
        mx = P.sb("mx", [128, 8], F32)
        msk = P.sb("msk", [128, NE], F32)
        ex = P.sb("ex", [128, NE], F32)
        ssum = P.sb("ssum", [128, 1], F32)
        gts = [P.sb("gts%d" % i, [128, NE], F32) for i in range(2)]
        gTt = P.sb("gTt", [NE, 512], F32)
        for it in range(NT):
            c0 = it * 512
            cat = cats[it % 2]
            xt = xts[it % 2]
            rt = rts[it % 2]
            srcA = catA[:, c0:c0 + 512].rearrange("(k p) t -> p k t", p=128)
            srcB = catB[:, c0:c0 + 512].rearrange("(k p) t -> p k t", p=128)
            srcx = res_src[:, c0:c0 + 512].rearrange("(k p) t -> p k t", p=128)
            for kk in range(8):
                if kk < 4:
                    P.dma("sp", cat[:, kk, :], srcA[:, kk, :], reads=[catAb], writes=[cat])
                else:
                    P.dma("sp", cat[:, kk, :], srcB[:, kk - 4, :], reads=[catBb], writes=[cat])
                P.dma("sp", xt[:, kk, :], srcx[:, kk, :], reads=[res_srcb], writes=[xt])
            for m in range(8):
                py = psb[P.alt("ps", 8)]
                P.proj_fm(py, wo, m * 128, cat, [wo, cat])
                P.op("act", lambda e: e.activation(out=xt[:, m, :], in_=xt[:, m, :], func=AF.Identity, scale=ALPHA),
                     reads=[], writes=[xt])
                P.op("dve", lambda e: e.scalar_tensor_tensor(rt[:, m, :], py[:], MOD(l, 2, m), xt[:, m, :],
                                                             ALU.mult, ALU.add),
                     reads=[py, modT, xt], writes=[rt])
            mean, rstd = ln_stats([(rt[:, m, :], rt) for m in range(8)], 8, 512, 1.0 / D, "ln1")
            ub = u2b[it % 2]
            for m in range(8):
                P.op("dve", lambda e: e.tensor_tensor(rt[:, m, :], rt[:, m, :], mean[:], ALU.subtract),
                     reads=[mean], writes=[rt])
                P.op("pool", lambda e: e.tensor_tensor(rt[:, m, :], rt[:, m, :], rstd[:], ALU.mult),
                     reads=[rstd], writes=[rt])
                P.op("dve", lambda e: e.tensor_scalar(rt[:, m, :], rt[:, m, :], lnv[:, l, 0, m:m + 1],
                                                      lnv[:, l, 1, m:m + 1], ALU.mult, ALU.add),
                     reads=[lnv], writes=[rt])
                P.op("act", lambda e: e.activation(out=u2f[:, m, :], in_=rt[:, m, :], func=AF.Identity,
                                                   bias=MOD(l, 3, m), scale=MOD(l, 4, m, 0, True)),
                     reads=[rt, modT, mod1p], writes=[u2f])
                P.op("pool", lambda e: e.tensor_copy(ub[:, m, :], u2f[:, m, :]), reads=[u2f], writes=[ub])
            dsth = h1T_d[:, c0:c0 + 512].rearrange("(k p) t -> p k t", p=128)
            dstu = u2T_d[:, c0:c0 + 512].rearrange("(k p) t -> p k t", p=128)
            for kk in range(8):
                P.dma("sp", dsth[:, kk, :], rt[:, kk, :], reads=[rt], writes=[h1T_db])
                P.dma("sp", dstu[:, kk, :], ub[:, kk, :], reads=[ub], writes=[u2T_db])
            for s in range(4):
                pr = psb[P.alt("ps", 8)]
                for kk in range(8):
                    P.mm(pr, u2f[:, kk, s * 128:(s + 1) * 128], rw[:, kk, :], kk == 0, kk == 7,
                         reads=[u2f, rw], out=pr[:, 0:NE])
                P.op("dve", lambda e: e.tensor_tensor(lg[:], pr[:, 0:NE], rb[:], ALU.add), reads=[pr, rb], writes=[lg])
                P.op("dve", lambda e: e.max(out=mx[:], in_=lg[:]), reads=[lg], writes=[mx])
                P.op("dve", lambda e: e.tensor_scalar(msk[:], lg[:], mx[:, 3:4], None, ALU.is_ge),
                     reads=[lg, mx], writes=[msk])
                P.op("dve", lambda e: e.tensor_scalar(ex[:], lg[:], mx[:, 0:1], None, ALU.subtract),
                     reads=[lg, mx], writes=[ex])
                P.op("act", lambda e: e.activation(out=ex[:], in_=ex[:], func=AF.Exp), reads=[], writes=[ex])
                P.op("dve", lambda e: e.tensor_tensor(ex[:], ex[:], msk[:], ALU.mult), reads=[msk], writes=[ex])
                P.op("dve", lambda e: e.tensor_reduce(ssum[:], ex[:], AX.X, ALU.add), reads=[ex], writes=[ssum])
                P.op("dve", lambda e: e.reciprocal(ssum[:], ssum[:]), reads=[], writes=[ssum])
                gt = gts[s % 2]
                P.op("dve", lambda e: e.tensor_scalar(gt[:], ex[:], ssum[:, 0:1], None, ALU.mult),
                     reads=[ex, ssum], writes=[gt])
                pg = psb[P.alt("ps", 8)]
                P.mm(pg, gt[:], ident_sb[:], True, True, reads=[gt, ident_sb], out=pg[0:NE, 0:128])
                P.op("act", lambda e: e.activation(out=gTt[:, s * 128:(s + 1) * 128], in_=pg[0:NE, 0:128], func=AF.Copy),
                     reads=[pg], writes=[gTt])
            P.dma("sp", gT_d[:, c0:c0 + 512], gTt[:], reads=[gTt], writes=[gT_db])
        P.release(mk)

    def sum_ln2(l, out_dst, out_dstb):
        mk = P.mark()
        alloc_ln("ln2", 8)
        hts = [P.sb("htS%d" % i, [128, 8, 512], F32) for i in range(2)]
        pbs = [P.sb("pbS%d" % i, [128, 8, 512], F32) for i in range(2)]
        acc = P.sb("accS", [128, 8, 512], F32)
        for it in range(NT):
            c0 = it * 512
            ht = hts[it % 2]
            srch = h1T_d[:, c0:c0 + 512].rearrange("(k p) t -> p k t", p=128)
            for kk in range(8):
                P.dma("sp", ht[:, kk, :], srch[:, kk, :], reads=[h1T_db], writes=[ht])
            for pr in range(2):
                for i in range(2):
                    srcp = y2p4[pr * 2 + i, :, c0:c0 + 512].rearrange("(k p) t -> p k t", p=128)
                    for kk in range(8):
                        P.dma("sp", pbs[i][:, kk, :], srcp[:, kk, :], reads=[y2p4b], writes=[pbs[i]])
                if pr == 0:
                    P.op("dve", lambda e: e.tensor_tensor(acc[:], pbs[0][:], pbs[1][:], ALU.add),
                         reads=[pbs[0], pbs[1]], writes=[acc])
                else:
                    P.op("pool", lambda e: e.tensor_tensor(acc[:], acc[:], pbs[0][:], ALU.add),
                         reads=[pbs[0]], writes=[acc])
                    P.op("dve", lambda e: e.tensor_tensor(acc[:], acc[:], pbs[1][:], ALU.add),
                         reads=[pbs[1]], writes=[acc])
            P.op("act", lambda e: e.activation(out=ht[:], in_=ht[:], func=AF.Identity, scale=ALPHA),
                 reads=[], writes=[ht])
            for m in range(8):
                P.op("dve", lambda e: e.scalar_tensor_tensor(ht[:, m, :], acc[:, m, :], MOD(l, 5, m), ht[:, m, :],
                                                             ALU.mult, ALU.add),
                     reads=[acc, modT], writes=[ht])
            mean, rstd = ln_stats([(ht[:, m, :], ht) for m in range(8)], 8, 512, 1.0 / D, "ln2")
            for m in range(8):
                P.op("dve", lambda e: e.tensor_tensor(ht[:, m, :], ht[:, m, :], mean[:], ALU.subtract),
                     reads=[mean], writes=[ht])
                P.op("pool", lambda e: e.tensor_tensor(ht[:, m, :], ht[:, m, :], rstd[:], ALU.mult),
                     reads=[rstd], writes=[ht])
                P.op("dve", lambda e: e.tensor_scalar(ht[:, m, :], ht[:, m, :], lnv[:, l, 2, m:m + 1],
                                                      lnv[:, l, 3, m:m + 1], ALU.mult, ALU.add),
                     reads=[lnv], writes=[ht])
            dsto = out_dst[:, c0:c0 + 512].rearrange("(k p) t -> p k t", p=128)
            for kk in range(8):
                P.dma("sp", dsto[:, kk, :], ht[:, kk, :], reads=[ht], writes=[out_dstb])
        P.release(mk)

    if L1:
        xres = xT_own[:, 0:TOK]
        post_mixer(0, ev_w_out, catT_d[0:512, :], catT_db, catT_d[512:1024, :], catT_db, xres, Buf("xres"))
    if L3:
        sum_ln2(0, h2T_d, h2T_db)
    if L6:
        sum_ln2(1, outT, outT_b)

    if L3:
        mk = P.mark()
        wu = P.load_w_bf16("wu", od_w_in[:, 0:512], 512)
        wv1 = P.load_w_bf16("wv1", od_w_in[:, 512:1024], 512)
        wf = P.load_w_bf16("wf", od_w_in[:, 1024:1536], 512)
        wsT = P.sb("wsT", [128, 4, 128], BF16)
        P.dma("pool", wsT[:], gmlp_wsT, writes=[wsT])
        bsb = P.sb("bsb", [128, 4, 128], F32)
        P.dma("sp", bsb[:], gmlp_bs.partition_broadcast(128), writes=[bsb])
        gln = P.sb("gln", [128, 2, 512], F32)
        P.dma("sp", gln[:], gmlp_ln.partition_broadcast(128), writes=[gln])
        fl = P.sb("fl", [128, 2, 4], F32)
        P.dma("sp", fl[:], four_ln, writes=[fl])
        dcs = P.sb("dcs", [128, 256], BF16)
        P.dma("sp", dcs[:], dft_cs, writes=[dcs])
        alloc_ln("fln", 1)
        xts = [P.sb("xtF%d" % i, [128, 8, 512], F32) for i in range(2)]
        ubs = [P.sb("ubF%d" % i, [128, 8, 512], BF16) for i in range(2)]
        ugs = [P.sb("ugF%d" % i, [128, 4, 512], F32) for i in range(2)]
        vg = P.sb("vgF", [128, 512], F32)
        bst = P.sb("bst", [128, 2, 6], F32)
        bag = P.sb("bag", [128, 2], F32)
        vrs = P.sb("vrs", [128, 1], F32)
        vln = [P.sb("vln%d" % i, [128, 512], BF16) for i in range(2)]
        svt = P.sb("svt", [128, 128], F32)
        spo = [P.sb("spo%d" % i, [128, 4, 512], BF16) for i in range(2)]
        fch = [P.sb("fch%d" % i, [128, 512], F32) for i in range(2)]
        flb = [P.sb("flb%d" % i, [128, 4, 512], BF16) for i in range(2)]
        abo = [P.sb("abo%d" % i, [128, 1024], BF16) for i in range(2)]

        def gelu_tanh(dst, dst_t, src, reads, tmp):
            c = 2.0 * math.sqrt(2.0 / math.pi)
            P.op("act", lambda e: e.activation(out=tmp[:], in_=src, func=AF.Square), reads=reads, writes=[tmp])
            P.op("dve", lambda e: e.tensor_scalar(tmp[:], tmp[:], 0.044715 * c, c, ALU.mult, ALU.add), reads=[], writes=[tmp])
            P.op("dve", lambda e: e.tensor_tensor(tmp[:], tmp[:], src, ALU.mult), reads=reads, writes=[tmp])
            P.op("act", lambda e: e.activation(out=tmp[:], in_=tmp[:], func=AF.Sigmoid), reads=[], writes=[tmp])
            P.op("dve", lambda e: e.tensor_tensor(dst, tmp[:], src, ALU.mult), reads=reads + [tmp], writes=[dst_t])

        gtmp = [P.sb("gtmp%d" % i, [128, 512], F32) for i in range(2)]
        for it in range(NT):
            c0 = it * 512
            xt = xts[it % 2]
            ub = ubs[it % 2]
            srcx = h2T_d[:, c0:c0 + 512].rearrange("(k p) t -> p k t", p=128)
            for kk in range(8):
                P.dma("sp", xt[:, kk, :], srcx[:, kk, :], reads=[h2T_db], writes=[xt])
            modulate_tile(xt, ub, 1, 0, 1, 0, 512)
            ug = ugs[it % 2]
            for ch in range(4):
                pu = psb[P.alt("ps", 8)]
                P.proj_fm(pu, wu, ch * 128, ub, [wu, ub])
                tmp = gtmp[ch % 2]
                gelu_tanh(ug[:, ch, :], ug, pu[:], [pu], tmp)
            sp = spo[it % 2]
            for s in range(4):
                pv = psb[P.alt("ps", 8)]
                for kk in range(8):
                    P.mm(pv, ub[:, kk, s * 128:(s + 1) * 128], wv1[:, kk, :], kk == 0, kk == 7, reads=[ub, wv1])
                tmp = gtmp[s % 2]
                gelu_tanh(vg[:], vg, pv[:], [pv], tmp)
                P.op("dve", lambda e: e.bn_stats(bst[:, 0, :], vg[:, 0:256]), reads=[vg], writes=[bst])
                P.op("dve", lambda e: e.bn_stats(bst[:, 1, :], vg[:, 256:512]), reads=[vg], writes=[bst])
                P.op("dve", lambda e: e.bn_aggr(bag[:], bst[:]), reads=[bst], writes=[bag])
                P.op("dve", lambda e: e.tensor_scalar_add(vrs[:], bag[:, 1:2], EPS), reads=[bag], writes=[vrs])
                P.op("act", lambda e: e.sqrt(vrs[:], vrs[:]), reads=[], writes=[vrs])
                P.op("dve", lambda e: e.reciprocal(vrs[:], vrs[:]), reads=[], writes=[vrs])
                P.op("dve", lambda e: e.tensor_scalar(vg[:], vg[:], bag[:, 0:1], vrs[:, 0:1], ALU.subtract, ALU.mult),
                     reads=[bag, vrs], writes=[vg])
                P.op("pool", lambda e: e.tensor_tensor(vg[:], vg[:], gln[:, 0, :], ALU.mult), reads=[gln], writes=[vg])
                vl = vln[s % 2]
                P.op("pool", lambda e: e.tensor_tensor(vl[:], vg[:], gln[:, 1, :], ALU.add), reads=[vg, gln], writes=[vl])
                for g in range(4):
                    pss = psb[P.alt("ps", 8)]
                    P.mm(pss, vl[:, g * 128:(g + 1) * 128], wsT[:, g, :], True, True, reads=[vl, wsT], out=pss[:, 0:128])
                    P.op("dve", lambda e: e.tensor_tensor(svt[:], pss[:, 0:128], bsb[:, g, :], ALU.add),
                         reads=[pss, bsb], writes=[svt])
                    P.op("dve", lambda e: e.tensor_tensor(sp[:, g, s * 128:(s + 1) * 128], svt[:],
                                                          ug[:, g, s * 128:(s + 1) * 128], ALU.mult),
                         reads=[svt, ug], writes=[sp])
            dsts = spT_d[:, c0:c0 + 512].rearrange("(k p) t -> p k t", p=128)
            P.dma("sp", dsts, sp[:], reads=[sp], writes=[spT_db])
            fb = flb[it % 2]
            for g in range(4):
                pf = psb[P.alt("ps", 8)]
                P.proj_fm(pf, wf, g * 128, ub, [wf, ub])
                fc = fch[g % 2]
                P.op("act", lambda e: e.activation(out=fc[:], in_=pf[:], func=AF.Copy), reads=[pf], writes=[fc])
                mean, rstd = ln_stats([(fc[:], fc)], 1, 512, 1.0 / 128, "fln")
                P.op("dve", lambda e: e.tensor_tensor(fc[:], fc[:], mean[:], ALU.subtract), reads=[mean], writes=[fc])
                P.op("pool", lambda e: e.tensor_tensor(fc[:], fc[:], rstd[:], ALU.mult), reads=[rstd], writes=[fc])
                P.op("dve", lambda e: e.tensor_scalar(fb[:, g, :], fc[:], fl[:, 0, g:g + 1], fl[:, 1, g:g + 1],
                                                      ALU.mult, ALU.add),
                     reads=[fc, fl], writes=[fb])
            for s in range(4):
                ab = abo[s % 2]
                for g in range(4):
                    pab = psb[P.alt("ps", 8)]
                    P.mm(pab, fb[:, g, s * 128:(s + 1) * 128], dcs[:], True, True, reads=[fb, dcs], out=pab[:, 0:256])
                    P.op("act", lambda e: e.activation(out=ab[:, g * 256:(g + 1) * 256], in_=pab[:, 0:256], func=AF.Copy),
                         reads=[pab], writes=[ab])
                P.dma("sp", ab_own[c0 + s * 128:c0 + (s + 1) * 128, :], ab[:], reads=[ab], writes=[ab_ownb])
        P.release(mk)

    if L4:
        mk = P.mark()
        c128s = P.sb("c128s", [128, 256], BF16)
        P.dma("sp", c128s[:], c128, writes=[c128s])
        tws = P.sb("tws", [128, 3, 128], F32)
        P.dma("sp", tws[:], tw_c, writes=[tws])
        ins_ = [P.sb("fin%d" % i, [128, 1024], BF16) for i in range(3)]
        p2s = [P.sb("p2s%d" % i, [128, 512], F32) for i in range(2)]
        yr = [P.sb("yr%d" % i, [128, 2, 128], F32) for i in range(2)]
        ym = [P.sb("ym%d" % i, [128, 2, 128], F32) for i in range(2)]
        t1 = [P.sb("tt1%d" % i, [128, 2, 128], F32) for i in range(2)]
        t2 = [P.sb("tt2%d" % i, [128, 2, 128], F32) for i in range(2)]
        zo = [P.sb("zo%d" % i, [128, 1024], BF16) for i in range(3)]
        abv = ab_full.rearrange("(n1 n2) c -> n1 n2 c", n2=128)
        for n2 in range(128):
            fin = ins_[n2 % 3]
            P.dma("sp", fin[:], abv[:, n2, :], reads=[ab_fullb], writes=[fin])
            z = zo[n2 % 3]
            for hf in range(2):
                p1 = psb[P.alt("ps", 8)]
                p2 = psb[P.alt("ps", 8)]
                P.mm(p1, c128s[:, 0:128], fin[:, hf * 512:(hf + 1) * 512], True, True, reads=[c128s, fin])
                P.mm(p2, c128s[:, 128:256], fin[:, hf * 512:(hf + 1) * 512], True, True, reads=[c128s, fin])
                s2 = p2s[hf]
                P.op("act", lambda e: e.activation(out=s2[:], in_=p2[:], func=AF.Copy), reads=[p2], writes=[s2])
                p1v = p1[:].rearrange("p (g a c) -> p g a c", g=2, a=2)
                s2v = s2[:].rearrange("p (g a c) -> p g a c", g=2, a=2)
                Yr, Ym, T1, T2 = yr[hf], ym[hf], t1[hf], t2[hf]
                P.op("dve", lambda e: e.tensor_tensor(Yr[:], p1v[:, :, 0, :], s2v[:, :, 1, :], ALU.subtract),
                     reads=[p1, s2], writes=[Yr])
                P.op("dve", lambda e: e.tensor_tensor(Ym[:], p1v[:, :, 1, :], s2v[:, :, 0, :], ALU.add),
                     reads=[p1, s2], writes=[Ym])
                P.op("act", lambda e: e.activation(out=T1[:], in_=Yr[:], func=AF.Identity, scale=tws[:, 0, n2:n2 + 1]),
                     reads=[Yr, tws], writes=[T1])
                P.op("act", lambda e: e.activation(out=T2[:], in_=Ym[:], func=AF.Identity, scale=tws[:, 0, n2:n2 + 1]),
                     reads=[Ym, tws], writes=[T2])
                zr = z[:, hf * 256:(hf + 1) * 256].rearrange("p (g c) -> p g c", g=2)
                zi = z[:, 512 + hf * 256:512 + (hf + 1) * 256].rearrange("p (g c) -> p g c", g=2)
                P.op("dve", lambda e: e.scalar_tensor_tensor(zr, Ym[:], tws[:, 2, n2:n2 + 1], T1[:], ALU.mult, ALU.add),
                     reads=[Ym, T1, tws], writes=[z])
                P.op("dve", lambda e: e.scalar_tensor_tensor(zi, Yr[:], tws[:, 1, n2:n2 + 1], T2[:], ALU.mult, ALU.add),
                     reads=[Yr, T2, tws], writes=[z])
            P.dma("sp", Zd[:, n2, :], z[:], reads=[z], writes=[Zdb])
        P.release(mk)

        mk = P.mark()
        cs32s = P.sb("cs32s", [128, 64], BF16)
        P.dma("sp", cs32s[:], cs32, writes=[cs32s])
        zts = [P.sb("zt%d" % i, [128, 1024], BF16) for i in range(3)]
        fo_sb = P.sb("fo_sb", [128, 4, 32, 128], BF16)
        for k1 in range(128):
            zt = zts[k1 % 3]
            P.dma("sp", zt[:], Zd[k1, :, :], reads=[Zdb], writes=[zt])
            px = psb[P.alt("ps", 8)]
            for g in range(4):
                P.mm(px, zt[:, g * 128:(g + 1) * 128], cs32s[:, 0:32], True, False, reads=[zt, cs32s],
                     out=px[:, g * 32:(g + 1) * 32], last=False)
                P.mm(px, zt[:, 512 + g * 128:512 + (g + 1) * 128], cs32s[:, 32:64], False, True, reads=[zt, cs32s],
                     out=px[:, g * 32:(g + 1) * 32], last=(g == 3))
            P.op("act", lambda e: e.activation(out=fo_sb[:, :, :, k1],
                                               in_=px[:, 0:128].rearrange("p (g k) -> p g k", g=4), func=AF.Copy),
                 reads=[px], writes=[fo_sb])
        dstf = fouT_d.rearrange("(g p) t -> p g t", p=128)
        for g in range(4):
            P.dma("sp", dstf[:, g, :], fo_sb[:, g, :, :].rearrange("p a b -> p (a b)"), reads=[fo_sb], writes=[fouT_db])
        P.release(mk)
        post_mixer(1, od_w_out, spT_d, spT_db, fouT_d, fouT_db, h2T_d, h2T_db)

    P.barrier(engines=["sp"])
    P.close()
    return P


def build_E():
    P = Prog("E")
    nc = P.nc
    NL = 8
    NTK = SEQ
    u2T_all = P.inp("u2T_all", [D, NTK], BF16)
    gT_loc = P.inp("gT_loc", [NL, NTK])
    wgu_d = P.inp("wgu", [NL, D, 2 * D])
    wd_d = P.inp("wd", [NL, D, D])
    bguT = P.inp("bguT", [128, NL, 16])
    bd_d = P.inp("bd", [NL, D])
    y2p = nc.dram_tensor("y2p", [D, NTK], F32, kind="ExternalOutput").ap()
    y2pb = Buf("y2p")
    psb = P.psb
    QT = 1024
    u2q = P.sb("u2q", [128, 8, QT], BF16)
    y2 = P.sb("y2", [128, 8, QT], F32)
    gTqs = [P.sb("gTq%d" % i, [NL, QT], F32) for i in range(2)]
    bd = P.sb("bd", [NL, D], F32)
    P.dma("sp", bd[:], bd_d, writes=[bd])
    bgu = P.sb("bgu", [128, NL, 16], F32)
    P.dma("sp", bgu[:], bguT, writes=[bgu])
    wgus = [P.sb("wgu%d" % i, [128, 8, 2 * D], BF16) for i in range(2)]
    wds = [P.sb("wd%d" % i, [128, 8, D], BF16) for i in range(1)]
    Ge = [P.sb("Ge%d" % i, [128, 512], F32) for i in range(2)]
    xg = [P.sb("xg%d" % i, [128, 512], F32) for i in range(2)]
    sg = [P.sb("sgm%d" % i, [128, 512], F32) for i in range(2)]
    xl = [P.sb("xl%d" % i, [128, 512], F32) for i in range(2)]
    actb = [P.sb("actb%d" % i, [128, 8, 512], BF16) for i in range(2)]
    wgu_bf = nc.dram_tensor("wgu_bf", [NL, D, 2 * D], BF16, kind="Internal").ap()
    wd_bf = nc.dram_tensor("wd_bf", [NL, D, D], BF16, kind="Internal").ap()
    wgu_bfb = [Buf("wgu_bf%d" % i) for i in range(NL)]
    wd_bfb = [Buf("wd_bf%d" % i) for i in range(NL)]
    for ei in range(NL):
        stg = wgus[ei % 2]
        std = wds[0]
        srcg = wgu_d[ei].rearrange("(k p) n -> p k n", p=128)
        srcd = wd_d[ei].rearrange("(k p) n -> p k n", p=128)
        dstg = wgu_bf[ei].rearrange("(k p) n -> p k n", p=128)
        dstd = wd_bf[ei].rearrange("(k p) n -> p k n", p=128)
        for kk in range(8):
            P.dma("pool", stg[:, kk, :], srcg[:, kk, :], writes=[stg])
        for kk in range(8):
            P.dma("sp", dstg[:, kk, :], stg[:, kk, :], reads=[stg], writes=[wgu_bfb[ei]])
        for kk in range(8):
            P.dma("pool", std[:, kk, :], srcd[:, kk, :], writes=[std])
        for kk in range(8):
            P.dma("act", dstd[:, kk, :], std[:, kk, :], reads=[std], writes=[wd_bfb[ei]])
    for q in range(NTK // QT):
        q0 = q * QT
        gTq = gTqs[q % 2]
        srcu = u2T_all[:, q0:q0 + QT].rearrange("(k p) t -> p k t", p=128)
        for kk in range(8):
            P.dma("sp", u2q[:, kk, :], srcu[:, kk, :], writes=[u2q])
        P.dma("sp", gTq[:], gT_loc[:, q0:q0 + QT], writes=[gTq])
        for tt in range(QT // 512):
            for m in range(8):
                pb_ = psb[P.alt("ps", 8)]
                P.mm(pb_, bd[:, m * 128:(m + 1) * 128], gTq[:, tt * 512:(tt + 1) * 512], True, True, reads=[bd, gTq])
                P.op("act", lambda e: e.activation(out=y2[:, m, tt * 512:(tt + 1) * 512], in_=pb_[:], func=AF.Copy),
                     reads=[pb_], writes=[y2])
        for ei in range(NL):
            wgu = wgus[ei % 2]
            wd = wds[0]
            srcg = wgu_bf[ei].rearrange("(k p) n -> p k n", p=128)
            srcd = wd_bf[ei].rearrange("(k p) n -> p k n", p=128)
            for kk in range(8):
                P.dma("act" if kk % 2 else "sp", wgu[:, kk, :], srcg[:, kk, :], reads=[wgu_bfb[ei]], writes=[wgu])
            for kk in range(8):
                P.dma("act" if kk % 2 else "sp", wd[:, kk, :], srcd[:, kk, :], reads=[wd_bfb[ei]], writes=[wd])
            abs_ = []
            for tt in range(QT // 512):
                t0 = tt * 512
                G = Ge[P.alt("Ge", 2)]
                P.dma("sp", G[:], gT_loc[ei:ei + 1, q0 + t0:q0 + t0 + 512].partition_broadcast(128), writes=[G])
                ab = actb[P.alt("actb", 2)]
                for m in range(8):
                    pgl = psb[P.alt("ps", 8)]
                    pll = psb[P.alt("ps", 8)]
                    for kk in range(8):
                        P.mm(pgl, wgu[:, kk, m * 128:(m + 1) * 128], u2q[:, kk, t0:t0 + 512], kk == 0, kk == 7,
                             reads=[wgu, u2q])
                    for kk in range(8):
                        P.mm(pll, wgu[:, kk, D + m * 128:D + (m + 1) * 128], u2q[:, kk, t0:t0 + 512], kk == 0, kk == 7,
                             reads=[wgu, u2q])
                    a1 = xg[m % 2]
                    a2 = sg[m % 2]
                    a3 = xl[m % 2]
                    P.op("dve", lambda e: e.tensor_scalar(a1[:], pgl[:], bgu[:, ei, m:m + 1], 7.0, ALU.add, ALU.min),
                         reads=[pgl, bgu], writes=[a1])
                    P.op("act", lambda e: e.activation(out=a2[:], in_=a1[:], func=AF.Sigmoid, scale=1.702),
                         reads=[a1], writes=[a2])
                    P.op("dve", lambda e: e.tensor_scalar(a3[:], pll[:], bgu[:, ei, 8 + m:9 + m], -7.0, ALU.add, ALU.max),
                         reads=[pll, bgu], writes=[a3])
                    P.op("dve", lambda e: e.tensor_scalar(a3[:], a3[:], 7.0, 1.0, ALU.min, ALU.add),
                         reads=[], writes=[a3])
                    P.op("pool", lambda e: e.tensor_tensor(a1[:], a1[:], a2[:], ALU.mult), reads=[a2], writes=[a1])
                    P.op("pool", lambda e: e.tensor_tensor(a1[:], a1[:], a3[:], ALU.mult), reads=[a3], writes=[a1])
                    P.op("pool", lambda e: e.tensor_tensor(ab[:, m, :], a1[:], G[:], ALU.mult), reads=[a1, G], writes=[ab])
                abs_.append((ab, t0))
            for ab, t0 in abs_:
                for m in range(8):
                    pd = psb[P.alt("ps", 8)]
                    for kk in range(8):
                        P.mm(pd, wd[:, kk, m * 128:(m + 1) * 128], ab[:, kk, :], kk == 0, kk == 7, reads=[wd, ab])
                    P.op("dve", lambda e: e.tensor_tensor(y2[:, m, t0:t0 + 512], y2[:, m, t0:t0 + 512], pd[:], ALU.add),
                         reads=[pd], writes=[y2])
        dsty = y2p[:, q0:q0 + QT].rearrange("(k p) t -> p k t", p=128)
        for kk in range(8):
            P.dma("sp", dsty[:, kk, :], y2[:, kk, :], reads=[y2], writes=[y2pb])
    P.barrier(engines=["sp"])
    P.close()
    return P


def _fm(v, nch):
    return np.ascontiguousarray(np.asarray(v, np.float32).reshape(nch, 128).T)


def _rope_tables():
    t = np.arange(SEQ)
    row = (t // 64).astype(np.float64)
    col = (t % 64).astype(np.float64)
    inv = 10000.0 ** (-np.arange(16, dtype=np.float64) / 16)
    cos = np.zeros((64, SEQ))
    ssin = np.zeros((64, SEQ))
    for half, pos in enumerate((row, col)):
        ang = inv[:, None] * pos[None, :]
        c, s = np.cos(ang), np.sin(ang)
        b = half * 32
        cos[b:b + 16] = c
        cos[b + 16:b + 32] = c
        ssin[b:b + 16] = -s
        ssin[b + 16:b + 32] = s
    cos = np.concatenate([cos, cos], 0)
    ssin = np.concatenate([ssin, ssin], 0)
    return np.stack([cos, ssin], 1).astype(np.float32)


def _perm64():
    p = np.arange(64)
    out = p.copy()
    for b in (0, 32):
        out[b:b + 16] = p[b + 16:b + 32]
        out[b + 16:b + 32] = p[b:b + 16]
    return out


_CACHE = {}


def _get_prog(mode):
    if mode not in _CACHE:
        _CACHE[mode] = build_E() if mode == "E" else build(mode)
    return _CACHE[mode]


def _run(mode, maps):
    p = _get_prog(mode)
    maps = [{k: v for k, v in m.items() if k in p.ins} for m in maps]
    for m in maps:
        missing = [k for k in p.ins if k not in m]
        assert not missing, (mode, missing)
    return run_bass_kernel_spmd(p.nc, maps, core_ids=list(range(len(maps)))).results


def _common(inp):
    f32 = np.float32
    ln_vecs = np.zeros((128, 2, 4, 8), f32)
    for l in range(2):
        for i, nm in enumerate(("ln1_g", "ln1_b", "ln2_g", "ln2_b")):
            ln_vecs[:, l, i, :] = _fm(inp[nm][l], 8)
    return dict(ln_vecs=ln_vecs, router_w=np.asarray(inp["router_w"], f32),
                router_b=np.asarray(inp["router_b"], f32).reshape(2, 1, NE), ident=np.eye(128, dtype=f32))


def _maps_L1(inp):
    f32 = np.float32
    x = np.asarray(inp["x"], f32)
    ctx = np.asarray(inp["ctx"], f32)
    c = np.asarray(inp["c"], f32)
    c_ctx = np.asarray(inp["c_ctx"], f32)
    cs = _rope_tables()
    perm = np.concatenate([_perm64() + 64 * i for i in range(16)])
    w_in = np.asarray(inp["ev_w_in"][0], f32)
    w_qk_perm = np.ascontiguousarray(w_in[:, :1024][:, perm])
    b_modT = np.ascontiguousarray(np.asarray(inp["b_mod"], f32).reshape(2, 48, 128).transpose(2, 0, 1))
    common = _common(inp)
    xT = [np.ascontiguousarray(x[b].T) for b in range(NB)]
    maps = []
    for core in range(8):
        b, j = core // 4, core % 4
        t0 = j * TOK
        own = np.zeros((D, TOK + 30), f32)
        own[:, :TOK] = xT[b][:, t0:t0 + TOK]
        hmask = np.zeros((128, 30), f32)
        if t0 > 0:
            own[:, TOK:TOK + 15] = xT[b][:, t0 - 15:t0]
            hmask[:, 0:15] = 1.0
        if t0 + TOK < SEQ:
            own[:, TOK + 15:TOK + 30] = xT[b][:, t0 + TOK:t0 + TOK + 15]
            hmask[:, 15:30] = 1.0
        cT = np.zeros((128, 8, 2), f32)
        cT[:, :, 0] = _fm(c[b], 8)
        cT[:, :, 1] = _fm(c_ctx, 8)
        m = dict(common)
        m.update(
            xT_full=xT[b], xT_own=own, halo_mask=hmask, ctxT=np.ascontiguousarray(ctx[b].T),
            cT=cT.reshape(128, 16), w_mod=np.asarray(inp["w_mod"], f32), b_modT=b_modT,
            ev_w_in=w_in, w_qk_perm=w_qk_perm, cs_full=cs, cs_own=np.ascontiguousarray(cs[:, :, t0:t0 + TOK]),
            conv_wT=np.ascontiguousarray(np.asarray(inp["conv_w"][0], f32).T.reshape(4, 128, 31).transpose(1, 0, 2)),
            conv_vecs=np.ascontiguousarray(np.stack([_fm(inp["conv_b"][0], 4), _fm(inp["conv_ln_g"][0], 4),
                                                     _fm(inp["conv_ln_b"][0], 4)], 1)),
            lam_vecs=np.stack([inp["lam_q1"][0], inp["lam_k1"][0], inp["lam_q2"][0], inp["lam_k2"][0]], 0
                              ).astype(f32).reshape(1, 4, 64),
            diff_g=np.asarray(inp["diff_norm_g"][0], f32).reshape(128, 1),
            ev_w_out=np.asarray(inp["ev_w_out"][0], f32))
        maps.append(m)
    return maps


def _maps_E(inp, l, prev):
    f32 = np.float32
    maps = []
    for core in range(8):
        b, j = core // 4, core % 4
        e0 = 8 * j
        u2 = np.concatenate([np.asarray(prev[b * 4 + jj]["u2T_d"]) for jj in range(4)], 1)
        g = np.concatenate([np.asarray(prev[b * 4 + jj]["gT_d"], f32)[e0:e0 + 8] for jj in range(4)], 1)
        bgu = np.asarray(inp["b_gate_up"][l, e0:e0 + 8], f32).reshape(8, 16, 128).transpose(2, 0, 1)
        maps.append(dict(u2T_all=np.ascontiguousarray(u2), gT_loc=np.ascontiguousarray(g),
                         wgu=np.ascontiguousarray(inp["w_gate_up"][l, e0:e0 + 8], dtype=f32),
                         wd=np.ascontiguousarray(inp["w_down"][l, e0:e0 + 8], dtype=f32),
                         bguT=np.ascontiguousarray(bgu), bd=np.ascontiguousarray(inp["b_down"][l, e0:e0 + 8], dtype=f32)))
    return maps


def _partials(rE, core):
    b, j = core // 4, core % 4
    return np.ascontiguousarray(np.stack([np.asarray(rE[b * 4 + jj]["y2p"], np.float32)[:, j * TOK:(j + 1) * TOK]
                                          for jj in range(4)], 0))


def _maps_L3(inp, r1, rE):
    f32 = np.float32
    common = _common(inp)
    cidx = np.arange(128)
    ang = 2 * np.pi * np.outer(cidx, cidx) / 128.0
    sc = 1.0 / math.sqrt(SEQ * 128.0)
    dft_cs = np.concatenate([np.cos(ang) * sc, np.sin(ang) * sc], 1).astype(ml_dtypes.bfloat16)
    maps = []
    for core in range(8):
        m = dict(common)
        m.update(
            y2p4=_partials(rE, core), h1T_d=np.asarray(r1[core]["h1T_d"], f32), modT_d=np.asarray(r1[core]["modT_d"], f32),
            od_w_in=np.asarray(inp["od_w_in"][0], f32),
            gmlp_ln=np.stack([inp["gmlp_ln_g"][0], inp["gmlp_ln_b"][0]], 0).astype(f32).reshape(1, 2, 512),
            gmlp_wsT=np.ascontiguousarray(np.asarray(inp["gmlp_ws"][0], f32).transpose(2, 0, 1)),
            gmlp_bs=np.asarray(inp["gmlp_bs"][0], f32).reshape(1, 4, 128),
            four_ln=np.ascontiguousarray(np.stack([_fm(inp["four_ln_g"][0], 4), _fm(inp["four_ln_b"][0], 4)], 1)),
            dft_cs=dft_cs)
        maps.append(m)
    return maps


def _maps_L4(inp, r1, r3):
    f32 = np.float32
    common = _common(inp)
    idx = np.arange(128)
    ang = 2 * np.pi * np.outer(idx, idx) / 128.0
    c128 = np.concatenate([np.cos(ang), np.sin(ang)], 1).astype(ml_dtypes.bfloat16)
    angt = 2 * np.pi * np.outer(idx, idx) / float(SEQ)
    tw = np.ascontiguousarray(np.stack([np.cos(angt), np.sin(angt), -np.sin(angt)], 1).astype(f32))
    maps = []
    for core in range(8):
        b, j = core // 4, core % 4
        k2 = 32 * j + np.arange(32)
        a2 = 2 * np.pi * np.outer(idx, k2) / 128.0
        cs32 = np.concatenate([np.cos(a2), -np.sin(a2)], 1).astype(ml_dtypes.bfloat16)
        ab_full = np.concatenate([np.asarray(r3[b * 4 + jj]["ab_own"]) for jj in range(4)], 0)
        m = dict(common)
        m.update(od_w_out=np.asarray(inp["od_w_out"][0], f32), c128=c128, tw_c=tw, cs32=cs32,
                 ab_full=np.ascontiguousarray(ab_full), spT_d=np.asarray(r3[core]["spT_d"]),
                 h2T_d=np.asarray(r3[core]["h2T_d"], f32), modT_d=np.asarray(r1[core]["modT_d"], f32))
        maps.append(m)
    return maps


def _maps_L6(inp, r1, r4, rE):
    f32 = np.float32
    common = _common(inp)
    maps = []
    for core in range(8):
        m = dict(common)
        m.update(y2p4=_partials(rE, core), h1T_d=np.asarray(r4[core]["h1T_d"], f32),
                 modT_d=np.asarray(r1[core]["modT_d"], f32))
        maps.append(m)
    return maps


def kernel(**inputs):
    r1 = _run("L1", _maps_L1(inputs))
    rE0 = _run("E", _maps_E(inputs, 0, r1))
    r3 = _run("L3", _maps_L3(inputs, r1, rE0))
    del rE0
    r4 = _run("L4", _maps_L4(inputs, r1, r3))
    rE1 = _run("E", _maps_E(inputs, 1, r4))
    r6 = _run("L6", _maps_L6(inputs, r1, r4, rE1))
    out = np.zeros((NB, SEQ, D), np.float32)
    for core in range(8):
        b, j = core // 4, core % 4
        out[b, j * TOK:(j + 1) * TOK, :] = np.asarray(r6[core]["outT"], np.float32).T
    return out
```
